# Optimizing a Trainium2 kernel written in Bass

```python
import jax, jax.numpy as jnp
from jax import lax

D_MODEL = 1024
BATCH = 8
SEQ = 2048
DEPTH = 4

GRID_W = 64
CTX_LEN = 256
N_MIXERS = 2
N_LRU_LAYERS = (DEPTH + 1) // 2
N_ATTN_LAYERS = DEPTH // 2
LRU_WIDTH = D_MODEL
LRU_BLOCKS = 8
LRU_BLOCK_W = LRU_WIDTH // LRU_BLOCKS
CONV_W = 4
LRU_C = 8.0
HEAD_DIM = 128
N_HEADS = D_MODEL // HEAD_DIM
N_KV_HEADS = 2
GROUP = N_HEADS // N_KV_HEADS
WINDOW = 128
QBLOCK = 128
QKV_WIDTH = (N_HEADS + 2 * N_KV_HEADS) * HEAD_DIM
ROPE_BASE = 10000.0
N_EXPERTS = 16
EXPERT_FF = 1024
CAPACITY_FACTOR = 2
NORM_EPS = 1e-6
NEG_INF = -1e30

kernel_name = "hybrid_rglru_swa_ecmoe_diffusion"


def rmsnorm(x, g):
    xf = x.astype(jnp.float32)
    y = xf * lax.rsqrt(jnp.mean(xf * xf, axis=-1, keepdims=True) + NORM_EPS)
    return y.astype(x.dtype) * g


def modulate(h, shift, scale):
    return h * (1 + scale) + shift


def short_conv(x, w, b):
    n = x.shape[1]
    left = (CONV_W - 1) // 2
    right = CONV_W - 1 - left
    xp = jnp.pad(x, ((0, 0), (left, right), (0, 0)))
    return sum(xp[:, k:k + n] * w[k] for k in range(CONV_W)) + b


def _lin_combine(e1, e2):
    a1, b1 = e1
    a2, b2 = e2
    return a1 * a2, a2 * b1 + b2


def linear_scan(a, b, h0, reverse):
    A, H = lax.associative_scan(_lin_combine, (a, b), axis=1, reverse=reverse)
    if h0 is None:
        return H
    return H + A * h0[:, None]


def rglru_gates(xb, gate_w, gate_b, lam):
    bsz, n = xb.shape[:2]
    xg = xb.reshape(bsz, n, LRU_BLOCKS, LRU_BLOCK_W)
    gates = (jnp.einsum('bnhi,hij->bnhj', xg, gate_w.astype(jnp.float32))
             + gate_b.astype(jnp.float32))
    r = jax.nn.sigmoid(gates[..., :LRU_BLOCK_W]).reshape(bsz, n, LRU_WIDTH)
    i = jax.nn.sigmoid(gates[..., LRU_BLOCK_W:]).reshape(bsz, n, LRU_WIDTH)
    log_a = LRU_C * r * jax.nn.log_sigmoid(lam.astype(jnp.float32))
    a = jnp.exp(log_a)
    b = jnp.sqrt(-jnp.expm1(2.0 * log_a)) * (i * xb)
    return a, b


def rglru_mixer(h_lat, h_ctx, w_in, conv_w, conv_b, gate_w, gate_b, lam, w_out, need_ctx):
    def branches(h):
        u = h @ w_in
        y = jax.nn.gelu(u[..., :LRU_WIDTH])
        xb = short_conv(u[..., LRU_WIDTH:], conv_w, conv_b).astype(jnp.float32)
        return y, xb

    y_l, x_l = branches(h_lat)
    y_c, x_c = branches(h_ctx)
    s_l = 0.0
    s_c = 0.0
    for d, rev in enumerate((False, True)):
        a_c, b_c = rglru_gates(x_c, gate_w[d], gate_b[d], lam[d])
        hc = linear_scan(a_c, b_c, None, rev)
        h0 = hc[:, 0] if rev else hc[:, -1]
        a_l, b_l = rglru_gates(x_l, gate_w[d], gate_b[d], lam[d])
        hl = linear_scan(a_l, b_l, h0, rev)
        s_l = s_l + hl
        s_c = s_c + hc
    out_l = (s_l.astype(h_lat.dtype) * y_l) @ w_out
    if not need_ctx:
        return out_l, None
    out_c = (s_c.astype(h_ctx.dtype) * y_c) @ w_out
    return out_l, out_c


def rope_axial(x, rows, cols):
    half = HEAD_DIM // 2
    freqs = ROPE_BASE ** (-jnp.arange(0, half, 2, dtype=jnp.float32) / half)

    def rot(xp, pos):
        ang = pos.astype(jnp.float32)[:, None] * freqs
        cos = jnp.cos(ang)[None, :, None].astype(x.dtype)
        sin = jnp.sin(ang)[None, :, None].astype(x.dtype)
        x1, x2 = jnp.split(xp, 2, axis=-1)
        return jnp.concatenate([x1 * cos - x2 * sin, x1 * sin + x2 * cos], axis=-1)

    return jnp.concatenate([rot(x[..., :half], rows), rot(x[..., half:], cols)], axis=-1)


def split_qkv(h, w_qkv):
    bsz, n = h.shape[:2]
    u = h @ w_qkv
    nq = N_HEADS * HEAD_DIM
    nk = N_KV_HEADS * HEAD_DIM
    q = u[..., :nq].reshape(bsz, n, N_HEADS, HEAD_DIM)
    k = u[..., nq:nq + nk].reshape(bsz, n, N_KV_HEADS, HEAD_DIM)
    v = u[..., nq + nk:].reshape(bsz, n, N_KV_HEADS, HEAD_DIM)
    return q, k, v


def attn_mixer(h_lat, h_ctx, w_qkv, sink, w_o, need_ctx):
    bsz, seq, _ = h_lat.shape
    lc = h_ctx.shape[1]
    rows_n = seq // GRID_W
    rows = jnp.repeat(jnp.arange(rows_n), GRID_W)
    cols = jnp.tile(jnp.arange(GRID_W), rows_n)
    scale = HEAD_DIM ** -0.5

    q, k, v = split_qkv(h_lat, w_qkv)
    q = rope_axial(q, rows, cols)
    k = rope_axial(k, rows, cols)
    qc, kc, vc = split_qkv(h_ctx, w_qkv)

    nblk = seq // QBLOCK
    qb = (q * scale).reshape(bsz, nblk, QBLOCK, N_KV_HEADS, GROUP, HEAD_DIM)

    def band(t):
        tp = jnp.pad(t, ((0, 0), (QBLOCK, QBLOCK), (0, 0), (0, 0)))
        tp = tp.reshape(bsz, nblk + 2, QBLOCK, N_KV_HEADS, HEAD_DIM)
        return jnp.concatenate([tp[:, j:j + nblk] for j in range(3)], axis=2)

    kb, vb = band(k), band(v)
    s_band = jnp.einsum('bnqkgd,bnskd->bnkgqs', qb, kb).astype(jnp.float32)
    qi = jnp.arange(QBLOCK)[:, None]
    sj = jnp.arange(3 * QBLOCK)[None, :]
    in_win = jnp.abs(sj - QBLOCK - qi) <= WINDOW
    kpos = jnp.arange(nblk)[:, None] * QBLOCK - QBLOCK + jnp.arange(3 * QBLOCK)[None, :]
    valid = (kpos >= 0) & (kpos < seq)
    mask = in_win[None] & valid[:, None, :]
    s_band = jnp.where(mask[None, :, None, None], s_band, NEG_INF)
    s_ctx = jnp.einsum('bnqkgd,bckd->bnkgqc', qb, kc).astype(jnp.float32)
    sink_h = sink.astype(jnp.float32).reshape(N_KV_HEADS, GROUP)
    sink_l = jnp.broadcast_to(sink_h[None, None, :, :, None, None], s_band.shape[:-1] + (1,))
    p = jax.nn.softmax(jnp.concatenate([s_band, s_ctx, sink_l], axis=-1), axis=-1)
    p_band = p[..., :3 * QBLOCK].astype(v.dtype)
    p_ctx = p[..., 3 * QBLOCK:3 * QBLOCK + lc].astype(v.dtype)
    o = (jnp.einsum('bnkgqs,bnskd->bnqkgd', p_band, vb)
         + jnp.einsum('bnkgqc,bckd->bnqkgd', p_ctx, vc))
    out_l = o.reshape(bsz, seq, N_HEADS * HEAD_DIM) @ w_o
    if not need_ctx:
        return out_l, None
    qcg = (qc * scale).reshape(bsz, lc, N_KV_HEADS, GROUP, HEAD_DIM)
    sc = jnp.einsum('bqkgd,bckd->bkgqc', qcg, kc).astype(jnp.float32)
    sink_c = jnp.broadcast_to(sink_h[None, :, :, None, None], sc.shape[:-1] + (1,))
    pc = jax.nn.softmax(jnp.concatenate([sc, sink_c], axis=-1), axis=-1)[..., :lc].astype(vc.dtype)
    oc = jnp.einsum('bkgqc,bckd->bqkgd', pc, vc).reshape(bsz, lc, N_HEADS * HEAD_DIM) @ w_o
    return out_l, oc


def ec_moe(h, router, w_gate, w_up, w_down):
    bsz, n, d = h.shape
    cap = CAPACITY_FACTOR * n // N_EXPERTS
    aff = jax.nn.softmax((h @ router).astype(jnp.float32), axis=-1)
    g, idx = lax.top_k(jnp.swapaxes(aff, 1, 2), cap)
    xg = jax.vmap(lambda hb, ib: hb[ib])(h, idx)
    a = jnp.einsum('becd,edf->becf', xg, w_gate)
    u = jnp.einsum('becd,edf->becf', xg, w_up)
    y = jnp.einsum('becf,efd->becd', jax.nn.silu(a) * u, w_down) * g[..., None].astype(h.dtype)
    return jax.vmap(lambda yb, ib: jnp.zeros((n, d), yb.dtype).at[ib.reshape(-1)].add(yb.reshape(-1, d)))(y, idx)


def setup_inputs(seed: int = 0) -> dict:
    key = jax.random.key(seed)
    ks = jax.random.split(key, 24)
    f32 = jnp.float32
    nrm = lambda k, shape, s: jax.random.normal(k, shape, f32) * s
    u = jax.random.uniform(ks[12], (N_LRU_LAYERS, 2, LRU_WIDTH), f32, minval=0.9, maxval=0.999)
    p = u ** (1.0 / LRU_C)
    lru_lambda = jnp.log(p) - jnp.log1p(-p)
    return {
        "x": nrm(ks[0], (BATCH, SEQ, D_MODEL), 1.0),
        "c": nrm(ks[1], (BATCH, D_MODEL), 1.0),
        "ctx": nrm(ks[2], (BATCH, CTX_LEN, D_MODEL), 1.0),
        "c_ctx": nrm(ks[3], (D_MODEL,), 1.0),
        "ada_w": nrm(ks[4], (DEPTH, D_MODEL, 6 * D_MODEL), 0.5 * D_MODEL ** -0.5),
        "ada_b": nrm(ks[5], (DEPTH, 6 * D_MODEL), 0.02),
        "norm1_g": 1.0 + nrm(ks[6], (DEPTH, D_MODEL), 0.02),
        "norm2_g": 1.0 + nrm(ks[7], (DEPTH, D_MODEL), 0.02),
        "lru_w_in": nrm(ks[8], (N_LRU_LAYERS, D_MODEL, 2 * LRU_WIDTH), D_MODEL ** -0.5),
        "lru_conv_w": nrm(ks[9], (N_LRU_LAYERS, CONV_W, LRU_WIDTH), CONV_W ** -0.5),
        "lru_conv_b": nrm(ks[10], (N_LRU_LAYERS, LRU_WIDTH), 0.02),
        "lru_gate_w": nrm(ks[11], (N_LRU_LAYERS, 2, LRU_BLOCKS, LRU_BLOCK_W, 2 * LRU_BLOCK_W), LRU_BLOCK_W ** -0.5),
        "lru_gate_b": nrm(ks[13], (N_LRU_LAYERS, 2, LRU_BLOCKS, 2 * LRU_BLOCK_W), 0.02),
        "lru_lambda": lru_lambda,
        "lru_w_out": nrm(ks[14], (N_LRU_LAYERS, LRU_WIDTH, D_MODEL), LRU_WIDTH ** -0.5),
        "attn_w_qkv": nrm(ks[15], (N_ATTN_LAYERS, D_MODEL, QKV_WIDTH), D_MODEL ** -0.5),
        "attn_sink": nrm(ks[16], (N_ATTN_LAYERS, N_HEADS), 0.5),
        "attn_w_o": nrm(ks[17], (N_ATTN_LAYERS, N_HEADS * HEAD_DIM, D_MODEL), (N_HEADS * HEAD_DIM) ** -0.5),
        "moe_router": nrm(ks[18], (DEPTH, D_MODEL, N_EXPERTS), D_MODEL ** -0.5),
        "moe_w_gate": nrm(ks[19], (DEPTH, N_EXPERTS, D_MODEL, EXPERT_FF), D_MODEL ** -0.5),
        "moe_w_up": nrm(ks[20], (DEPTH, N_EXPERTS, D_MODEL, EXPERT_FF), D_MODEL ** -0.5),
        "moe_w_down": nrm(ks[21], (DEPTH, N_EXPERTS, EXPERT_FF, D_MODEL), EXPERT_FF ** -0.5),
        "final_g": 1.0 + nrm(ks[22], (D_MODEL,), 0.02),
    }


def reference(x, c, ctx, c_ctx, ada_w, ada_b, norm1_g, norm2_g,
              lru_w_in, lru_conv_w, lru_conv_b, lru_gate_w, lru_gate_b, lru_lambda, lru_w_out,
              attn_w_qkv, attn_sink, attn_w_o,
              moe_router, moe_w_gate, moe_w_up, moe_w_down, final_g):
    sc = jax.nn.silu(c)[:, None, :]
    sc_ctx = jax.nn.silu(c_ctx)
    for l in range(DEPTH):
        need_ctx = l < DEPTH - 1
        sh1, sc1, g1, sh2, sc2, g2 = jnp.split(sc @ ada_w[l] + ada_b[l], 6, axis=-1)
        ch1, cs1, cg1, ch2, cs2, cg2 = jnp.split(sc_ctx @ ada_w[l] + ada_b[l], 6, axis=-1)

        h_l = modulate(rmsnorm(x, norm1_g[l]), sh1, sc1)
        h_c = modulate(rmsnorm(ctx, norm1_g[l]), ch1, cs1)
        j = l // N_MIXERS
        if l % N_MIXERS == 0:
            y_l, y_c = rglru_mixer(h_l, h_c, lru_w_in[j], lru_conv_w[j], lru_conv_b[j],
                                   lru_gate_w[j], lru_gate_b[j], lru_lambda[j], lru_w_out[j], need_ctx)
        else:
            y_l, y_c = attn_mixer(h_l, h_c, attn_w_qkv[j], attn_sink[j], attn_w_o[j], need_ctx)
        x = x + g1 * y_l
        if need_ctx:
            ctx = ctx + cg1 * y_c

        h_l = modulate(rmsnorm(x, norm2_g[l]), sh2, sc2)
        x = x + g2 * ec_moe(h_l, moe_router[l], moe_w_gate[l], moe_w_up[l], moe_w_down[l])
        if need_ctx:
            h_c = modulate(rmsnorm(ctx, norm2_g[l]), ch2, cs2)
            ctx = ctx + cg2 * ec_moe(h_c, moe_router[l], moe_w_gate[l], moe_w_up[l], moe_w_down[l])
    return rmsnorm(x, final_g)
```

```python
import numpy as np
import concourse.bass as bass
import concourse.mybir as mybir

F32 = mybir.dt.float32
BF16 = mybir.dt.bfloat16
I32 = mybir.dt.int32
U32 = mybir.dt.uint32
AF = mybir.ActivationFunctionType
ALU = mybir.AluOpType
AX = mybir.AxisListType

ENGS = ['pe', 'act', 'dve', 'pool', 'sp']
NDSEM = 6
PE_DELAY_OPS = {'act': 1, 'dve': 4, 'pool': 4}


def _prod(xs):
    r = 1
    for v in xs:
        r *= int(v)
    return r


def region(ap):
    t = ap.tensor
    name = ap.name
    shape = tuple(t.shape)
    pairs = [(int(s), int(c)) for s, c in ap.ap]
    off = int(ap.offset)
    sp = str(ap.space)
    if 'DRAM' in sp.upper() or 'HBM' in sp.upper():
        lo = off
        hi = off
        for s, c in pairs:
            if s >= 0:
                hi += s * (c - 1)
            else:
                lo += s * (c - 1)
        return (name, 0, 1, lo, hi + 1)
    fsz = _prod(shape[1:])
    p0 = off // fsz
    rem = off % fsz
    ps, pc = pairs[0]
    p1 = p0 + (ps // fsz) * (pc - 1) + 1 if pc > 1 else p0 + 1
    lo = rem
    hi = rem
    for s, c in pairs[1:]:
        if s >= 0:
            hi += s * (c - 1)
        else:
            lo += s * (c - 1)
    return (name, p0, p1, lo, hi + 1)


def _overlap(a, b):
    return a[1] < b[2] and b[1] < a[2] and a[3] < b[4] and b[3] < a[4]


def _contains(a, b):
    return a[1] <= b[1] and b[2] <= a[2] and a[3] <= b[3] and b[4] <= a[4]


class Prog:
    def __init__(self, nc, stack):
        self.nc = nc
        self.ops = {e: [] for e in ENGS}
        self.cnt = {e: 0 for e in ENGS}
        self.sems = {}
        for e in ENGS:
            self.sems[('e', e)] = stack.enter_context(nc.semaphore(f"s_{e}"))
        self.dcnt = {}
        self.dnext = {}
        for q in ['sp', 'act', 'pool']:
            self.dnext[q] = 0
            for i in range(NDSEM):
                self.sems[('d', q, i)] = stack.enter_context(nc.semaphore(f"d_{q}{i}"))
                self.dcnt[(q, i)] = 0
        self.waited = {e: {} for e in ENGS}
        self.wr = {}
        self.rd = {}
        self.nwaits = 0
        self.flush = False
        self.pe_delay = True
        self.scratch = {}

    def _deps(self, reads, writes, token):
        deps = {}

        def add(k, v):
            if deps.get(k, 0) < v:
                deps[k] = v
        for ap in reads:
            r = region(ap)
            w = self.wr.setdefault(r[0], {})
            for (rg, sk), v in w.items():
                if _overlap(rg, r):
                    add(sk, v)
        for ap in writes:
            r = region(ap)
            w = self.wr.setdefault(r[0], {})
            d = self.rd.setdefault(r[0], {})
            for tab in (w, d):
                dead = []
                for (rg, sk), v in tab.items():
                    if _overlap(rg, r):
                        add(sk, v)
                        if _contains(r, rg):
                            dead.append((rg, sk))
                for k in dead:
                    del tab[k]
        for ap in reads:
            r = region(ap)
            self.rd.setdefault(r[0], {})[(r, token[0])] = token[1]
        for ap in writes:
            r = region(ap)
            self.wr.setdefault(r[0], {})[(r, token[0])] = token[1]
        return deps

    def _emit_waits(self, eng, deps):
        pe_wait = False
        for sk, v in deps.items():
            if eng == 'pe' and sk == ('e', 'pe'):
                continue
            if self.waited[eng].get(sk, 0) >= v:
                continue
            self.waited[eng][sk] = v
            self.ops[eng].append(('wait', sk, v))
            self.nwaits += 1
            if sk == ('e', 'pe'):
                pe_wait = True
        if pe_wait and self.pe_delay and eng in self.scratch:
            sc = self.scratch[eng]
            for _ in range(PE_DELAY_OPS[eng]):
                if eng == 'act':
                    self.ops[eng].append(('raw', lambda e: e.copy(sc[:], sc[:])))
                else:
                    self.ops[eng].append(('raw', lambda e: e.memset(sc[:], 0.0)))

    def op(self, eng, fn, reads=(), writes=()):
        fl = self.flush and eng in ('act', 'dve', 'pool') and eng in self.scratch
        token = (('e', eng), self.cnt[eng] + (2 if fl else 1))
        deps = self._deps(list(reads), list(writes), token)
        self._emit_waits(eng, deps)
        self.cnt[eng] += 1
        self.ops[eng].append(('op', fn, token[0]))
        if fl:
            sc = self.scratch[eng]
            self.cnt[eng] += 1
            if eng == 'act':
                self.ops[eng].append(('op', lambda e: e.copy(sc[:], sc[:]), token[0]))
            else:
                self.ops[eng].append(('op', lambda e: e.memset(sc[:], 0.0), token[0]))

    def dma(self, q, out, in_, **kw):
        i = self.dnext[q] % NDSEM
        self.dnext[q] += 1
        sk = ('d', q, i)
        prev = self.dcnt[(q, i)]
        self.dcnt[(q, i)] = prev + 16
        token = (sk, prev + 16)
        deps = self._deps([in_], [out], token)
        if prev > 0:
            if deps.get(sk, 0) < prev:
                deps[sk] = prev
        self._emit_waits(q, deps)
        self.ops[q].append(('dma', (out, in_, kw), sk))

    def dma_custom(self, q, out, in_, fn, extra_reads=()):
        i = self.dnext[q] % NDSEM
        self.dnext[q] += 1
        sk = ('d', q, i)
        prev = self.dcnt[(q, i)]
        self.dcnt[(q, i)] = prev + 16
        token = (sk, prev + 16)
        deps = self._deps([in_] + list(extra_reads), [out], token)
        if prev > 0:
            if deps.get(sk, 0) < prev:
                deps[sk] = prev
        self._emit_waits(q, deps)
        self.ops[q].append(('dmac', (out, in_, fn), sk))

    def barrier(self):
        for e in ENGS:
            deps = {}
            for e2 in ENGS:
                if e2 != e and self.cnt[e2] > 0:
                    deps[('e', e2)] = self.cnt[e2]
            if e != 'pe' and self.cnt[e] > 0:
                deps[('e', e)] = self.cnt[e]
            for (q, i), v in self.dcnt.items():
                if v > 0:
                    deps[('d', q, i)] = v
            self._emit_waits(e, deps)
        self.wr = {}
        self.rd = {}

    def emit(self):
        nc = self.nc
        sems = self.sems
        ops = self.ops

        def run(eng_name, eng):
            for item in ops[eng_name]:
                if item[0] == 'wait':
                    eng.wait_ge(sems[item[1]], item[2])
                elif item[0] == 'op':
                    ins = item[1](eng)
                    ins.then_inc(sems[item[2]], 1)
                elif item[0] == 'raw':
                    item[1](eng)
                elif item[0] == 'dmac':
                    out, in_, fn = item[1]
                    fn(eng, out, in_, {}).then_inc(sems[item[2]], 16)
                else:
                    out, in_, kw = item[1]
                    eng.dma_start(out=out, in_=in_, **kw).then_inc(sems[item[2]], 16)

        with nc.Block() as block:
            @block.tensor
            def _(e):
                run('pe', e)

            @block.scalar
            def _(e):
                run('act', e)

            @block.vector
            def _(e):
                run('dve', e)

            @block.gpsimd
            def _(e):
                run('pool', e)

            @block.sync
            def _(e):
                run('sp', e)

from contextlib import ExitStack
import os
MOE_CUT = int(os.environ.get('MOE_CUT', '99'))
MOE_E0 = int(os.environ.get('MOE_E0', '0'))
MOE_E1 = int(os.environ.get('MOE_E1', '16'))
from concourse.bass_utils import run_bass_kernel_spmd

D = 1024
KC = 8
NL = 2048
NCX = 256
T = NL + NCX
DEPTH = 4
NE = 16
CAPL = 256
CAPC = 32
EPS = 1e-6
QSCALE = 128 ** -0.5
TILES = [(0, 256, 1), (256, 512, 0), (768, 512, 0), (1280, 512, 0), (1792, 512, 0)]


class B:
    def __init__(self, nc, P, st):
        self.nc = nc
        self.P = P
        self.st = st
        self.psn = 0
        self.rr = 0

    def mm(self, out, lhsT, rhs, start=True, stop=True):
        self.P.op('pe', lambda e: e.matmul(out, lhsT, rhs, start=start, stop=stop),
                  reads=[lhsT, rhs], writes=[out])

    def tr(self, out, in_, ident):
        self.mm(out, in_, ident)

    def act(self, out, in_, func, bias=None, scale=None):
        kw = {}
        rd = [in_]
        if bias is not None:
            kw['bias'] = bias
            if not isinstance(bias, float):
                rd.append(bias)
        if scale is not None:
            kw['scale'] = scale
            if not isinstance(scale, float):
                rd.append(scale)
        self.P.op('act', lambda e: e.activation(out, in_, func, **kw), reads=rd, writes=[out])

    def tt(self, eng, out, in0, in1, op):
        self.P.op(eng, lambda e: e.tensor_tensor(out, in0, in1, op), reads=[in0, in1], writes=[out])

    def ts(self, eng, out, in0, s1, s2, op0, op1=None):
        rd = [in0]
        if not isinstance(s1, float):
            rd.append(s1)
        if s2 is not None and not isinstance(s2, float):
            rd.append(s2)
        if op1 is None:
            self.P.op(eng, lambda e: e.tensor_scalar(out, in0, s1, None, op0), reads=rd, writes=[out])
        else:
            self.P.op(eng, lambda e: e.tensor_scalar(out, in0, s1, s2, op0, op1), reads=rd, writes=[out])

    def stt(self, eng, out, in0, scalar, in1, op0, op1):
        rd = [in0, in1]
        if not isinstance(scalar, float):
            rd.append(scalar)
        self.P.op(eng, lambda e: e.scalar_tensor_tensor(out, in0, scalar, in1, op0, op1), reads=rd, writes=[out])

    def cp(self, eng, out, in_):
        if eng == 'act':
            self.P.op('act', lambda e: e.copy(out, in_), reads=[in_], writes=[out])
        else:
            self.P.op(eng, lambda e: e.tensor_copy(out, in_), reads=[in_], writes=[out])

    def memset(self, eng, out, val):
        self.P.op(eng, lambda e: e.memset(out, val), reads=[], writes=[out])

    def scan(self, out, a, b, init):
        rd = [a, b]
        if not isinstance(init, float):
            rd.append(init)
        self.P.op('dve', lambda e: e.tensor_tensor_scan(out, a, b, init, ALU.mult, ALU.add), reads=rd, writes=[out])

    def dma(self, q, out, in_):
        self.P.dma(q, out, in_)

    def ps(self):
        b = self.banks[self.psn % len(self.banks)]
        self.psn += 1
        return b

    def alt(self, engs=('act', 'dve')):
        self.rr += 1
        return engs[self.rr % len(engs)]


def build_program(stop_after=None, n_layers=DEPTH):
    nc = bass.Bass("TRN2", target_bir_lowering=False)
    dram = {}

    def din(name, shape, dt=F32):
        dram[name] = nc.dram_tensor(name, list(shape), dt, kind="ExternalInput").ap()
        return dram[name]

    xT_d = din("xT", [D, T])
    cc_d = din("cc", [128, KC, 2])
    ada_w_d = din("ada_w", [DEPTH, D, 6 * D])
    ada_b_d = din("ada_b", [128, DEPTH, 48])
    n1g_d = din("n1g", [128, DEPTH, KC])
    n2g_d = din("n2g", [128, DEPTH, KC])
    fing_d = din("fing", [128, KC])
    lwin_d = din("lru_w_in", [2, D, 2 * D])
    lwout_d = din("lru_w_out", [2, D, D])
    convw_d = din("conv_w", [128, 2, KC, 4])
    convb_d = din("conv_b", [128, 2, KC])
    gatew_d = din("gate_w", [2, 2, 8, 128, 256])
    gateb_d = din("gate_b", [128, 2, 2, 8, 2])
    lam_d = din("lam", [128, 2, 2, 8])
    wqkv_d = din("w_qkv", [2, D, 1536])
    wqsw_d = din("w_qkv_sw", [2, D, 1280])
    sink_d = din("sink", [128, 2, 8])
    wo_d = din("w_o", [2, D, D])
    router_d = din("router", [128, DEPTH, KC, NE])
    wg_d = din("moe_w_gate", [DEPTH, NE, D, D])
    wu_d = din("moe_w_up", [DEPTH, NE, D, D])
    wd_d = din("moe_w_down", [DEPTH, NE, D, D])
    ident_d = din("ident", [128, 128])
    ropec_d = din("rope_c", [128, NL])
    ropes_d = din("rope_s", [128, NL])
    iota_d = din("iota_row", [128, NL])
    tokidx_d = din("tokidx", [128, 16])
    mprev_d = din("mask_prev", [128, 128])
    mnext_d = din("mask_next", [128, 128])
    pidx_d = din("pidx16", [16, 128])
    halfoff_d = din("halfoff", [32, 1])
    out_d = nc.dram_tensor("outT", [D, NL], F32, kind="ExternalOutput").ap()
    scr_d = nc.dram_tensor("scr_tabs", [DEPTH, 2, 16, 288], F32).ap()
    scr2_d = nc.dram_tensor("scr_half", [DEPTH, 2, 32, CAPL], F32).ap()
    hscr_d = [nc.dram_tensor(f"scr_htok{i}", [T, D], BF16).ap() for i in range(DEPTH)]
    dbg_d = None
    if stop_after is not None:
        dbg_d = nc.dram_tensor("dbgx", [D, T], F32, kind="ExternalOutput").ap()
        dbg_aff = nc.dram_tensor("dbg_aff", [16, T], F32, kind="ExternalOutput").ap()
        dbg_idx = nc.dram_tensor("dbg_idx", [16, 288], F32, kind="ExternalOutput").ap()
        dbg_val = nc.dram_tensor("dbg_val", [16, 288], F32, kind="ExternalOutput").ap()
        dbg_it = nc.dram_tensor("dbg_it", [128, 3, NE], F32, kind="ExternalOutput").ap()
        dbg_vt = nc.dram_tensor("dbg_vt", [128, 3, NE], F32, kind="ExternalOutput").ap()

    with ExitStack() as st:
        P = Prog(nc, st)
        bld = B(nc, P, st)
        mm, act, tt, ts, stt, cp, dma = bld.mm, bld.act, bld.tt, bld.ts, bld.stt, bld.cp, bld.dma

        def SB(stack, name, shape, dt):
            return stack.enter_context(nc.sbuf_tensor(name, list(shape), dt))

        banks = [st.enter_context(nc.psum_tensor(f"ps{i}", [128, 512], F32)) for i in range(7)]
        psb = st.enter_context(nc.psum_tensor("psb", [128, 1024], BF16))
        bld.banks = banks
        for en in ('act', 'dve', 'pool'):
            P.scratch[en] = SB(st, f"flush_{en}", [128, 1], F32)
        P.flush = os.environ.get('FLUSH', '0') == '1'
        P.pe_delay = os.environ.get('PE_DELAY', '1') == '1'

        xT = SB(st, "xT_sb", [128, KC, T], F32)
        ident_f = SB(st, "ident_f", [128, 128], F32)
        ident_b = SB(st, "ident_b", [128, 128], BF16)
        ones_b = SB(st, "ones_b", [128, 128], BF16)
        mod = SB(st, "mod", [128, DEPTH, 48, 2], F32)
        gp1 = SB(st, "gp1", [128, DEPTH, KC, 2], F32)
        gp2 = SB(st, "gp2", [128, DEPTH, KC, 2], F32)
        n1g = SB(st, "n1g_sb", [128, DEPTH, KC], F32)
        n2g = SB(st, "n2g_sb", [128, DEPTH, KC], F32)
        fing = SB(st, "fing_sb", [128, KC], F32)
        ada_b = SB(st, "ada_b_sb", [128, DEPTH, 48], F32)
        convw = SB(st, "convw_sb", [128, 2, KC, 4], F32)
        convb = SB(st, "convb_sb", [128, 2, KC], F32)
        gateb = SB(st, "gateb_sb", [128, 2, 2, 8, 2], F32)
        gatebh = SB(st, "gatebh_sb", [128, 2, 2, 8, 2], F32)
        lam = SB(st, "lam_sb", [128, 2, 2, 8], F32)
        ls4 = SB(st, "ls4", [128, 2, 2, 8], F32)
        ls8 = SB(st, "ls8", [128, 2, 2, 8], F32)
        esink = SB(st, "esink", [128, 2, 8], F32)
        router_b = SB(st, "router_b", [128, DEPTH, KC, NE], BF16)
        tokidx = SB(st, "tokidx_sb", [128, 16], F32)
        ntok = SB(st, "ntok_sb", [128, 16], F32)
        pidx16 = SB(st, "pidx16_sb", [16, 128], F32)
        halfoff = SB(st, "halfoff_sb", [32, 1], F32)
        scc = SB(st, "scc", [128, KC, 2], BF16)

        for k in range(KC):
            dma('sp', xT[:, k, :], xT_d[k * 128:(k + 1) * 128, :])
        rstk = ExitStack()
        router_f = SB(rstk, "router_f", [128, DEPTH, KC, NE], F32)
        for sb_t, d_t in [(ident_f, ident_d), (ada_b, ada_b_d), (n1g, n1g_d), (n2g, n2g_d), (fing, fing_d),
                          (convw, convw_d), (convb, convb_d), (gateb, gateb_d), (lam, lam_d), (esink, sink_d),
                          (router_f, router_d), (tokidx, tokidx_d), (pidx16, pidx_d), (halfoff, halfoff_d)]:
            dma('sp', sb_t[:], d_t)
        cp('dve', ident_b[:], ident_f[:])
        cp('dve', router_b[:], router_f[:])
        bld.memset('dve', ones_b[:], 1.0)
        act(esink[:], esink[:], AF.Exp)
        act(lam[:], lam[:], AF.Exp, scale=-1.0)
        ts('dve', lam[:], lam[:], 1.0, None, ALU.add)
        act(lam[:], lam[:], AF.Ln)
        ts('dve', ls4[:], lam[:], -4.0, None, ALU.mult)
        ts('dve', ls8[:], lam[:], -8.0, None, ALU.mult)
        ts('dve', gatebh[:], gateb[:], 0.5, None, ALU.mult)
        ts('dve', ntok[:], tokidx[:], -1.0, None, ALU.mult)
        P.barrier()
        rstk.close()

        with ExitStack() as ph:
            cc = SB(ph, "cc_sb", [128, KC, 2], F32)
            adw = [SB(ph, f"adw{i}", [128, KC, 1024], BF16) for i in range(2)]
            dma('sp', cc[:], cc_d)
            act(scc[:], cc[:], AF.Silu)
            pc = 0
            for l in range(n_layers):
                psm = bld.ps()
                for g in range(6):
                    wb = adw[pc % 2]
                    pc += 1
                    dma('pool', wb[:], ada_w_d[l, :, g * 1024:(g + 1) * 1024].rearrange("(c p) f -> p c f", p=128))
                    for jj in range(8):
                        j = g * 8 + jj
                        for k in range(KC):
                            mm(psm[:, 2 * j:2 * j + 2], wb[:, k, jj * 128:(jj + 1) * 128], scc[:, k, :],
                               start=(k == 0), stop=(k == KC - 1))
                tt('dve', mod[:, l], psm[:, 0:96].rearrange("p (j s) -> p j s", s=2),
                   ada_b[:, l, :].unsqueeze(2).to_broadcast([128, 48, 2]), ALU.add)
                for (gp, ng, grp) in ((gp1, n1g, 1), (gp2, n2g, 4)):
                    ts('dve', gp[:, l], mod[:, l, grp * 8:(grp + 1) * 8, :], 1.0, None, ALU.add)
                    tt('dve', gp[:, l], gp[:, l], ng[:, l, :].unsqueeze(2).to_broadcast([128, KC, 2]), ALU.mult)
            P.barrier()

        def mod_ap(l, grp, k, s):
            return mod[:, l, grp * 8 + k, s:s + 1]

        def norm_mod(ph, hT, l, which, tiles):
            gp = gp1 if which == 1 else gp2
            shg = 0 if which == 1 else 3
            sq = [SB(ph, f"nm_sq{i}_{l}_{which}", [128, 512], BF16) for i in range(4)]
            rs = [SB(ph, f"nm_rs{i}_{l}_{which}", [128, 512], F32) for i in range(2)]
            tmp = [SB(ph, f"nm_tmp{i}_{l}_{which}", [128, 512], F32) for i in range(4)]
            for ti, (t0, n, s) in enumerate(tiles):
                pss = bld.ps()
                for k in range(KC):
                    q = sq[k % 4]
                    if k % 2 == 0:
                        act(q[:, 0:n], xT[:, k, t0:t0 + n], AF.Square)
                    else:
                        tt('dve', q[:, 0:n], xT[:, k, t0:t0 + n], xT[:, k, t0:t0 + n], ALU.mult)
                    mm(pss[:, 0:n], ones_b[:], q[:, 0:n], start=(k == 0), stop=(k == KC - 1))
                r = rs[ti % 2]
                act(r[:, 0:n], pss[:, 0:n], AF.Ln, scale=1.0 / D, bias=EPS)
                act(r[:, 0:n], r[:, 0:n], AF.Exp, scale=-0.5)
                for k in range(KC):
                    tm = tmp[k % 4]
                    tt('dve', tm[:, 0:n], xT[:, k, t0:t0 + n], r[:, 0:n], ALU.mult)
                    act(hT[:, k, t0:t0 + n], tm[:, 0:n], AF.Identity,
                        scale=gp[:, l, k, s:s + 1], bias=mod_ap(l, shg, k, s))

        def out_proj(ph, name, w_dram, mT_, l, ggrp, tiles):
            wo = SB(ph, name, [128, KC, D], BF16)
            for h in range(2):
                dma('pool', wo[:, :, h * 512:(h + 1) * 512],
                    w_dram[:, h * 512:(h + 1) * 512].rearrange("(k p) f -> p k f", p=128))
            for (t0, n, s) in tiles:
                for dm in range(KC):
                    po = bld.ps()
                    for c in range(KC):
                        mm(po[:, 0:n], wo[:, c, dm * 128:(dm + 1) * 128], mT_[:, c, t0:t0 + n],
                           start=(c == 0), stop=(c == KC - 1))
                    stt('dve', xT[:, dm, t0:t0 + n], po[:, 0:n], mod_ap(l, ggrp, dm, s), xT[:, dm, t0:t0 + n],
                        ALU.mult, ALU.add)

        def lru_phase(l, need_ctx):
            j = l // 2
            with ExitStack() as ph:
                hT = SB(ph, f"hT_l{l}", [128, KC, T], BF16)
                mT = SB(ph, f"mT_l{l}", [128, KC, T], BF16)
                with ExitStack() as nt:
                    norm_mod(nt, hT, l, 1, TILES)
                    P.barrier()
                with ExitStack() as lt:
                    ub = SB(lt, f"ub{l}", [128, T], F32)
                    xb = SB(lt, f"xb{l}", [128, T], F32)
                    xbb = SB(lt, f"xbb{l}", [128, T], BF16)
                    winA = SB(lt, f"winA{l}", [128, KC, 128], BF16)
                    winB = SB(lt, f"winB{l}", [128, KC, 128], BF16)
                    gw = [SB(lt, f"gw{l}_{i}", [128, 256], BF16) for i in range(4)]
                    tr_ = [SB(lt, f"tr{l}_{i}", [128, 512], F32) for i in range(4)]
                    ti_ = [SB(lt, f"ti{l}_{i}", [128, 512], F32) for i in range(4)]
                    tb_ = [SB(lt, f"tb{l}_{i}", [128, 512], F32) for i in range(4)]
                    th_ = [SB(lt, f"th{l}_{i}", [128, 512], F32) for i in range(2)]
                    carry = SB(lt, f"carry{l}", [128, 2], F32)
                    for d in range(2):
                        dma('pool', gw[d][:], gatew_d[j, d, 0])
                    orders = [TILES, [TILES[0]] + TILES[:0:-1]]
                    first_dir = {0: 0, 256: 0, 768: 0, 1280: 1, 1792: 1}

                    def ldA(c):
                        dma('pool', winA[:], lwin_d[j, :, c * 128:(c + 1) * 128].rearrange("(k p) f -> p k f", p=128))

                    def ldB(c):
                        dma('pool', winB[:], lwin_d[j, :, D + c * 128:D + (c + 1) * 128].rearrange("(k p) f -> p k f", p=128))
                    ldB(0)
                    ldA(0)
                    for c in range(KC):
                        if c + 1 < KC:
                            for d in range(2):
                                dma('pool', gw[((c + 1) % 2) * 2 + d][:], gatew_d[j, d, c + 1])
                        for (t0, n, s) in TILES:
                            pu = bld.ps()
                            for k in range(KC):
                                mm(pu[:, 0:n], winB[:, k, :], hT[:, k, t0:t0 + n], start=(k == 0), stop=(k == KC - 1))
                            cp('act', ub[:, t0:t0 + n], pu[:, 0:n])
                        if c + 1 < KC:
                            ldB(c + 1)
                        for (t0, n, s) in TILES:
                            s0, sn = (0, NCX) if s == 1 else (NCX, NL)
                            act(xb[:, t0:t0 + n], ub[:, t0:t0 + n], AF.Identity, scale=convw[:, j, c, 1:2], bias=convb[:, j, c:c + 1])
                            for (o, kk) in ((-1, 0), (1, 2), (2, 3)):
                                lo = max(t0, s0 - o)
                                hi = min(t0 + n, s0 + sn - o)
                                stt('dve', xb[:, lo:hi], ub[:, lo + o:hi + o], convw[:, j, c, kk:kk + 1], xb[:, lo:hi], ALU.mult, ALU.add)
                            cp('act', xbb[:, t0:t0 + n], xb[:, t0:t0 + n])
                        for (ga, gb_) in ((0, 1), (1, 3), (3, 5)):
                            for d in range(2):
                                g = gw[(c % 2) * 2 + d]
                                for gi, (t0, n, s) in enumerate(orders[d][ga:gb_]):
                                    slot = d * 2 + gi
                                    pr = bld.ps()
                                    pi = bld.ps()
                                    mm(pr[:, 0:n], g[:, 0:128], xbb[:, t0:t0 + n])
                                    mm(pi[:, 0:n], g[:, 128:256], xbb[:, t0:t0 + n])
                                    r_ = tr_[slot]
                                    i_ = ti_[slot]
                                    b_ = tb_[slot]
                                    act(r_[:, 0:n], pr[:, 0:n], AF.Tanh, scale=0.5, bias=gatebh[:, j, d, c, 0:1])
                                    act(i_[:, 0:n], pi[:, 0:n], AF.Tanh, scale=0.5, bias=gatebh[:, j, d, c, 1:2])
                                    act(r_[:, 0:n], r_[:, 0:n], AF.Exp, scale=ls4[:, j, d, c:c + 1], bias=ls4[:, j, d, c:c + 1])
                                    stt('dve', b_[:, 0:n], r_[:, 0:n], -1.0, r_[:, 0:n], ALU.mult, ALU.mult)
                                    act(b_[:, 0:n], b_[:, 0:n], AF.Relu, scale=1.0, bias=1.0)
                            for d in range(2):
                                for gi, (t0, n, s) in enumerate(orders[d][ga:gb_]):
                                    slot = d * 2 + gi
                                    r_ = tr_[slot]
                                    i_ = ti_[slot]
                                    b_ = tb_[slot]
                                    act(b_[:, 0:n], b_[:, 0:n], AF.Sqrt, scale=0.25)
                                    stt('dve', i_[:, 0:n], i_[:, 0:n], 1.0, xb[:, t0:t0 + n], ALU.add, ALU.mult)
                                    tt('dve', b_[:, 0:n], b_[:, 0:n], i_[:, 0:n], ALU.mult)
                                    init = 0.0 if ga == 0 else carry[:, d:d + 1]
                                    first = first_dir[t0] == d
                                    dst = ub[:, t0:t0 + n] if first else th_[gi][:, 0:n]
                                    if d == 0:
                                        bld.scan(dst, r_[:, 0:n], b_[:, 0:n], init)
                                        cp('dve', carry[:, 0:1], dst[:, n - 1:n])
                                    else:
                                        bld.scan(dst[:, ::-1], r_[:, n - 1::-1], b_[:, n - 1::-1], init)
                                        cp('dve', carry[:, 1:2], dst[:, 0:1])
                                    if not first:
                                        tt('pool', ub[:, t0:t0 + n], ub[:, t0:t0 + n], dst, ALU.add)
                        for ti, (t0, n, s) in enumerate(TILES):
                            if s == 1 and not need_ctx:
                                continue
                            pg = bld.ps()
                            for k in range(KC):
                                mm(pg[:, 0:n], winA[:, k, :], hT[:, k, t0:t0 + n], start=(k == 0), stop=(k == KC - 1))
                            y_ = th_[ti % 2]
                            act(y_[:, 0:n], pg[:, 0:n], AF.Gelu_apprx_tanh)
                            tt('dve', mT[:, c, t0:t0 + n], y_[:, 0:n], ub[:, t0:t0 + n], ALU.mult)
                        if c + 1 < KC:
                            ldA(c + 1)
                    P.barrier()
                with ExitStack() as ot:
                    tiles = TILES if need_ctx else TILES[1:]
                    out_proj(ot, f"lwo{l}", lwout_d[j], mT, l, 2, tiles)
                    P.barrier()

        def attn_phase(l, need_ctx):
            j = l // 2
            with ExitStack() as ph:
                hT = SB(ph, f"hT_l{l}", [128, KC, T], BF16)
                qT = SB(ph, f"qT_l{l}", [128, 8, T], BF16)
                kT = SB(ph, f"kT_l{l}", [128, 2, T], BF16)
                V = SB(ph, f"V_l{l}", [128, 18, 256], BF16)
                with ExitStack() as nt:
                    norm_mod(nt, hT, l, 1, TILES)
                    P.barrier()
                with ExitStack() as qt:
                    rc = SB(qt, f"rc{l}", [128, NL], F32)
                    rs_ = SB(qt, f"rs{l}", [128, NL], F32)
                    dma('sp', rc[:], ropec_d)
                    dma('sp', rs_[:], ropes_d)
                    wq = [SB(qt, f"wq{l}_{i}", [128, KC, 128], BF16) for i in range(2)]
                    ws = [SB(qt, f"ws{l}_{i}", [128, KC, 128], BF16) for i in range(2)]
                    wv = SB(qt, f"wv{l}", [128, KC, 256], BF16)
                    t1 = [SB(qt, f"t1{l}_{i}", [128, 512], F32) for i in range(2)]
                    t2 = [SB(qt, f"t2{l}_{i}", [128, 512], F32) for i in range(2)]
                    for hh in range(10):
                        a = wq[hh % 2]
                        b = ws[hh % 2]
                        dma('pool', a[:], wqkv_d[j, :, hh * 128:(hh + 1) * 128].rearrange("(k p) f -> p k f", p=128))
                        dma('pool', b[:], wqsw_d[j, :, hh * 128:(hh + 1) * 128].rearrange("(k p) f -> p k f", p=128))
                        dst = qT[:, hh, :] if hh < 8 else kT[:, hh - 8, :]
                        for ti, (t0, n, s) in enumerate(TILES):
                            if s == 1 and hh < 8 and not need_ctx:
                                continue
                            p1 = bld.ps()
                            for k in range(KC):
                                mm(p1[:, 0:n], a[:, k, :], hT[:, k, t0:t0 + n], start=(k == 0), stop=(k == KC - 1))
                            if s == 1:
                                cp('act', dst[:, t0:t0 + n], p1[:, 0:n])
                                continue
                            p2 = bld.ps()
                            for k in range(KC):
                                mm(p2[:, 0:n], b[:, k, :], hT[:, k, t0:t0 + n], start=(k == 0), stop=(k == KC - 1))
                            a1 = t1[ti % 2]
                            a2 = t2[ti % 2]
                            tt('dve', a1[:, 0:n], p1[:, 0:n], rc[:, t0 - NCX:t0 - NCX + n], ALU.mult)
                            tt('dve', a2[:, 0:n], p2[:, 0:n], rs_[:, t0 - NCX:t0 - NCX + n], ALU.mult)
                            tt('pool', dst[:, t0:t0 + n], a1[:, 0:n], a2[:, 0:n], ALU.add)
                    dma('pool', wv[:], wqkv_d[j, :, 1280:1536].rearrange("(k p) f -> p k f", p=128))
                    for blk in range(18):
                        pv = bld.ps()
                        for k in range(KC):
                            mm(pv[:, 0:256], hT[:, k, blk * 128:(blk + 1) * 128], wv[:, k, :], start=(k == 0), stop=(k == KC - 1))
                        cp(bld.alt(), V[:, blk, :], pv[:, 0:256])
                    P.barrier()
                with ExitStack() as at:
                    oT = hT
                    mprev = SB(at, f"mprev{l}", [128, 128], BF16)
                    mnext = SB(at, f"mnext{l}", [128, 128], BF16)
                    dma('pool', mprev[:], mprev_d)
                    dma('pool', mnext[:], mnext_d)
                    ex = [SB(at, f"ex{l}_{i}", [128, 512], BF16) for i in range(6)]
                    dn = [SB(at, f"dn{l}_{i}", [128, 512], F32) for i in range(2)]
                    exc = 0
                    qblocks = [(b, True) for b in range(2, 18)]
                    if need_ctx:
                        qblocks += [(0, False), (1, False)]
                    for qi, (qb, is_lat) in enumerate(qblocks):
                        q0 = qb * 128
                        for kvh in range(2):
                            keys = [(0, None), (1, None)]
                            if is_lat:
                                if qb > 2:
                                    keys.append((qb - 1, mprev))
                                keys.append((qb, None))
                                if qb < 17:
                                    keys.append((qb + 1, mnext))
                            qsl = qT[:, kvh * 4:(kvh + 1) * 4, q0:q0 + 128]
                            po = bld.ps()
                            pd = bld.ps()
                            es = []
                            for (kb, msk) in keys:
                                psc = bld.ps()
                                mm(psc[:, :].rearrange("p (g q) -> p g q", g=4), kT[:, kvh, kb * 128:(kb + 1) * 128], qsl)
                                e_ = ex[exc % 6]
                                exc += 1
                                act(e_[:], psc[:], AF.Exp, scale=QSCALE)
                                if msk is not None:
                                    e3 = e_[:, :].rearrange("p (g q) -> p g q", g=4)
                                    tt('pool', e3, e3, msk[:, :].unsqueeze(1).to_broadcast([128, 4, 128]), ALU.mult)
                                es.append((kb, e_))
                            for i, (kb, e_) in enumerate(es):
                                mm(po[:], V[:, kb, kvh * 128:(kvh + 1) * 128], e_[:], start=(i == 0), stop=(i == len(es) - 1))
                            for i, (kb, e_) in enumerate(es):
                                mm(pd[:], ones_b[:], e_[:], start=(i == 0), stop=(i == len(es) - 1))
                            d_ = dn[(qi * 2 + kvh) % 2]
                            tt('dve', d_[:, :].rearrange("p (g q) -> p g q", g=4), pd[:, :].rearrange("p (g q) -> p g q", g=4),
                               esink[:, j, kvh * 4:(kvh + 1) * 4].unsqueeze(2).to_broadcast([128, 4, 128]), ALU.add)
                            P.op('dve', (lambda dd: lambda e: e.reciprocal(dd[:], dd[:]))(d_), reads=[d_[:]], writes=[d_[:]])
                            tt('dve', oT[:, kvh * 4:(kvh + 1) * 4, q0:q0 + 128], po[:, :].rearrange("p (g q) -> p g q", g=4),
                               d_[:, :].rearrange("p (g q) -> p g q", g=4), ALU.mult)
                    P.barrier()
                with ExitStack() as ot:
                    tiles = TILES if need_ctx else TILES[1:]
                    out_proj(ot, f"awo{l}", wo_d[j], oT, l, 2, tiles)
                    P.barrier()

        def moe_phase(l, need_ctx):
            tiles = TILES if need_ctx else TILES[1:]
            CT = CAPL + CAPC if need_ctx else CAPL
            cbs = [(0, 128, 0), (1, 128, 128)] + ([(2, 32, 256)] if need_ctx else [])
            with ExitStack() as ph:
                idx_i = SB(ph, f"idxi{l}", [128, 3, NE], I32)
                val_tok = SB(ph, f"valtok{l}", [128, 3, NE], F32)
                nbias = SB(ph, f"nbias{l}", [128, 3, NE, 4], F32)
                iota_row = SB(ph, f"iota{l}", [128, 512], F32)
                dma('sp', iota_row[:], iota_d[:, 0:512])
                NW = 6
                wring = [SB(ph, f"wr{l}_{i}", [128, KC, 512], BF16) for i in range(NW)]
                pieces = []
                for e in range(NE):
                    for fh in range(2):
                        pieces.append(wg_d[l, e, :, fh * 512:(fh + 1) * 512])
                        pieces.append(wu_d[l, e, :, fh * 512:(fh + 1) * 512])
                    for dh in range(2):
                        pieces.append(wd_d[l, e, :, dh * 512:(dh + 1) * 512])
                issued = [6 * MOE_E0]

                def ensure(i):
                    while issued[0] < min(len(pieces), i + NW):
                        pi_ = issued[0]
                        dma('pool', wring[pi_ % NW][:], pieces[pi_].rearrange("(k p) f -> p k f", p=128))
                        issued[0] += 1

                ensure(6 * MOE_E0)
                with ExitStack() as rt:
                    hT = SB(rt, f"hTm{l}", [128, KC, T], BF16)
                    htok = [SB(rt, f"htok{l}_{i}", [128, D], BF16) for i in range(3)]
                    with ExitStack() as nt:
                        norm_mod(nt, hT, l, 2, tiles)
                        P.barrier()
                    aff = SB(rt, f"aff{l}", [128, 18, NE], F32)
                    mx = SB(rt, f"mx{l}", [128, 18], F32)
                    affC = SB(rt, f"affC{l}", [16, NCX], F32)
                    affL = SB(rt, f"affL{l}", [32, 1024], F32)
                    valsL = SB(rt, f"valsL{l}", [32, CAPL], F32)
                    idxL_u = SB(rt, f"idxLu{l}", [32, CAPL], U32)
                    idxL_f = SB(rt, f"idxLf{l}", [32, CAPL], F32)
                    Bv = SB(rt, f"Bv{l}", [16, CAPL], F32)
                    Bi = SB(rt, f"Bi{l}", [16, CAPL], F32)
                    sel = SB(rt, f"sel{l}", [16, CAPL], F32)
                    dd = SB(rt, f"dd{l}", [16, CAPL], F32)
                    vals = SB(rt, f"vals{l}", [16, 288], F32)
                    idx_u = SB(rt, f"idxu{l}", [16, 288], U32)
                    idx_f = SB(rt, f"idxf{l}", [16, 288], F32)
                    idxT = SB(rt, f"idxT{l}", [128, 3, NE], F32)
                    blks = list(range(18)) if need_ctx else list(range(2, 18))
                    pl = bld.ps()

                    def posb(b):
                        if b < 2:
                            return b
                        lb = b - 2
                        return 2 + (lb % 8) * 2 + lb // 8
                    for b in blks:
                        pb_ = posb(b)
                        for k in range(KC):
                            mm(pl[:, pb_ * NE:(pb_ + 1) * NE], hT[:, k, b * 128:(b + 1) * 128], router_b[:, l, k, :],
                               start=(k == 0), stop=(k == KC - 1))
                    b0, nb = blks[0], len(blks)
                    pl3 = pl[:, b0 * NE:(b0 + nb) * NE].rearrange("p (b e) -> p b e", e=NE)
                    a3 = aff[:, b0:b0 + nb, :]
                    P.op('dve', lambda e: e.tensor_reduce(mx[:, b0:b0 + nb], pl3, AX.X, ALU.max), reads=[pl3], writes=[mx[:, b0:b0 + nb]])
                    tt('dve', a3, pl3, mx[:, b0:b0 + nb].unsqueeze(2).to_broadcast([128, nb, NE]), ALU.subtract)
                    act(a3, a3, AF.Exp)
                    P.op('dve', lambda e: e.tensor_reduce(mx[:, b0:b0 + nb], a3, AX.X, ALU.add), reads=[a3], writes=[mx[:, b0:b0 + nb]])
                    P.op('dve', lambda e: e.reciprocal(mx[:, b0:b0 + nb], mx[:, b0:b0 + nb]), reads=[mx[:, b0:b0 + nb]], writes=[mx[:, b0:b0 + nb]])
                    tt('dve', a3, a3, mx[:, b0:b0 + nb].unsqueeze(2).to_broadcast([128, nb, NE]), ALU.mult)
                    if need_ctx:
                        pt = bld.ps()
                        for b in range(2):
                            bld.tr(pt[0:16, b * 128:(b + 1) * 128], aff[:, b, :], ident_f[:])
                        cp('act', affC[:, 0:NCX], pt[0:16, 0:NCX])
                    for g in range(2):
                        pt = bld.ps()
                        for bb in range(4):
                            b = g * 4 + bb
                            bld.tr(pt[0:32, bb * 128:(bb + 1) * 128], aff[:, 2 + 2 * b:4 + 2 * b, :].rearrange("p a e -> p (a e)"), ident_f[:])
                        cp('act', affL[:, g * 512:(g + 1) * 512], pt[0:32, :])
                    for b in blks:
                        for half in range(2):
                            pt = bld.ps()
                            for kk in range(4):
                                k = half * 4 + kk
                                bld.tr(pt[:, kk * 128:(kk + 1) * 128], hT[:, k, b * 128:(b + 1) * 128], ident_b[:])
                            cp('act', htok[b % 3][:, half * 512:(half + 1) * 512], pt[:])
                        dma('sp', hscr_d[l][b * 128:(b + 1) * 128, :], htok[b % 3][:])
                    def topk(w, vout, iout, cap):
                        for it in range(cap // 8):
                            v8 = vout[:, it * 8:it * 8 + 8]
                            i8 = iout[:, it * 8:it * 8 + 8]
                            P.op('dve', (lambda v8, w: lambda e: e.max(out=v8, in_=w))(v8, w), reads=[w], writes=[v8])
                            P.op('dve', (lambda v8, i8, w: lambda e: e.max_index(out=i8, in_max=v8, in_values=w))(v8, i8, w), reads=[w, v8], writes=[i8])
                            P.op('dve', (lambda v8, w: lambda e: e.match_replace(out=w, in_to_replace=v8, in_values=w, imm_value=0.0))(v8, w), reads=[w, v8], writes=[w])
                    topk(affL[:], valsL[:], idxL_u[:], CAPL)
                    if need_ctx:
                        topk(affC[:], vals[:, CAPL:CAPL + CAPC], idx_u[:, CAPL:CAPL + CAPC], CAPC)
                        cp('dve', idx_f[:, CAPL:CAPL + CAPC], idx_u[:, CAPL:CAPL + CAPC])
                    cp('dve', idxL_f[:], idxL_u[:])
                    ts('dve', idxL_f[:], idxL_f[:], halfoff[:, 0:1], None, ALU.add)
                    dma('sp', scr2_d[l, 0], valsL[:])
                    dma('sp', scr2_d[l, 1], idxL_f[:])
                    dma('sp', Bv[:], scr2_d[l, 0, 16:32, :])
                    dma('sp', Bi[:], scr2_d[l, 1, 16:32, :])
                    tt('dve', sel[:], valsL[0:16, :], Bv[:, ::-1], ALU.is_gt)
                    for (A_, B_, out_) in ((valsL[0:16, :], Bv, vals[:, 0:CAPL]), (idxL_f[0:16, :], Bi, idx_f[:, 0:CAPL])):
                        tt('dve', dd[:], A_, B_[:, ::-1], ALU.subtract)
                        tt('dve', dd[:], dd[:], sel[:], ALU.mult)
                        tt('dve', out_, dd[:], B_[:, ::-1], ALU.add)
                    if not need_ctx:
                        bld.memset('dve', idx_f[:, CAPL:288], 0.0)
                        bld.memset('dve', vals[:, CAPL:288], 0.0)
                    dma('sp', scr_d[l, 0], idx_f[:])
                    dma('sp', scr_d[l, 1], vals[:])
                    for (cb, m, c0) in cbs:
                        P.dma('sp', idxT[0:m, cb, :], scr_d[l, 0, :, c0:c0 + m].rearrange("e c -> c e"), allow_slow_non_contiguous=True)
                        P.dma('sp', val_tok[0:m, cb, :], scr_d[l, 1, :, c0:c0 + m].rearrange("e c -> c e"), allow_slow_non_contiguous=True)
                    for (cb, m, c0) in cbs:
                        if cb < 2:
                            ts('dve', nbias[0:m, cb, :, 0], idxT[0:m, cb, :], float(NCX), None, ALU.add)
                            cp('dve', idx_i[0:m, cb, :], nbias[0:m, cb, :, 0])
                            for q4 in range(4):
                                ts('dve', nbias[0:m, cb, :, q4], idxT[0:m, cb, :], -1.0, float(q4 * 512), ALU.mult, ALU.add)
                        else:
                            cp('dve', idx_i[0:m, cb, :], idxT[0:m, cb, :])
                            ts('dve', nbias[0:m, cb, :, 0], idxT[0:m, cb, :], -1.0, None, ALU.mult)
                    P.barrier()
                with ExitStack() as et:
                    xgt = [SB(et, f"xgt{l}_{i}", [128, 3, D], BF16) for i in range(2)]
                    dtmp = [SB(et, f"dtmp{l}_{i}", [128, 512], F32) for i in range(3)]
                    xg = SB(et, f"xg{l}", [128, KC, 288], BF16)
                    sa = [SB(et, f"sa{l}_{i}", [128, 288], F32) for i in range(2)]
                    actT = SB(et, f"actT{l}", [128, KC, 288], BF16)
                    ye = [SB(et, f"ye{l}_{i}", [128, 3, D], BF16) for i in range(2)]
                    ST_lat = [SB(et, f"STl{l}_{i}", [128, 2, NL], BF16) for i in range(2)]
                    ST_ctx = [SB(et, f"STc{l}_{i}", [32, NCX], BF16) for i in range(2)]

                    def wget(i, base):
                        ensure(base)
                        assert i < issued[0]
                        return wring[i % NW]

                    def gather(e):
                        buf = xgt[e % 2]
                        for (cb, m, c0) in cbs:
                            def mk(cb, m, e):
                                return lambda en, out, in_, kw: en.indirect_dma_start(
                                    out=out, out_offset=None, in_=in_,
                                    in_offset=bass.IndirectOffsetOnAxis(ap=idx_i[0:m, cb, e:e + 1], axis=0))
                            P.dma_custom('pool', buf[0:m, cb, :], hscr_d[l][:, :], mk(cb, m, e), extra_reads=[idx_i[0:m, cb, e:e + 1]])

                    gather(MOE_E0)
                    for e in range(MOE_E0, MOE_E1):
                        par = e % 2
                        if e + 1 < MOE_E1:
                            gather(e + 1)
                        ensure(6 * e)
                        st_jobs = []
                        for cb in range(2):
                            for q4 in range(4):
                                def job(cb=cb, q4=q4, e=e, par=par):
                                    dt_ = dtmp[(cb * 4 + q4) % 3]
                                    act(dt_[:], iota_row[:], AF.Abs, bias=nbias[:, cb, e, q4:q4 + 1])
                                    act(ST_lat[par][:, cb, q4 * 512:(q4 + 1) * 512], dt_[:], AF.Relu, scale=-1.0, bias=1.0)
                                st_jobs.append(job)
                        if need_ctx:
                            def jobc(e=e, par=par):
                                dt_ = dtmp[2]
                                act(dt_[0:32, 0:NCX], iota_row[0:32, 0:NCX], AF.Abs, bias=nbias[0:32, 2, e, 0:1])
                                act(ST_ctx[par][:], dt_[0:32, 0:NCX], AF.Relu, scale=-1.0, bias=1.0)
                            st_jobs.append(jobc)
                        buf = xgt[par]
                        for k in range(KC):
                            pg = bld.ps()
                            for (cb, m, c0) in cbs:
                                mm(pg[:, c0:c0 + m], buf[0:m, cb, k * 128:(k + 1) * 128], ident_b[0:m, 0:m])
                            cp(bld.alt(), xg[:, k, 0:CT], pg[:, 0:CT])
                        for fh in range(2):
                            wgb = wget(6 * e + 2 * fh, 6 * e + 2 * fh)
                            wub = wget(6 * e + 2 * fh + 1, 6 * e + 2 * fh)
                            for ff in range(4):
                                f = fh * 4 + ff
                                pa = bld.ps()
                                pu = bld.ps()
                                for k in range(KC):
                                    mm(pa[:, 0:CT], wgb[:, k, ff * 128:(ff + 1) * 128], xg[:, k, 0:CT], start=(k == 0), stop=(k == KC - 1))
                                for k in range(KC):
                                    mm(pu[:, 0:CT], wub[:, k, ff * 128:(ff + 1) * 128], xg[:, k, 0:CT], start=(k == 0), stop=(k == KC - 1))
                                s_ = sa[f % 2]
                                act(s_[:, 0:CT], pa[:, 0:CT], AF.Silu)
                                tt('dve', actT[:, f, 0:CT], s_[:, 0:CT], pu[:, 0:CT], ALU.mult)
                                if st_jobs:
                                    st_jobs.pop(0)()
                                if f == KC - 1:
                                    while st_jobs:
                                        st_jobs.pop(0)()
                        for dh in range(2):
                            wdb = wget(6 * e + 4 + dh, 6 * e + 4 + dh)
                            for (cb, m, c0) in cbs:
                                py = bld.ps()
                                for f in range(KC):
                                    mm(py[0:m, :], actT[:, f, c0:c0 + m], wdb[:, f, :], start=(f == 0), stop=(f == KC - 1))
                                act(ye[par][0:m, cb, dh * 512:(dh + 1) * 512], py[0:m, :], AF.Copy, scale=val_tok[0:m, cb, e:e + 1])
                        if par == 1:
                            for k in range(KC):
                                for (t0, n, s) in TILES[1:]:
                                    pz = bld.ps()
                                    i = 0
                                    for pp in range(2):
                                        for cb in range(2):
                                            mm(pz[:, 0:n], ye[pp][:, cb, k * 128:(k + 1) * 128], ST_lat[pp][:, cb, t0 - NCX:t0 - NCX + n],
                                               start=(i == 0), stop=(i == 3))
                                            i += 1
                                    stt('dve', xT[:, k, t0:t0 + n], pz[:, 0:n], mod_ap(l, 5, k, 0), xT[:, k, t0:t0 + n], ALU.mult, ALU.add)
                                if need_ctx:
                                    pz = bld.ps()
                                    for pp in range(2):
                                        mm(pz[:, 0:NCX], ye[pp][0:32, 2, k * 128:(k + 1) * 128], ST_ctx[pp][:], start=(pp == 0), stop=(pp == 1))
                                    stt('dve', xT[:, k, 0:NCX], pz[:, 0:NCX], mod_ap(l, 5, k, 1), xT[:, k, 0:NCX], ALU.mult, ALU.add)
                    P.barrier()

        def final_phase():
            with ExitStack() as ph:
                sq = [SB(ph, f"fn_sq{i}", [128, 512], BF16) for i in range(4)]
                rs = [SB(ph, f"fn_rs{i}", [128, 512], F32) for i in range(2)]
                ot = [SB(ph, f"fn_o{i}", [128, 512], F32) for i in range(3)]
                oc = 0
                for ti, (t0, n, s) in enumerate(TILES[1:]):
                    pss = bld.ps()
                    for k in range(KC):
                        q = sq[k % 4]
                        act(q[:, 0:n], xT[:, k, t0:t0 + n], AF.Square)
                        mm(pss[:, 0:n], ones_b[:], q[:, 0:n], start=(k == 0), stop=(k == KC - 1))
                    r = rs[ti % 2]
                    act(r[:, 0:n], pss[:, 0:n], AF.Ln, scale=1.0 / D, bias=EPS)
                    act(r[:, 0:n], r[:, 0:n], AF.Exp, scale=-0.5)
                    for k in range(KC):
                        o = ot[oc % 3]
                        oc += 1
                        stt('dve', o[:, 0:n], xT[:, k, t0:t0 + n], fing[:, k:k + 1], r[:, 0:n], ALU.mult, ALU.mult)
                        dma('sp', out_d[k * 128:(k + 1) * 128, t0 - NCX:t0 - NCX + n], o[:, 0:n])
                P.barrier()

        def dump():
            for k in range(KC):
                dma('sp', dbg_d[k * 128:(k + 1) * 128, :], xT[:, k, :])
            P.barrier()

        done = False
        for l in range(n_layers):
            need_ctx = l < DEPTH - 1
            if l % 2 == 0:
                lru_phase(l, need_ctx)
            else:
                attn_phase(l, need_ctx)
            if stop_after == (l, 'mix'):
                dump()
                done = True
                break
            moe_phase(l, need_ctx)
            if stop_after == (l, 'moe'):
                dump()
                done = True
                break
        if not done:
            final_phase()
            if stop_after is not None:
                dump()
        P.emit()
    return nc, P


def _fm(v):
    v = np.asarray(v, np.float32)
    lead = v.shape[:-1]
    r = v.reshape(lead + (8, 128))
    return np.ascontiguousarray(np.moveaxis(r, -1, 0))


def make_in_maps(inp):
    f32 = np.float32
    x = np.asarray(inp["x"], f32)
    ctx = np.asarray(inp["ctx"], f32)
    c = np.asarray(inp["c"], f32)
    c_ctx = np.asarray(inp["c_ctx"], f32)
    nb = x.shape[0]
    shared = {}
    shared["ada_w"] = np.ascontiguousarray(inp["ada_w"], f32)
    shared["ada_b"] = np.ascontiguousarray(np.asarray(inp["ada_b"], f32).reshape(4, 48, 128).transpose(2, 0, 1))
    shared["n1g"] = _fm(inp["norm1_g"])
    shared["n2g"] = _fm(inp["norm2_g"])
    shared["fing"] = _fm(inp["final_g"])
    shared["lru_w_in"] = np.ascontiguousarray(inp["lru_w_in"], f32)
    shared["lru_w_out"] = np.ascontiguousarray(inp["lru_w_out"], f32)
    cw = np.asarray(inp["lru_conv_w"], f32)
    shared["conv_w"] = np.ascontiguousarray(cw.reshape(2, 4, 8, 128).transpose(3, 0, 2, 1))
    shared["conv_b"] = _fm(inp["lru_conv_b"])
    shared["gate_w"] = np.ascontiguousarray(inp["lru_gate_w"], f32)
    gb = np.asarray(inp["lru_gate_b"], f32)
    shared["gate_b"] = np.ascontiguousarray(gb.reshape(2, 2, 8, 2, 128).transpose(4, 0, 1, 2, 3))
    shared["lam"] = _fm(inp["lru_lambda"])
    wqkv = np.asarray(inp["attn_w_qkv"], f32)
    shared["w_qkv"] = np.ascontiguousarray(wqkv)
    perm = np.concatenate([np.arange(32, 64), np.arange(0, 32), np.arange(96, 128), np.arange(64, 96)])
    cols = (np.arange(10)[:, None] * 128 + perm[None, :]).reshape(-1)
    shared["w_qkv_sw"] = np.ascontiguousarray(wqkv[:, :, cols])
    shared["sink"] = np.ascontiguousarray(np.broadcast_to(np.asarray(inp["attn_sink"], f32)[None], (128, 2, 8)))
    shared["w_o"] = np.ascontiguousarray(inp["attn_w_o"], f32)
    r = np.asarray(inp["moe_router"], f32)
    shared["router"] = np.ascontiguousarray(r.reshape(4, 8, 128, 16).transpose(2, 0, 1, 3))
    shared["moe_w_gate"] = np.ascontiguousarray(inp["moe_w_gate"], f32)
    shared["moe_w_up"] = np.ascontiguousarray(inp["moe_w_up"], f32)
    shared["moe_w_down"] = np.ascontiguousarray(inp["moe_w_down"], f32)
    shared["ident"] = np.eye(128, dtype=f32)
    half = 64
    freqs = (10000.0 ** (-np.arange(0, half, 2, dtype=np.float32) / half)).astype(f32)
    t = np.arange(NL)
    rows = (t // 64).astype(f32)
    colsp = (t % 64).astype(f32)
    rc = np.zeros((128, NL), f32)
    rs = np.zeros((128, NL), f32)
    for p in range(128):
        pos = rows if p < 64 else colsp
        jf = p % 32
        ang = (pos * freqs[jf]).astype(f32)
        rc[p] = np.cos(ang)
        sgn = -1.0 if (p % 64) < 32 else 1.0
        rs[p] = sgn * np.sin(ang)
    shared["rope_c"] = rc
    shared["rope_s"] = rs
    shared["iota_row"] = np.ascontiguousarray(np.broadcast_to(np.arange(NL, dtype=f32)[None], (128, NL)))
    shared["tokidx"] = (np.arange(128, dtype=f32)[:, None] + 128.0 * np.arange(16, dtype=f32)[None, :]).astype(f32)
    s_i = np.arange(128)[:, None]
    q_i = np.arange(128)[None, :]
    shared["mask_prev"] = (s_i >= q_i).astype(f32)
    shared["mask_next"] = (s_i <= q_i).astype(f32)
    shared["halfoff"] = np.concatenate([np.zeros((16, 1), f32), np.full((16, 1), 1024.0, f32)], axis=0)
    shared["pidx16"] = np.ascontiguousarray(np.broadcast_to(np.arange(16, dtype=f32)[:, None], (16, 128)))
    maps = []
    for b in range(nb):
        m = dict(shared)
        m["xT"] = np.ascontiguousarray(np.concatenate([ctx[b].T, x[b].T], axis=1))
        cc = np.stack([c[b].reshape(8, 128).T, c_ctx.reshape(8, 128).T], axis=-1)
        m["cc"] = np.ascontiguousarray(cc, dtype=f32)
        maps.append(m)
    return maps


_CACHE = {}


def kernel(**inputs):
    if "nc" not in _CACHE:
        _CACHE["nc"] = build_program()[0]
    nc = _CACHE["nc"]
    maps = make_in_maps(inputs)
    res = run_bass_kernel_spmd(nc, maps, core_ids=list(range(len(maps))))
    out = np.stack([np.ascontiguousarray(r["outT"].T) for r in res.results], axis=0)
    return out.astype(np.float32)
```

```python
import numpy as np
import concourse.bass as bass
import concourse.mybir as mybir

F32 = mybir.dt.float32
BF16 = mybir.dt.bfloat16
I32 = mybir.dt.int32
U32 = mybir.dt.uint32
AF = mybir.ActivationFunctionType
ALU = mybir.AluOpType
AX = mybir.AxisListType

ENGS = ['pe', 'act', 'dve', 'pool', 'sp']
NDSEM = 6
PE_DELAY_OPS = {'act': 1, 'dve': 4, 'pool': 4}


def _prod(xs):
    r = 1
    for v in xs:
        r *= int(v)
    return r


def region(ap):
    t = ap.tensor
    name = ap.name
    shape = tuple(t.shape)
    pairs = [(int(s), int(c)) for s, c in ap.ap]
    off = int(ap.offset)
    sp = str(ap.space)
    if 'DRAM' in sp.upper() or 'HBM' in sp.upper():
        lo = off
        hi = off
        for s, c in pairs:
            if s >= 0:
                hi += s * (c - 1)
            else:
                lo += s * (c - 1)
        return (name, 0, 1, lo, hi + 1)
    fsz = _prod(shape[1:])
    p0 = off // fsz
    rem = off % fsz
    ps, pc = pairs[0]
    p1 = p0 + (ps // fsz) * (pc - 1) + 1 if pc > 1 else p0 + 1
    lo = rem
    hi = rem
    for s, c in pairs[1:]:
        if s >= 0:
            hi += s * (c - 1)
        else:
            lo += s * (c - 1)
    return (name, p0, p1, lo, hi + 1)


def _overlap(a, b):
    return a[1] < b[2] and b[1] < a[2] and a[3] < b[4] and b[3] < a[4]


def _contains(a, b):
    return a[1] <= b[1] and b[2] <= a[2] and a[3] <= b[3] and b[4] <= a[4]


class Prog:
    def __init__(self, nc, stack):
        self.nc = nc
        self.ops = {e: [] for e in ENGS}
        self.cnt = {e: 0 for e in ENGS}
        self.sems = {}
        for e in ENGS:
            self.sems[('e', e)] = stack.enter_context(nc.semaphore(f"s_{e}"))
        self.dcnt = {}
        self.dnext = {}
        for q in ['sp', 'act', 'pool']:
            self.dnext[q] = 0
            for i in range(NDSEM):
                self.sems[('d', q, i)] = stack.enter_context(nc.semaphore(f"d_{q}{i}"))
                self.dcnt[(q, i)] = 0
        self.waited = {e: {} for e in ENGS}
        self.wr = {}
        self.rd = {}
        self.nwaits = 0
        self.flush = False
        self.pe_delay = True
        self.scratch = {}

    def _deps(self, reads, writes, token):
        deps = {}

        def add(k, v):
            if deps.get(k, 0) < v:
                deps[k] = v
        for ap in reads:
            r = region(ap)
            w = self.wr.setdefault(r[0], {})
            for (rg, sk), v in w.items():
                if _overlap(rg, r):
                    add(sk, v)
        for ap in writes:
            r = region(ap)
            w = self.wr.setdefault(r[0], {})
            d = self.rd.setdefault(r[0], {})
            for tab in (w, d):
                dead = []
                for (rg, sk), v in tab.items():
                    if _overlap(rg, r):
                        add(sk, v)
                        if _contains(r, rg):
                            dead.append((rg, sk))
                for k in dead:
                    del tab[k]
        for ap in reads:
            r = region(ap)
            self.rd.setdefault(r[0], {})[(r, token[0])] = token[1]
        for ap in writes:
            r = region(ap)
            self.wr.setdefault(r[0], {})[(r, token[0])] = token[1]
        return deps

    def _emit_waits(self, eng, deps):
        pe_wait = False
        for sk, v in deps.items():
            if eng == 'pe' and sk == ('e', 'pe'):
                continue
            if self.waited[eng].get(sk, 0) >= v:
                continue
            self.waited[eng][sk] = v
            self.ops[eng].append(('wait', sk, v))
            self.nwaits += 1
            if sk == ('e', 'pe'):
                pe_wait = True
        if pe_wait and self.pe_delay and eng in self.scratch:
            sc = self.scratch[eng]
            for _ in range(PE_DELAY_OPS[eng]):
                if eng == 'act':
                    self.ops[eng].append(('raw', lambda e: e.copy(sc[:], sc[:])))
                else:
                    self.ops[eng].append(('raw', lambda e: e.memset(sc[:], 0.0)))

    def op(self, eng, fn, reads=(), writes=()):
        fl = self.flush and eng in ('act', 'dve', 'pool') and eng in self.scratch
        token = (('e', eng), self.cnt[eng] + (2 if fl else 1))
        deps = self._deps(list(reads), list(writes), token)
        self._emit_waits(eng, deps)
        self.cnt[eng] += 1
        self.ops[eng].append(('op', fn, token[0]))
        if fl:
            sc = self.scratch[eng]
            self.cnt[eng] += 1
            if eng == 'act':
                self.ops[eng].append(('op', lambda e: e.copy(sc[:], sc[:]), token[0]))
            else:
                self.ops[eng].append(('op', lambda e: e.memset(sc[:], 0.0), token[0]))

    def dma(self, q, out, in_, **kw):
        i = self.dnext[q] % NDSEM
        self.dnext[q] += 1
        sk = ('d', q, i)
        prev = self.dcnt[(q, i)]
        self.dcnt[(q, i)] = prev + 16
        token = (sk, prev + 16)
        deps = self._deps([in_], [out], token)
        if prev > 0:
            if deps.get(sk, 0) < prev:
                deps[sk] = prev
        self._emit_waits(q, deps)
        self.ops[q].append(('dma', (out, in_, kw), sk))

    def dma_custom(self, q, out, in_, fn, extra_reads=()):
        i = self.dnext[q] % NDSEM
        self.dnext[q] += 1
        sk = ('d', q, i)
        prev = self.dcnt[(q, i)]
        self.dcnt[(q, i)] = prev + 16
        token = (sk, prev + 16)
        deps = self._deps([in_] + list(extra_reads), [out], token)
        if prev > 0:
            if deps.get(sk, 0) < prev:
                deps[sk] = prev
        self._emit_waits(q, deps)
        self.ops[q].append(('dmac', (out, in_, fn), sk))

    def barrier(self):
        for e in ENGS:
            deps = {}
            for e2 in ENGS:
                if e2 != e and self.cnt[e2] > 0:
                    deps[('e', e2)] = self.cnt[e2]
            if e != 'pe' and self.cnt[e] > 0:
                deps[('e', e)] = self.cnt[e]
            for (q, i), v in self.dcnt.items():
                if v > 0:
                    deps[('d', q, i)] = v
            self._emit_waits(e, deps)
        self.wr = {}
        self.rd = {}

    def emit(self):
        nc = self.nc
        sems = self.sems
        ops = self.ops

        def run(eng_name, eng):
            for item in ops[eng_name]:
                if item[0] == 'wait':
                    eng.wait_ge(sems[item[1]], item[2])
                elif item[0] == 'op':
                    ins = item[1](eng)
                    ins.then_inc(sems[item[2]], 1)
                elif item[0] == 'raw':
                    item[1](eng)
                elif item[0] == 'dmac':
                    out, in_, fn = item[1]
                    fn(eng, out, in_, {}).then_inc(sems[item[2]], 16)
                else:
                    out, in_, kw = item[1]
                    eng.dma_start(out=out, in_=in_, **kw).then_inc(sems[item[2]], 16)

        with nc.Block() as block:
            @block.tensor
            def _(e):
                run('pe', e)

            @block.scalar
            def _(e):
                run('act', e)

            @block.vector
            def _(e):
                run('dve', e)

            @block.gpsimd
            def _(e):
                run('pool', e)

            @block.sync
            def _(e):
                run('sp', e)

from contextlib import ExitStack
import os
MOE_CUT = int(os.environ.get('MOE_CUT', '99'))
MOE_E0 = int(os.environ.get('MOE_E0', '0'))
MOE_E1 = int(os.environ.get('MOE_E1', '16'))
from concourse.bass_utils import run_bass_kernel_spmd

D = 1024
KC = 8
NL = 2048
NCX = 256
T = NL + NCX
DEPTH = 4
NE = 16
CAPL = 256
CAPC = 32
EPS = 1e-6
QSCALE = 128 ** -0.5
TILES = [(0, 256, 1), (256, 512, 0), (768, 512, 0), (1280, 512, 0), (1792, 512, 0)]


class B:
    def __init__(self, nc, P, st):
        self.nc = nc
        self.P = P
        self.st = st
        self.psn = 0
        self.rr = 0

    def mm(self, out, lhsT, rhs, start=True, stop=True):
        self.P.op('pe', lambda e: e.matmul(out, lhsT, rhs, start=start, stop=stop),
                  reads=[lhsT, rhs], writes=[out])

    def tr(self, out, in_, ident):
        self.mm(out, in_, ident)

    def act(self, out, in_, func, bias=None, scale=None):
        kw = {}
        rd = [in_]
        if bias is not None:
            kw['bias'] = bias
            if not isinstance(bias, float):
                rd.append(bias)
        if scale is not None:
            kw['scale'] = scale
            if not isinstance(scale, float):
                rd.append(scale)
        self.P.op('act', lambda e: e.activation(out, in_, func, **kw), reads=rd, writes=[out])

    def tt(self, eng, out, in0, in1, op):
        self.P.op(eng, lambda e: e.tensor_tensor(out, in0, in1, op), reads=[in0, in1], writes=[out])

    def ts(self, eng, out, in0, s1, s2, op0, op1=None):
        rd = [in0]
        if not isinstance(s1, float):
            rd.append(s1)
        if s2 is not None and not isinstance(s2, float):
            rd.append(s2)
        if op1 is None:
            self.P.op(eng, lambda e: e.tensor_scalar(out, in0, s1, None, op0), reads=rd, writes=[out])
        else:
            self.P.op(eng, lambda e: e.tensor_scalar(out, in0, s1, s2, op0, op1), reads=rd, writes=[out])

    def stt(self, eng, out, in0, scalar, in1, op0, op1):
        rd = [in0, in1]
        if not isinstance(scalar, float):
            rd.append(scalar)
        self.P.op(eng, lambda e: e.scalar_tensor_tensor(out, in0, scalar, in1, op0, op1), reads=rd, writes=[out])

    def cp(self, eng, out, in_):
        if eng == 'act':
            self.P.op('act', lambda e: e.copy(out, in_), reads=[in_], writes=[out])
        else:
            self.P.op(eng, lambda e: e.tensor_copy(out, in_), reads=[in_], writes=[out])

    def memset(self, eng, out, val):
        self.P.op(eng, lambda e: e.memset(out, val), reads=[], writes=[out])

    def scan(self, out, a, b, init):
        rd = [a, b]
        if not isinstance(init, float):
            rd.append(init)
        self.P.op('dve', lambda e: e.tensor_tensor_scan(out, a, b, init, ALU.mult, ALU.add), reads=rd, writes=[out])

    def dma(self, q, out, in_):
        self.P.dma(q, out, in_)

    def ps(self):
        b = self.banks[self.psn % len(self.banks)]
        self.psn += 1
        return b

    def alt(self, engs=('act', 'dve')):
        self.rr += 1
        return engs[self.rr % len(engs)]


def build_program(stop_after=None, n_layers=DEPTH):
    nc = bass.Bass("TRN2", target_bir_lowering=False)
    dram = {}

    def din(name, shape, dt=F32):
        dram[name] = nc.dram_tensor(name, list(shape), dt, kind="ExternalInput").ap()
        return dram[name]

    xT_d = din("xT", [D, T])
    cc_d = din("cc", [128, KC, 2])
    ada_w_d = din("ada_w", [DEPTH, D, 6 * D])
    ada_b_d = din("ada_b", [128, DEPTH, 48])
    n1g_d = din("n1g", [128, DEPTH, KC])
    n2g_d = din("n2g", [128, DEPTH, KC])
    fing_d = din("fing", [128, KC])
    lwin_d = din("lru_w_in", [2, D, 2 * D])
    lwout_d = din("lru_w_out", [2, D, D])
    convw_d = din("conv_w", [128, 2, KC, 4])
    convb_d = din("conv_b", [128, 2, KC])
    gatew_d = din("gate_w", [2, 2, 8, 128, 256])
    gateb_d = din("gate_b", [128, 2, 2, 8, 2])
    lam_d = din("lam", [128, 2, 2, 8])
    wqkv_d = din("w_qkv", [2, D, 1536])
    wqsw_d = din("w_qkv_sw", [2, D, 1280])
    sink_d = din("sink", [128, 2, 8])
    wo_d = din("w_o", [2, D, D])
    router_d = din("router", [128, DEPTH, KC, NE])
    wg_d = din("moe_w_gate", [DEPTH, NE, D, D])
    wu_d = din("moe_w_up", [DEPTH, NE, D, D])
    wd_d = din("moe_w_down", [DEPTH, NE, D, D])
    ident_d = din("ident", [128, 128])
    ropec_d = din("rope_c", [128, NL])
    ropes_d = din("rope_s", [128, NL])
    iota_d = din("iota_row", [128, NL])
    tokidx_d = din("tokidx", [128, 16])
    mprev_d = din("mask_prev", [128, 128])
    mnext_d = din("mask_next", [128, 128])
    pidx_d = din("pidx16", [16, 128])
    halfoff_d = din("halfoff", [32, 1])
    out_d = nc.dram_tensor("outT", [D, NL], F32, kind="ExternalOutput").ap()
    scr_d = nc.dram_tensor("scr_tabs", [DEPTH, 2, 16, 288], F32).ap()
    scr2_d = nc.dram_tensor("scr_half", [DEPTH, 2, 32, CAPL], F32).ap()
    hscr_d = [nc.dram_tensor(f"scr_htok{i}", [T, D], BF16).ap() for i in range(DEPTH)]
    dbg_d = None
    if stop_after is not None:
        dbg_d = nc.dram_tensor("dbgx", [D, T], F32, kind="ExternalOutput").ap()
        dbg_aff = nc.dram_tensor("dbg_aff", [16, T], F32, kind="ExternalOutput").ap()
        dbg_idx = nc.dram_tensor("dbg_idx", [16, 288], F32, kind="ExternalOutput").ap()
        dbg_val = nc.dram_tensor("dbg_val", [16, 288], F32, kind="ExternalOutput").ap()
        dbg_it = nc.dram_tensor("dbg_it", [128, 3, NE], F32, kind="ExternalOutput").ap()
        dbg_vt = nc.dram_tensor("dbg_vt", [128, 3, NE], F32, kind="ExternalOutput").ap()

    with ExitStack() as st:
        P = Prog(nc, st)
        bld = B(nc, P, st)
        mm, act, tt, ts, stt, cp, dma = bld.mm, bld.act, bld.tt, bld.ts, bld.stt, bld.cp, bld.dma

        def SB(stack, name, shape, dt):
            return stack.enter_context(nc.sbuf_tensor(name, list(shape), dt))

        banks = [st.enter_context(nc.psum_tensor(f"ps{i}", [128, 512], F32)) for i in range(7)]
        psb = st.enter_context(nc.psum_tensor("psb", [128, 1024], BF16))
        bld.banks = banks
        for en in ('act', 'dve', 'pool'):
            P.scratch[en] = SB(st, f"flush_{en}", [128, 1], F32)
        P.flush = os.environ.get('FLUSH', '0') == '1'
        P.pe_delay = os.environ.get('PE_DELAY', '1') == '1'

        xT = SB(st, "xT_sb", [128, KC, T], F32)
        ident_f = SB(st, "ident_f", [128, 128], F32)
        ident_b = SB(st, "ident_b", [128, 128], BF16)
        ones_b = SB(st, "ones_b", [128, 128], BF16)
        mod = SB(st, "mod", [128, DEPTH, 48, 2], F32)
        gp1 = SB(st, "gp1", [128, DEPTH, KC, 2], F32)
        gp2 = SB(st, "gp2", [128, DEPTH, KC, 2], F32)
        n1g = SB(st, "n1g_sb", [128, DEPTH, KC], F32)
        n2g = SB(st, "n2g_sb", [128, DEPTH, KC], F32)
        fing = SB(st, "fing_sb", [128, KC], F32)
        ada_b = SB(st, "ada_b_sb", [128, DEPTH, 48], F32)
        convw = SB(st, "convw_sb", [128, 2, KC, 4], F32)
        convb = SB(st, "convb_sb", [128, 2, KC], F32)
        gateb = SB(st, "gateb_sb", [128, 2, 2, 8, 2], F32)
        gatebh = SB(st, "gatebh_sb", [128, 2, 2, 8, 2], F32)
        lam = SB(st, "lam_sb", [128, 2, 2, 8], F32)
        ls4 = SB(st, "ls4", [128, 2, 2, 8], F32)
        ls8 = SB(st, "ls8", [128, 2, 2, 8], F32)
        esink = SB(st, "esink", [128, 2, 8], F32)
        router_b = SB(st, "router_b", [128, DEPTH, KC, NE], BF16)
        tokidx = SB(st, "tokidx_sb", [128, 16], F32)
        ntok = SB(st, "ntok_sb", [128, 16], F32)
        pidx16 = SB(st, "pidx16_sb", [16, 128], F32)
        halfoff = SB(st, "halfoff_sb", [32, 1], F32)
        scc = SB(st, "scc", [128, KC, 2], BF16)

        for k in range(KC):
            dma('sp', xT[:, k, :], xT_d[k * 128:(k + 1) * 128, :])
        rstk = ExitStack()
        router_f = SB(rstk, "router_f", [128, DEPTH, KC, NE], F32)
        for sb_t, d_t in [(ident_f, ident_d), (ada_b, ada_b_d), (n1g, n1g_d), (n2g, n2g_d), (fing, fing_d),
                          (convw, convw_d), (convb, convb_d), (gateb, gateb_d), (lam, lam_d), (esink, sink_d),
                          (router_f, router_d), (tokidx, tokidx_d), (pidx16, pidx_d), (halfoff, halfoff_d)]:
            dma('sp', sb_t[:], d_t)
        cp('dve', ident_b[:], ident_f[:])
        cp('dve', router_b[:], router_f[:])
        bld.memset('dve', ones_b[:], 1.0)
        act(esink[:], esink[:], AF.Exp)
        act(lam[:], lam[:], AF.Exp, scale=-1.0)
        ts('dve', lam[:], lam[:], 1.0, None, ALU.add)
        act(lam[:], lam[:], AF.Ln)
        ts('dve', ls4[:], lam[:], -4.0, None, ALU.mult)
        ts('dve', ls8[:], lam[:], -8.0, None, ALU.mult)
        ts('dve', gatebh[:], gateb[:], 0.5, None, ALU.mult)
        ts('dve', ntok[:], tokidx[:], -1.0, None, ALU.mult)
        P.barrier()
        rstk.close()

        with ExitStack() as ph:
            cc = SB(ph, "cc_sb", [128, KC, 2], F32)
            adw = [SB(ph, f"adw{i}", [128, KC, 1024], BF16) for i in range(2)]
            dma('sp', cc[:], cc_d)
            act(scc[:], cc[:], AF.Silu)
            pc = 0
            for l in range(n_layers):
                psm = bld.ps()
                for g in range(6):
                    wb = adw[pc % 2]
                    pc += 1
                    dma('pool', wb[:], ada_w_d[l, :, g * 1024:(g + 1) * 1024].rearrange("(c p) f -> p c f", p=128))
                    for jj in range(8):
                        j = g * 8 + jj
                        for k in range(KC):
                            mm(psm[:, 2 * j:2 * j + 2], wb[:, k, jj * 128:(jj + 1) * 128], scc[:, k, :],
                               start=(k == 0), stop=(k == KC - 1))
                tt('dve', mod[:, l], psm[:, 0:96].rearrange("p (j s) -> p j s", s=2),
                   ada_b[:, l, :].unsqueeze(2).to_broadcast([128, 48, 2]), ALU.add)
                for (gp, ng, grp) in ((gp1, n1g, 1), (gp2, n2g, 4)):
                    ts('dve', gp[:, l], mod[:, l, grp * 8:(grp + 1) * 8, :], 1.0, None, ALU.add)
                    tt('dve', gp[:, l], gp[:, l], ng[:, l, :].unsqueeze(2).to_broadcast([128, KC, 2]), ALU.mult)
            P.barrier()

        def mod_ap(l, grp, k, s):
            return mod[:, l, grp * 8 + k, s:s + 1]

        def norm_mod(ph, hT, l, which, tiles):
            gp = gp1 if which == 1 else gp2
            shg = 0 if which == 1 else 3
            sq = [SB(ph, f"nm_sq{i}_{l}_{which}", [128, 512], BF16) for i in range(4)]
            rs = [SB(ph, f"nm_rs{i}_{l}_{which}", [128, 512], F32) for i in range(2)]
            tmp = [SB(ph, f"nm_tmp{i}_{l}_{which}", [128, 512], F32) for i in range(4)]
            for ti, (t0, n, s) in enumerate(tiles):
                pss = bld.ps()
                for k in range(KC):
                    q = sq[k % 4]
                    if k % 2 == 0:
                        act(q[:, 0:n], xT[:, k, t0:t0 + n], AF.Square)
                    else:
                        tt('dve', q[:, 0:n], xT[:, k, t0:t0 + n], xT[:, k, t0:t0 + n], ALU.mult)
                    mm(pss[:, 0:n], ones_b[:], q[:, 0:n], start=(k == 0), stop=(k == KC - 1))
                r = rs[ti % 2]
                act(r[:, 0:n], pss[:, 0:n], AF.Ln, scale=1.0 / D, bias=EPS)
                act(r[:, 0:n], r[:, 0:n], AF.Exp, scale=-0.5)
                for k in range(KC):
                    tm = tmp[k % 4]
                    tt('dve', tm[:, 0:n], xT[:, k, t0:t0 + n], r[:, 0:n], ALU.mult)
                    act(hT[:, k, t0:t0 + n], tm[:, 0:n], AF.Identity,
                        scale=gp[:, l, k, s:s + 1], bias=mod_ap(l, shg, k, s))

        def out_proj(ph, name, w_dram, mT_, l, ggrp, tiles):
            wo = SB(ph, name, [128, KC, D], BF16)
            for h in range(2):
                dma('pool', wo[:, :, h * 512:(h + 1) * 512],
                    w_dram[:, h * 512:(h + 1) * 512].rearrange("(k p) f -> p k f", p=128))
            for (t0, n, s) in tiles:
                for dm in range(KC):
                    po = bld.ps()
                    for c in range(KC):
                        mm(po[:, 0:n], wo[:, c, dm * 128:(dm + 1) * 128], mT_[:, c, t0:t0 + n],
                           start=(c == 0), stop=(c == KC - 1))
                    stt('dve', xT[:, dm, t0:t0 + n], po[:, 0:n], mod_ap(l, ggrp, dm, s), xT[:, dm, t0:t0 + n],
                        ALU.mult, ALU.add)

        def lru_phase(l, need_ctx):
            j = l // 2
            with ExitStack() as ph:
                hT = SB(ph, f"hT_l{l}", [128, KC, T], BF16)
                mT = SB(ph, f"mT_l{l}", [128, KC, T], BF16)
                with ExitStack() as nt:
                    norm_mod(nt, hT, l, 1, TILES)
                    P.barrier()
                with ExitStack() as lt:
                    ub = SB(lt, f"ub{l}", [128, T], F32)
                    xb = SB(lt, f"xb{l}", [128, T], F32)
                    xbb = SB(lt, f"xbb{l}", [128, T], BF16)
                    winA = SB(lt, f"winA{l}", [128, KC, 128], BF16)
                    winB = SB(lt, f"winB{l}", [128, KC, 128], BF16)
                    gw = [SB(lt, f"gw{l}_{i}", [128, 256], BF16) for i in range(4)]
                    tr_ = [SB(lt, f"tr{l}_{i}", [128, 512], F32) for i in range(4)]
                    ti_ = [SB(lt, f"ti{l}_{i}", [128, 512], F32) for i in range(4)]
                    tb_ = [SB(lt, f"tb{l}_{i}", [128, 512], F32) for i in range(4)]
                    th_ = [SB(lt, f"th{l}_{i}", [128, 512], F32) for i in range(2)]
                    carry = SB(lt, f"carry{l}", [128, 2], F32)
                    for d in range(2):
                        dma('pool', gw[d][:], gatew_d[j, d, 0])
                    orders = [TILES, [TILES[0]] + TILES[:0:-1]]
                    first_dir = {0: 0, 256: 0, 768: 0, 1280: 1, 1792: 1}

                    def ldA(c):
                        dma('pool', winA[:], lwin_d[j, :, c * 128:(c + 1) * 128].rearrange("(k p) f -> p k f", p=128))

                    def ldB(c):
                        dma('pool', winB[:], lwin_d[j, :, D + c * 128:D + (c + 1) * 128].rearrange("(k p) f -> p k f", p=128))
                    ldB(0)
                    ldA(0)
                    for c in range(KC):
                        if c + 1 < KC:
                            for d in range(2):
                                dma('pool', gw[((c + 1) % 2) * 2 + d][:], gatew_d[j, d, c + 1])
                        for (t0, n, s) in TILES:
                            pu = bld.ps()
                            for k in range(KC):
                                mm(pu[:, 0:n], winB[:, k, :], hT[:, k, t0:t0 + n], start=(k == 0), stop=(k == KC - 1))
                            cp(bld.alt(), ub[:, t0:t0 + n], pu[:, 0:n])
                        if c + 1 < KC:
                            ldB(c + 1)
                        for (t0, n, s) in TILES:
                            s0, sn = (0, NCX) if s == 1 else (NCX, NL)
                            ts('dve', xb[:, t0:t0 + n], ub[:, t0:t0 + n], convw[:, j, c, 1:2], convb[:, j, c:c + 1], ALU.mult, ALU.add)
                            for (o, kk) in ((-1, 0), (1, 2), (2, 3)):
                                lo = max(t0, s0 - o)
                                hi = min(t0 + n, s0 + sn - o)
                                stt('dve', xb[:, lo:hi], ub[:, lo + o:hi + o], convw[:, j, c, kk:kk + 1], xb[:, lo:hi], ALU.mult, ALU.add)
                            cp('pool', xbb[:, t0:t0 + n], xb[:, t0:t0 + n])
                        for (ga, gb_) in ((0, 1), (1, 3), (3, 5)):
                            for d in range(2):
                                g = gw[(c % 2) * 2 + d]
                                for gi, (t0, n, s) in enumerate(orders[d][ga:gb_]):
                                    slot = d * 2 + gi
                                    pr = bld.ps()
                                    pi = bld.ps()
                                    mm(pr[:, 0:n], g[:, 0:128], xbb[:, t0:t0 + n])
                                    mm(pi[:, 0:n], g[:, 128:256], xbb[:, t0:t0 + n])
                                    r_ = tr_[slot]
                                    i_ = ti_[slot]
                                    b_ = tb_[slot]
                                    act(r_[:, 0:n], pr[:, 0:n], AF.Tanh, scale=0.5, bias=gatebh[:, j, d, c, 0:1])
                                    act(i_[:, 0:n], pi[:, 0:n], AF.Tanh, scale=0.5, bias=gatebh[:, j, d, c, 1:2])
                                    act(b_[:, 0:n], r_[:, 0:n], AF.Exp, scale=ls8[:, j, d, c:c + 1], bias=ls8[:, j, d, c:c + 1])
                                    act(r_[:, 0:n], r_[:, 0:n], AF.Exp, scale=ls4[:, j, d, c:c + 1], bias=ls4[:, j, d, c:c + 1])
                                    ts('dve', b_[:, 0:n], b_[:, 0:n], 1.0, None, ALU.min)
                            for d in range(2):
                                for gi, (t0, n, s) in enumerate(orders[d][ga:gb_]):
                                    slot = d * 2 + gi
                                    r_ = tr_[slot]
                                    i_ = ti_[slot]
                                    b_ = tb_[slot]
                                    act(b_[:, 0:n], b_[:, 0:n], AF.Sqrt, scale=-0.25, bias=0.25)
                                    stt('dve', i_[:, 0:n], i_[:, 0:n], 1.0, xb[:, t0:t0 + n], ALU.add, ALU.mult)
                                    tt('dve', b_[:, 0:n], b_[:, 0:n], i_[:, 0:n], ALU.mult)
                                    init = 0.0 if ga == 0 else carry[:, d:d + 1]
                                    first = first_dir[t0] == d
                                    dst = ub[:, t0:t0 + n] if first else th_[gi][:, 0:n]
                                    if d == 0:
                                        bld.scan(dst, r_[:, 0:n], b_[:, 0:n], init)
                                        cp('dve', carry[:, 0:1], dst[:, n - 1:n])
                                    else:
                                        bld.scan(dst[:, ::-1], r_[:, n - 1::-1], b_[:, n - 1::-1], init)
                                        cp('dve', carry[:, 1:2], dst[:, 0:1])
                                    if not first:
                                        tt('pool', ub[:, t0:t0 + n], ub[:, t0:t0 + n], dst, ALU.add)
                        for ti, (t0, n, s) in enumerate(TILES):
                            if s == 1 and not need_ctx:
                                continue
                            pg = bld.ps()
                            for k in range(KC):
                                mm(pg[:, 0:n], winA[:, k, :], hT[:, k, t0:t0 + n], start=(k == 0), stop=(k == KC - 1))
                            y_ = th_[ti % 2]
                            act(y_[:, 0:n], pg[:, 0:n], AF.Gelu_apprx_tanh)
                            tt('dve', mT[:, c, t0:t0 + n], y_[:, 0:n], ub[:, t0:t0 + n], ALU.mult)
                        if c + 1 < KC:
                            ldA(c + 1)
                    P.barrier()
                with ExitStack() as ot:
                    tiles = TILES if need_ctx else TILES[1:]
                    out_proj(ot, f"lwo{l}", lwout_d[j], mT, l, 2, tiles)
                    P.barrier()

        def attn_phase(l, need_ctx):
            j = l // 2
            with ExitStack() as ph:
                hT = SB(ph, f"hT_l{l}", [128, KC, T], BF16)
                qT = SB(ph, f"qT_l{l}", [128, 8, T], BF16)
                kT = SB(ph, f"kT_l{l}", [128, 2, T], BF16)
                V = SB(ph, f"V_l{l}", [128, 18, 256], BF16)
                with ExitStack() as nt:
                    norm_mod(nt, hT, l, 1, TILES)
                    P.barrier()
                with ExitStack() as qt:
                    rc = SB(qt, f"rc{l}", [128, NL], F32)
                    rs_ = SB(qt, f"rs{l}", [128, NL], F32)
                    dma('sp', rc[:], ropec_d)
                    dma('sp', rs_[:], ropes_d)
                    wq = [SB(qt, f"wq{l}_{i}", [128, KC, 128], BF16) for i in range(2)]
                    ws = [SB(qt, f"ws{l}_{i}", [128, KC, 128], BF16) for i in range(2)]
                    wv = SB(qt, f"wv{l}", [128, KC, 256], BF16)
                    t1 = [SB(qt, f"t1{l}_{i}", [128, 512], F32) for i in range(2)]
                    t2 = [SB(qt, f"t2{l}_{i}", [128, 512], F32) for i in range(2)]
                    for hh in range(10):
                        a = wq[hh % 2]
                        b = ws[hh % 2]
                        dma('pool', a[:], wqkv_d[j, :, hh * 128:(hh + 1) * 128].rearrange("(k p) f -> p k f", p=128))
                        dma('pool', b[:], wqsw_d[j, :, hh * 128:(hh + 1) * 128].rearrange("(k p) f -> p k f", p=128))
                        dst = qT[:, hh, :] if hh < 8 else kT[:, hh - 8, :]
                        for ti, (t0, n, s) in enumerate(TILES):
                            if s == 1 and hh < 8 and not need_ctx:
                                continue
                            p1 = bld.ps()
                            for k in range(KC):
                                mm(p1[:, 0:n], a[:, k, :], hT[:, k, t0:t0 + n], start=(k == 0), stop=(k == KC - 1))
                            if s == 1:
                                cp('act', dst[:, t0:t0 + n], p1[:, 0:n])
                                continue
                            p2 = bld.ps()
                            for k in range(KC):
                                mm(p2[:, 0:n], b[:, k, :], hT[:, k, t0:t0 + n], start=(k == 0), stop=(k == KC - 1))
                            a1 = t1[ti % 2]
                            a2 = t2[ti % 2]
                            tt('dve', a1[:, 0:n], p1[:, 0:n], rc[:, t0 - NCX:t0 - NCX + n], ALU.mult)
                            tt('dve', a2[:, 0:n], p2[:, 0:n], rs_[:, t0 - NCX:t0 - NCX + n], ALU.mult)
                            tt('pool', dst[:, t0:t0 + n], a1[:, 0:n], a2[:, 0:n], ALU.add)
                    dma('pool', wv[:], wqkv_d[j, :, 1280:1536].rearrange("(k p) f -> p k f", p=128))
                    for blk in range(18):
                        pv = bld.ps()
                        for k in range(KC):
                            mm(pv[:, 0:256], hT[:, k, blk * 128:(blk + 1) * 128], wv[:, k, :], start=(k == 0), stop=(k == KC - 1))
                        cp(bld.alt(), V[:, blk, :], pv[:, 0:256])
                    P.barrier()
                with ExitStack() as at:
                    oT = hT
                    mprev = SB(at, f"mprev{l}", [128, 128], BF16)
                    mnext = SB(at, f"mnext{l}", [128, 128], BF16)
                    dma('pool', mprev[:], mprev_d)
                    dma('pool', mnext[:], mnext_d)
                    ex = [SB(at, f"ex{l}_{i}", [128, 512], BF16) for i in range(6)]
                    dn = [SB(at, f"dn{l}_{i}", [128, 512], F32) for i in range(2)]
                    exc = 0
                    qblocks = [(b, True) for b in range(2, 18)]
                    if need_ctx:
                        qblocks += [(0, False), (1, False)]
                    for qi, (qb, is_lat) in enumerate(qblocks):
                        q0 = qb * 128
                        for kvh in range(2):
                            keys = [(0, None), (1, None)]
                            if is_lat:
                                if qb > 2:
                                    keys.append((qb - 1, mprev))
                                keys.append((qb, None))
                                if qb < 17:
                                    keys.append((qb + 1, mnext))
                            qsl = qT[:, kvh * 4:(kvh + 1) * 4, q0:q0 + 128]
                            po = bld.ps()
                            pd = bld.ps()
                            es = []
                            for (kb, msk) in keys:
                                psc = bld.ps()
                                mm(psc[:, :].rearrange("p (g q) -> p g q", g=4), kT[:, kvh, kb * 128:(kb + 1) * 128], qsl)
                                e_ = ex[exc % 6]
                                exc += 1
                                act(e_[:], psc[:], AF.Exp, scale=QSCALE)
                                if msk is not None:
                                    e3 = e_[:, :].rearrange("p (g q) -> p g q", g=4)
                                    tt('pool', e3, e3, msk[:, :].unsqueeze(1).to_broadcast([128, 4, 128]), ALU.mult)
                                es.append((kb, e_))
                            for i, (kb, e_) in enumerate(es):
                                mm(po[:], V[:, kb, kvh * 128:(kvh + 1) * 128], e_[:], start=(i == 0), stop=(i == len(es) - 1))
                            for i, (kb, e_) in enumerate(es):
                                mm(pd[:], ones_b[:], e_[:], start=(i == 0), stop=(i == len(es) - 1))
                            d_ = dn[(qi * 2 + kvh) % 2]
                            tt('dve', d_[:, :].rearrange("p (g q) -> p g q", g=4), pd[:, :].rearrange("p (g q) -> p g q", g=4),
                               esink[:, j, kvh * 4:(kvh + 1) * 4].unsqueeze(2).to_broadcast([128, 4, 128]), ALU.add)
                            P.op('dve', (lambda dd: lambda e: e.reciprocal(dd[:], dd[:]))(d_), reads=[d_[:]], writes=[d_[:]])
                            tt('dve', oT[:, kvh * 4:(kvh + 1) * 4, q0:q0 + 128], po[:, :].rearrange("p (g q) -> p g q", g=4),
                               d_[:, :].rearrange("p (g q) -> p g q", g=4), ALU.mult)
                    P.barrier()
                with ExitStack() as ot:
                    tiles = TILES if need_ctx else TILES[1:]
                    out_proj(ot, f"awo{l}", wo_d[j], oT, l, 2, tiles)
                    P.barrier()

        def moe_phase(l, need_ctx):
            tiles = TILES if need_ctx else TILES[1:]
            CT = CAPL + CAPC if need_ctx else CAPL
            cbs = [(0, 128, 0), (1, 128, 128)] + ([(2, 32, 256)] if need_ctx else [])
            with ExitStack() as ph:
                idx_i = SB(ph, f"idxi{l}", [128, 3, NE], I32)
                val_tok = SB(ph, f"valtok{l}", [128, 3, NE], F32)
                nbias = SB(ph, f"nbias{l}", [128, 3, NE, 4], F32)
                iota_row = SB(ph, f"iota{l}", [128, 512], F32)
                dma('sp', iota_row[:], iota_d[:, 0:512])
                NW = 6
                wring = [SB(ph, f"wr{l}_{i}", [128, KC, 512], BF16) for i in range(NW)]
                pieces = []
                for e in range(NE):
                    for fh in range(2):
                        pieces.append(wg_d[l, e, :, fh * 512:(fh + 1) * 512])
                        pieces.append(wu_d[l, e, :, fh * 512:(fh + 1) * 512])
                    for dh in range(2):
                        pieces.append(wd_d[l, e, :, dh * 512:(dh + 1) * 512])
                issued = [6 * MOE_E0]

                def ensure(i):
                    while issued[0] < min(len(pieces), i + NW):
                        pi_ = issued[0]
                        dma('pool', wring[pi_ % NW][:], pieces[pi_].rearrange("(k p) f -> p k f", p=128))
                        issued[0] += 1

                ensure(6 * MOE_E0)
                with ExitStack() as rt:
                    hT = SB(rt, f"hTm{l}", [128, KC, T], BF16)
                    htok = [SB(rt, f"htok{l}_{i}", [128, D], BF16) for i in range(3)]
                    with ExitStack() as nt:
                        norm_mod(nt, hT, l, 2, tiles)
                        P.barrier()
                    aff = SB(rt, f"aff{l}", [128, 18, NE], F32)
                    mx = SB(rt, f"mx{l}", [128, 18], F32)
                    affC = SB(rt, f"affC{l}", [16, NCX], F32)
                    affL = SB(rt, f"affL{l}", [32, 1024], F32)
                    valsL = SB(rt, f"valsL{l}", [32, CAPL], F32)
                    idxL_u = SB(rt, f"idxLu{l}", [32, CAPL], U32)
                    idxL_f = SB(rt, f"idxLf{l}", [32, CAPL], F32)
                    Bv = SB(rt, f"Bv{l}", [16, CAPL], F32)
                    Bi = SB(rt, f"Bi{l}", [16, CAPL], F32)
                    sel = SB(rt, f"sel{l}", [16, CAPL], F32)
                    dd = SB(rt, f"dd{l}", [16, CAPL], F32)
                    vals = SB(rt, f"vals{l}", [16, 288], F32)
                    idx_u = SB(rt, f"idxu{l}", [16, 288], U32)
                    idx_f = SB(rt, f"idxf{l}", [16, 288], F32)
                    idxT = SB(rt, f"idxT{l}", [128, 3, NE], F32)
                    blks = list(range(18)) if need_ctx else list(range(2, 18))
                    pl = bld.ps()

                    def posb(b):
                        if b < 2:
                            return b
                        lb = b - 2
                        return 2 + (lb % 8) * 2 + lb // 8
                    for b in blks:
                        pb_ = posb(b)
                        for k in range(KC):
                            mm(pl[:, pb_ * NE:(pb_ + 1) * NE], hT[:, k, b * 128:(b + 1) * 128], router_b[:, l, k, :],
                               start=(k == 0), stop=(k == KC - 1))
                    b0, nb = blks[0], len(blks)
                    pl3 = pl[:, b0 * NE:(b0 + nb) * NE].rearrange("p (b e) -> p b e", e=NE)
                    a3 = aff[:, b0:b0 + nb, :]
                    P.op('dve', lambda e: e.tensor_reduce(mx[:, b0:b0 + nb], pl3, AX.X, ALU.max), reads=[pl3], writes=[mx[:, b0:b0 + nb]])
                    tt('dve', a3, pl3, mx[:, b0:b0 + nb].unsqueeze(2).to_broadcast([128, nb, NE]), ALU.subtract)
                    act(a3, a3, AF.Exp)
                    P.op('dve', lambda e: e.tensor_reduce(mx[:, b0:b0 + nb], a3, AX.X, ALU.add), reads=[a3], writes=[mx[:, b0:b0 + nb]])
                    P.op('dve', lambda e: e.reciprocal(mx[:, b0:b0 + nb], mx[:, b0:b0 + nb]), reads=[mx[:, b0:b0 + nb]], writes=[mx[:, b0:b0 + nb]])
                    tt('dve', a3, a3, mx[:, b0:b0 + nb].unsqueeze(2).to_broadcast([128, nb, NE]), ALU.mult)
                    if need_ctx:
                        pt = bld.ps()
                        for b in range(2):
                            bld.tr(pt[0:16, b * 128:(b + 1) * 128], aff[:, b, :], ident_f[:])
                        cp('act', affC[:, 0:NCX], pt[0:16, 0:NCX])
                    for g in range(2):
                        pt = bld.ps()
                        for bb in range(4):
                            b = g * 4 + bb
                            bld.tr(pt[0:32, bb * 128:(bb + 1) * 128], aff[:, 2 + 2 * b:4 + 2 * b, :].rearrange("p a e -> p (a e)"), ident_f[:])
                        cp('act', affL[:, g * 512:(g + 1) * 512], pt[0:32, :])
                    for b in blks:
                        for half in range(2):
                            pt = bld.ps()
                            for kk in range(4):
                                k = half * 4 + kk
                                bld.tr(pt[:, kk * 128:(kk + 1) * 128], hT[:, k, b * 128:(b + 1) * 128], ident_b[:])
                            cp('act', htok[b % 3][:, half * 512:(half + 1) * 512], pt[:])
                        dma('sp', hscr_d[l][b * 128:(b + 1) * 128, :], htok[b % 3][:])
                    def topk(w, vout, iout, cap):
                        for it in range(cap // 8):
                            v8 = vout[:, it * 8:it * 8 + 8]
                            i8 = iout[:, it * 8:it * 8 + 8]
                            P.op('dve', (lambda v8, w: lambda e: e.max(out=v8, in_=w))(v8, w), reads=[w], writes=[v8])
                            P.op('dve', (lambda v8, i8, w: lambda e: e.max_index(out=i8, in_max=v8, in_values=w))(v8, i8, w), reads=[w, v8], writes=[i8])
                            P.op('dve', (lambda v8, w: lambda e: e.match_replace(out=w, in_to_replace=v8, in_values=w, imm_value=0.0))(v8, w), reads=[w, v8], writes=[w])
                    topk(affL[:], valsL[:], idxL_u[:], CAPL)
                    if need_ctx:
                        topk(affC[:], vals[:, CAPL:CAPL + CAPC], idx_u[:, CAPL:CAPL + CAPC], CAPC)
                        cp('dve', idx_f[:, CAPL:CAPL + CAPC], idx_u[:, CAPL:CAPL + CAPC])
                    cp('dve', idxL_f[:], idxL_u[:])
                    ts('dve', idxL_f[:], idxL_f[:], halfoff[:, 0:1], None, ALU.add)
                    dma('sp', scr2_d[l, 0], valsL[:])
                    dma('sp', scr2_d[l, 1], idxL_f[:])
                    dma('sp', Bv[:], scr2_d[l, 0, 16:32, :])
                    dma('sp', Bi[:], scr2_d[l, 1, 16:32, :])
                    tt('dve', sel[:], valsL[0:16, :], Bv[:, ::-1], ALU.is_gt)
                    for (A_, B_, out_) in ((valsL[0:16, :], Bv, vals[:, 0:CAPL]), (idxL_f[0:16, :], Bi, idx_f[:, 0:CAPL])):
                        tt('dve', dd[:], A_, B_[:, ::-1], ALU.subtract)
                        tt('dve', dd[:], dd[:], sel[:], ALU.mult)
                        tt('dve', out_, dd[:], B_[:, ::-1], ALU.add)
                    if not need_ctx:
                        bld.memset('dve', idx_f[:, CAPL:288], 0.0)
                        bld.memset('dve', vals[:, CAPL:288], 0.0)
                    dma('sp', scr_d[l, 0], idx_f[:])
                    dma('sp', scr_d[l, 1], vals[:])
                    for (cb, m, c0) in cbs:
                        P.dma('sp', idxT[0:m, cb, :], scr_d[l, 0, :, c0:c0 + m].rearrange("e c -> c e"), allow_slow_non_contiguous=True)
                        P.dma('sp', val_tok[0:m, cb, :], scr_d[l, 1, :, c0:c0 + m].rearrange("e c -> c e"), allow_slow_non_contiguous=True)
                    for (cb, m, c0) in cbs:
                        if cb < 2:
                            ts('dve', nbias[0:m, cb, :, 0], idxT[0:m, cb, :], float(NCX), None, ALU.add)
                            cp('dve', idx_i[0:m, cb, :], nbias[0:m, cb, :, 0])
                            for q4 in range(4):
                                ts('dve', nbias[0:m, cb, :, q4], idxT[0:m, cb, :], -1.0, float(q4 * 512), ALU.mult, ALU.add)
                        else:
                            cp('dve', idx_i[0:m, cb, :], idxT[0:m, cb, :])
                            ts('dve', nbias[0:m, cb, :, 0], idxT[0:m, cb, :], -1.0, None, ALU.mult)
                    P.barrier()
                with ExitStack() as et:
                    xgt = [SB(et, f"xgt{l}_{i}", [128, 3, D], BF16) for i in range(2)]
                    dtmp = [SB(et, f"dtmp{l}_{i}", [128, 512], F32) for i in range(3)]
                    xg = SB(et, f"xg{l}", [128, KC, 288], BF16)
                    sa = [SB(et, f"sa{l}_{i}", [128, 288], F32) for i in range(2)]
                    actT = SB(et, f"actT{l}", [128, KC, 288], BF16)
                    ye = [SB(et, f"ye{l}_{i}", [128, 3, D], BF16) for i in range(2)]
                    ST_lat = [SB(et, f"STl{l}_{i}", [128, 2, NL], BF16) for i in range(2)]
                    ST_ctx = [SB(et, f"STc{l}_{i}", [32, NCX], BF16) for i in range(2)]

                    def wget(i, base):
                        ensure(base)
                        assert i < issued[0]
                        return wring[i % NW]

                    def gather(e):
                        buf = xgt[e % 2]
                        for (cb, m, c0) in cbs:
                            def mk(cb, m, e):
                                return lambda en, out, in_, kw: en.indirect_dma_start(
                                    out=out, out_offset=None, in_=in_,
                                    in_offset=bass.IndirectOffsetOnAxis(ap=idx_i[0:m, cb, e:e + 1], axis=0))
                            P.dma_custom('pool', buf[0:m, cb, :], hscr_d[l][:, :], mk(cb, m, e), extra_reads=[idx_i[0:m, cb, e:e + 1]])

                    gather(MOE_E0)
                    for e in range(MOE_E0, MOE_E1):
                        par = e % 2
                        if e + 1 < MOE_E1:
                            gather(e + 1)
                        ensure(6 * e)
                        st_jobs = []
                        for cb in range(2):
                            for q4 in range(4):
                                def job(cb=cb, q4=q4, e=e, par=par):
                                    dt_ = dtmp[(cb * 4 + q4) % 3]
                                    act(dt_[:], iota_row[:], AF.Abs, bias=nbias[:, cb, e, q4:q4 + 1])
                                    act(ST_lat[par][:, cb, q4 * 512:(q4 + 1) * 512], dt_[:], AF.Relu, scale=-1.0, bias=1.0)
                                st_jobs.append(job)
                        if need_ctx:
                            def jobc(e=e, par=par):
                                dt_ = dtmp[2]
                                act(dt_[0:32, 0:NCX], iota_row[0:32, 0:NCX], AF.Abs, bias=nbias[0:32, 2, e, 0:1])
                                act(ST_ctx[par][:], dt_[0:32, 0:NCX], AF.Relu, scale=-1.0, bias=1.0)
                            st_jobs.append(jobc)
                        buf = xgt[par]
                        for k in range(KC):
                            pg = bld.ps()
                            for (cb, m, c0) in cbs:
                                mm(pg[:, c0:c0 + m], buf[0:m, cb, k * 128:(k + 1) * 128], ident_b[0:m, 0:m])
                            cp(bld.alt(), xg[:, k, 0:CT], pg[:, 0:CT])
                        for fh in range(2):
                            wgb = wget(6 * e + 2 * fh, 6 * e + 2 * fh)
                            wub = wget(6 * e + 2 * fh + 1, 6 * e + 2 * fh)
                            for ff in range(4):
                                f = fh * 4 + ff
                                pa = bld.ps()
                                pu = bld.ps()
                                for k in range(KC):
                                    mm(pa[:, 0:CT], wgb[:, k, ff * 128:(ff + 1) * 128], xg[:, k, 0:CT], start=(k == 0), stop=(k == KC - 1))
                                for k in range(KC):
                                    mm(pu[:, 0:CT], wub[:, k, ff * 128:(ff + 1) * 128], xg[:, k, 0:CT], start=(k == 0), stop=(k == KC - 1))
                                s_ = sa[f % 2]
                                act(s_[:, 0:CT], pa[:, 0:CT], AF.Silu)
                                tt('dve', actT[:, f, 0:CT], s_[:, 0:CT], pu[:, 0:CT], ALU.mult)
                                if st_jobs:
                                    st_jobs.pop(0)()
                                if f == KC - 1:
                                    while st_jobs:
                                        st_jobs.pop(0)()
                        for dh in range(2):
                            wdb = wget(6 * e + 4 + dh, 6 * e + 4 + dh)
                            for (cb, m, c0) in cbs:
                                py = bld.ps()
                                for f in range(KC):
                                    mm(py[0:m, :], actT[:, f, c0:c0 + m], wdb[:, f, :], start=(f == 0), stop=(f == KC - 1))
                                act(ye[par][0:m, cb, dh * 512:(dh + 1) * 512], py[0:m, :], AF.Copy, scale=val_tok[0:m, cb, e:e + 1])
                        if par == 1:
                            for k in range(KC):
                                for (t0, n, s) in TILES[1:]:
                                    pz = bld.ps()
                                    i = 0
                                    for pp in range(2):
                                        for cb in range(2):
                                            mm(pz[:, 0:n], ye[pp][:, cb, k * 128:(k + 1) * 128], ST_lat[pp][:, cb, t0 - NCX:t0 - NCX + n],
                                               start=(i == 0), stop=(i == 3))
                                            i += 1
                                    stt('dve', xT[:, k, t0:t0 + n], pz[:, 0:n], mod_ap(l, 5, k, 0), xT[:, k, t0:t0 + n], ALU.mult, ALU.add)
                                if need_ctx:
                                    pz = bld.ps()
                                    for pp in range(2):
                                        mm(pz[:, 0:NCX], ye[pp][0:32, 2, k * 128:(k + 1) * 128], ST_ctx[pp][:], start=(pp == 0), stop=(pp == 1))
                                    stt('dve', xT[:, k, 0:NCX], pz[:, 0:NCX], mod_ap(l, 5, k, 1), xT[:, k, 0:NCX], ALU.mult, ALU.add)
                    P.barrier()

        def final_phase():
            with ExitStack() as ph:
                sq = [SB(ph, f"fn_sq{i}", [128, 512], BF16) for i in range(4)]
                rs = [SB(ph, f"fn_rs{i}", [128, 512], F32) for i in range(2)]
                ot = [SB(ph, f"fn_o{i}", [128, 512], F32) for i in range(3)]
                oc = 0
                for ti, (t0, n, s) in enumerate(TILES[1:]):
                    pss = bld.ps()
                    for k in range(KC):
                        q = sq[k % 4]
                        act(q[:, 0:n], xT[:, k, t0:t0 + n], AF.Square)
                        mm(pss[:, 0:n], ones_b[:], q[:, 0:n], start=(k == 0), stop=(k == KC - 1))
                    r = rs[ti % 2]
                    act(r[:, 0:n], pss[:, 0:n], AF.Ln, scale=1.0 / D, bias=EPS)
                    act(r[:, 0:n], r[:, 0:n], AF.Exp, scale=-0.5)
                    for k in range(KC):
                        o = ot[oc % 3]
                        oc += 1
                        stt('dve', o[:, 0:n], xT[:, k, t0:t0 + n], fing[:, k:k + 1], r[:, 0:n], ALU.mult, ALU.mult)
                        dma('sp', out_d[k * 128:(k + 1) * 128, t0 - NCX:t0 - NCX + n], o[:, 0:n])
                P.barrier()

        def dump():
            for k in range(KC):
                dma('sp', dbg_d[k * 128:(k + 1) * 128, :], xT[:, k, :])
            P.barrier()

        done = False
        for l in range(n_layers):
            need_ctx = l < DEPTH - 1
            if l % 2 == 0:
                lru_phase(l, need_ctx)
            else:
                attn_phase(l, need_ctx)
            if stop_after == (l, 'mix'):
                dump()
                done = True
                break
            moe_phase(l, need_ctx)
            if stop_after == (l, 'moe'):
                dump()
                done = True
                break
        if not done:
            final_phase()
            if stop_after is not None:
                dump()
        P.emit()
    return nc, P


def _fm(v):
    v = np.asarray(v, np.float32)
    lead = v.shape[:-1]
    r = v.reshape(lead + (8, 128))
    return np.ascontiguousarray(np.moveaxis(r, -1, 0))


def make_in_maps(inp):
    f32 = np.float32
    x = np.asarray(inp["x"], f32)
    ctx = np.asarray(inp["ctx"], f32)
    c = np.asarray(inp["c"], f32)
    c_ctx = np.asarray(inp["c_ctx"], f32)
    nb = x.shape[0]
    shared = {}
    shared["ada_w"] = np.ascontiguousarray(inp["ada_w"], f32)
    shared["ada_b"] = np.ascontiguousarray(np.asarray(inp["ada_b"], f32).reshape(4, 48, 128).transpose(2, 0, 1))
    shared["n1g"] = _fm(inp["norm1_g"])
    shared["n2g"] = _fm(inp["norm2_g"])
    shared["fing"] = _fm(inp["final_g"])
    shared["lru_w_in"] = np.ascontiguousarray(inp["lru_w_in"], f32)
    shared["lru_w_out"] = np.ascontiguousarray(inp["lru_w_out"], f32)
    cw = np.asarray(inp["lru_conv_w"], f32)
    shared["conv_w"] = np.ascontiguousarray(cw.reshape(2, 4, 8, 128).transpose(3, 0, 2, 1))
    shared["conv_b"] = _fm(inp["lru_conv_b"])
    shared["gate_w"] = np.ascontiguousarray(inp["lru_gate_w"], f32)
    gb = np.asarray(inp["lru_gate_b"], f32)
    shared["gate_b"] = np.ascontiguousarray(gb.reshape(2, 2, 8, 2, 128).transpose(4, 0, 1, 2, 3))
    shared["lam"] = _fm(inp["lru_lambda"])
    wqkv = np.asarray(inp["attn_w_qkv"], f32)
    shared["w_qkv"] = np.ascontiguousarray(wqkv)
    perm = np.concatenate([np.arange(32, 64), np.arange(0, 32), np.arange(96, 128), np.arange(64, 96)])
    cols = (np.arange(10)[:, None] * 128 + perm[None, :]).reshape(-1)
    shared["w_qkv_sw"] = np.ascontiguousarray(wqkv[:, :, cols])
    shared["sink"] = np.ascontiguousarray(np.broadcast_to(np.asarray(inp["attn_sink"], f32)[None], (128, 2, 8)))
    shared["w_o"] = np.ascontiguousarray(inp["attn_w_o"], f32)
    r = np.asarray(inp["moe_router"], f32)
    shared["router"] = np.ascontiguousarray(r.reshape(4, 8, 128, 16).transpose(2, 0, 1, 3))
    shared["moe_w_gate"] = np.ascontiguousarray(inp["moe_w_gate"], f32)
    shared["moe_w_up"] = np.ascontiguousarray(inp["moe_w_up"], f32)
    shared["moe_w_down"] = np.ascontiguousarray(inp["moe_w_down"], f32)
    shared["ident"] = np.eye(128, dtype=f32)
    half = 64
    freqs = (10000.0 ** (-np.arange(0, half, 2, dtype=np.float32) / half)).astype(f32)
    t = np.arange(NL)
    rows = (t // 64).astype(f32)
    colsp = (t % 64).astype(f32)
    rc = np.zeros((128, NL), f32)
    rs = np.zeros((128, NL), f32)
    for p in range(128):
        pos = rows if p < 64 else colsp
        jf = p % 32
        ang = (pos * freqs[jf]).astype(f32)
        rc[p] = np.cos(ang)
        sgn = -1.0 if (p % 64) < 32 else 1.0
        rs[p] = sgn * np.sin(ang)
    shared["rope_c"] = rc
    shared["rope_s"] = rs
    shared["iota_row"] = np.ascontiguousarray(np.broadcast_to(np.arange(NL, dtype=f32)[None], (128, NL)))
    shared["tokidx"] = (np.arange(128, dtype=f32)[:, None] + 128.0 * np.arange(16, dtype=f32)[None, :]).astype(f32)
    s_i = np.arange(128)[:, None]
    q_i = np.arange(128)[None, :]
    shared["mask_prev"] = (s_i >= q_i).astype(f32)
    shared["mask_next"] = (s_i <= q_i).astype(f32)
    shared["halfoff"] = np.concatenate([np.zeros((16, 1), f32), np.full((16, 1), 1024.0, f32)], axis=0)
    shared["pidx16"] = np.ascontiguousarray(np.broadcast_to(np.arange(16, dtype=f32)[:, None], (16, 128)))
    maps = []
    for b in range(nb):
        m = dict(shared)
        m["xT"] = np.ascontiguousarray(np.concatenate([ctx[b].T, x[b].T], axis=1))
        cc = np.stack([c[b].reshape(8, 128).T, c_ctx.reshape(8, 128).T], axis=-1)
        m["cc"] = np.ascontiguousarray(cc, dtype=f32)
        maps.append(m)
    return maps


_CACHE = {}


def kernel(**inputs):
    if "nc" not in _CACHE:
        _CACHE["nc"] = build_program()[0]
    nc = _CACHE["nc"]
    maps = make_in_maps(inputs)
    res = run_bass_kernel_spmd(nc, maps, core_ids=list(range(len(maps))))
    out = np.stack([np.ascontiguousarray(r["outT"].T) for r in res.results], axis=0)
    return out.astype(np.float32)
```

```python
import numpy as np
import concourse.bass as bass
import concourse.mybir as mybir

F32 = mybir.dt.float32
BF16 = mybir.dt.bfloat16
I32 = mybir.dt.int32
U32 = mybir.dt.uint32
AF = mybir.ActivationFunctionType
ALU = mybir.AluOpType
AX = mybir.AxisListType

ENGS = ['pe', 'act', 'dve', 'pool', 'sp']
NDSEM = 6
PE_DELAY_OPS = {'act': 1, 'dve': 4, 'pool': 4}


def _prod(xs):
    r = 1
    for v in xs:
        r *= int(v)
    return r


def region(ap):
    t = ap.tensor
    name = ap.name
    shape = tuple(t.shape)
    pairs = [(int(s), int(c)) for s, c in ap.ap]
    off = int(ap.offset)
    sp = str(ap.space)
    if 'DRAM' in sp.upper() or 'HBM' in sp.upper():
        lo = off
        hi = off
        for s, c in pairs:
            if s >= 0:
                hi += s * (c - 1)
            else:
                lo += s * (c - 1)
        return (name, 0, 1, lo, hi + 1)
    fsz = _prod(shape[1:])
    p0 = off // fsz
    rem = off % fsz
    ps, pc = pairs[0]
    p1 = p0 + (ps // fsz) * (pc - 1) + 1 if pc > 1 else p0 + 1
    lo = rem
    hi = rem
    for s, c in pairs[1:]:
        if s >= 0:
            hi += s * (c - 1)
        else:
            lo += s * (c - 1)
    return (name, p0, p1, lo, hi + 1)


def _overlap(a, b):
    return a[1] < b[2] and b[1] < a[2] and a[3] < b[4] and b[3] < a[4]


def _contains(a, b):
    return a[1] <= b[1] and b[2] <= a[2] and a[3] <= b[3] and b[4] <= a[4]


class Prog:
    def __init__(self, nc, stack):
        self.nc = nc
        self.ops = {e: [] for e in ENGS}
        self.cnt = {e: 0 for e in ENGS}
        self.sems = {}
        for e in ENGS:
            self.sems[('e', e)] = stack.enter_context(nc.semaphore(f"s_{e}"))
        self.dcnt = {}
        self.dnext = {}
        for q in ['sp', 'act', 'pool']:
            self.dnext[q] = 0
            for i in range(NDSEM):
                self.sems[('d', q, i)] = stack.enter_context(nc.semaphore(f"d_{q}{i}"))
                self.dcnt[(q, i)] = 0
        self.waited = {e: {} for e in ENGS}
        self.wr = {}
        self.rd = {}
        self.nwaits = 0
        self.flush = False
        self.pe_delay = True
        self.scratch = {}

    def _deps(self, reads, writes, token):
        deps = {}

        def add(k, v):
            if deps.get(k, 0) < v:
                deps[k] = v
        for ap in reads:
            r = region(ap)
            w = self.wr.setdefault(r[0], {})
            for (rg, sk), v in w.items():
                if _overlap(rg, r):
                    add(sk, v)
        for ap in writes:
            r = region(ap)
            w = self.wr.setdefault(r[0], {})
            d = self.rd.setdefault(r[0], {})
            for tab in (w, d):
                dead = []
                for (rg, sk), v in tab.items():
                    if _overlap(rg, r):
                        add(sk, v)
                        if _contains(r, rg):
                            dead.append((rg, sk))
                for k in dead:
                    del tab[k]
        for ap in reads:
            r = region(ap)
            self.rd.setdefault(r[0], {})[(r, token[0])] = token[1]
        for ap in writes:
            r = region(ap)
            self.wr.setdefault(r[0], {})[(r, token[0])] = token[1]
        return deps

    def _emit_waits(self, eng, deps):
        pe_wait = False
        for sk, v in deps.items():
            if eng == 'pe' and sk == ('e', 'pe'):
                continue
            if self.waited[eng].get(sk, 0) >= v:
                continue
            self.waited[eng][sk] = v
            self.ops[eng].append(('wait', sk, v))
            self.nwaits += 1
            if sk == ('e', 'pe'):
                pe_wait = True
        if pe_wait and self.pe_delay and eng in self.scratch:
            sc = self.scratch[eng]
            for _ in range(PE_DELAY_OPS[eng]):
                if eng == 'act':
                    self.ops[eng].append(('raw', lambda e: e.copy(sc[:], sc[:])))
                else:
                    self.ops[eng].append(('raw', lambda e: e.memset(sc[:], 0.0)))

    def op(self, eng, fn, reads=(), writes=()):
        fl = self.flush and eng in ('act', 'dve', 'pool') and eng in self.scratch
        token = (('e', eng), self.cnt[eng] + (2 if fl else 1))
        deps = self._deps(list(reads), list(writes), token)
        self._emit_waits(eng, deps)
        self.cnt[eng] += 1
        self.ops[eng].append(('op', fn, token[0]))
        if fl:
            sc = self.scratch[eng]
            self.cnt[eng] += 1
            if eng == 'act':
                self.ops[eng].append(('op', lambda e: e.copy(sc[:], sc[:]), token[0]))
            else:
                self.ops[eng].append(('op', lambda e: e.memset(sc[:], 0.0), token[0]))

    def dma(self, q, out, in_, **kw):
        i = self.dnext[q] % NDSEM
        self.dnext[q] += 1
        sk = ('d', q, i)
        prev = self.dcnt[(q, i)]
        self.dcnt[(q, i)] = prev + 16
        token = (sk, prev + 16)
        deps = self._deps([in_], [out], token)
        if prev > 0:
            if deps.get(sk, 0) < prev:
                deps[sk] = prev
        self._emit_waits(q, deps)
        self.ops[q].append(('dma', (out, in_, kw), sk))

    def dma_custom(self, q, out, in_, fn, extra_reads=()):
        i = self.dnext[q] % NDSEM
        self.dnext[q] += 1
        sk = ('d', q, i)
        prev = self.dcnt[(q, i)]
        self.dcnt[(q, i)] = prev + 16
        token = (sk, prev + 16)
        deps = self._deps([in_] + list(extra_reads), [out], token)
        if prev > 0:
            if deps.get(sk, 0) < prev:
                deps[sk] = prev
        self._emit_waits(q, deps)
        self.ops[q].append(('dmac', (out, in_, fn), sk))

    def barrier(self):
        for e in ENGS:
            deps = {}
            for e2 in ENGS:
                if e2 != e and self.cnt[e2] > 0:
                    deps[('e', e2)] = self.cnt[e2]
            if e != 'pe' and self.cnt[e] > 0:
                deps[('e', e)] = self.cnt[e]
            for (q, i), v in self.dcnt.items():
                if v > 0:
                    deps[('d', q, i)] = v
            self._emit_waits(e, deps)
        self.wr = {}
        self.rd = {}

    def emit(self):
        nc = self.nc
        sems = self.sems
        ops = self.ops

        def run(eng_name, eng):
            for item in ops[eng_name]:
                if item[0] == 'wait':
                    eng.wait_ge(sems[item[1]], item[2])
                elif item[0] == 'op':
                    ins = item[1](eng)
                    ins.then_inc(sems[item[2]], 1)
                elif item[0] == 'raw':
                    item[1](eng)
                elif item[0] == 'dmac':
                    out, in_, fn = item[1]
                    fn(eng, out, in_, {}).then_inc(sems[item[2]], 16)
                else:
                    out, in_, kw = item[1]
                    eng.dma_start(out=out, in_=in_, **kw).then_inc(sems[item[2]], 16)

        with nc.Block() as block:
            @block.tensor
            def _(e):
                run('pe', e)

            @block.scalar
            def _(e):
                run('act', e)

            @block.vector
            def _(e):
                run('dve', e)

            @block.gpsimd
            def _(e):
                run('pool', e)

            @block.sync
            def _(e):
                run('sp', e)

from contextlib import ExitStack
import os
MOE_CUT = int(os.environ.get('MOE_CUT', '99'))
MOE_E0 = int(os.environ.get('MOE_E0', '0'))
MOE_E1 = int(os.environ.get('MOE_E1', '16'))
from concourse.bass_utils import run_bass_kernel_spmd

D = 1024
KC = 8
NL = 2048
NCX = 256
T = NL + NCX
DEPTH = 4
NE = 16
CAPL = 256
CAPC = 32
EPS = 1e-6
QSCALE = 128 ** -0.5
TILES = [(0, 256, 1), (256, 512, 0), (768, 512, 0), (1280, 512, 0), (1792, 512, 0)]


class B:
    def __init__(self, nc, P, st):
        self.nc = nc
        self.P = P
        self.st = st
        self.psn = 0
        self.rr = 0

    def mm(self, out, lhsT, rhs, start=True, stop=True):
        self.P.op('pe', lambda e: e.matmul(out, lhsT, rhs, start=start, stop=stop),
                  reads=[lhsT, rhs], writes=[out])

    def tr(self, out, in_, ident):
        self.mm(out, in_, ident)

    def act(self, out, in_, func, bias=None, scale=None):
        kw = {}
        rd = [in_]
        if bias is not None:
            kw['bias'] = bias
            if not isinstance(bias, float):
                rd.append(bias)
        if scale is not None:
            kw['scale'] = scale
            if not isinstance(scale, float):
                rd.append(scale)
        self.P.op('act', lambda e: e.activation(out, in_, func, **kw), reads=rd, writes=[out])

    def tt(self, eng, out, in0, in1, op):
        self.P.op(eng, lambda e: e.tensor_tensor(out, in0, in1, op), reads=[in0, in1], writes=[out])

    def ts(self, eng, out, in0, s1, s2, op0, op1=None):
        rd = [in0]
        if not isinstance(s1, float):
            rd.append(s1)
        if s2 is not None and not isinstance(s2, float):
            rd.append(s2)
        if op1 is None:
            self.P.op(eng, lambda e: e.tensor_scalar(out, in0, s1, None, op0), reads=rd, writes=[out])
        else:
            self.P.op(eng, lambda e: e.tensor_scalar(out, in0, s1, s2, op0, op1), reads=rd, writes=[out])

    def stt(self, eng, out, in0, scalar, in1, op0, op1):
        rd = [in0, in1]
        if not isinstance(scalar, float):
            rd.append(scalar)
        self.P.op(eng, lambda e: e.scalar_tensor_tensor(out, in0, scalar, in1, op0, op1), reads=rd, writes=[out])

    def cp(self, eng, out, in_):
        if eng == 'act':
            self.P.op('act', lambda e: e.copy(out, in_), reads=[in_], writes=[out])
        else:
            self.P.op(eng, lambda e: e.tensor_copy(out, in_), reads=[in_], writes=[out])

    def memset(self, eng, out, val):
        self.P.op(eng, lambda e: e.memset(out, val), reads=[], writes=[out])

    def scan(self, out, a, b, init):
        rd = [a, b]
        if not isinstance(init, float):
            rd.append(init)
        self.P.op('dve', lambda e: e.tensor_tensor_scan(out, a, b, init, ALU.mult, ALU.add), reads=rd, writes=[out])

    def dma(self, q, out, in_):
        self.P.dma(q, out, in_)

    def ps(self):
        b = self.banks[self.psn % len(self.banks)]
        self.psn += 1
        return b

    def alt(self, engs=('act', 'dve')):
        self.rr += 1
        return engs[self.rr % len(engs)]


def build_program(stop_after=None, n_layers=DEPTH):
    nc = bass.Bass("TRN2", target_bir_lowering=False)
    dram = {}

    def din(name, shape, dt=F32):
        dram[name] = nc.dram_tensor(name, list(shape), dt, kind="ExternalInput").ap()
        return dram[name]

    xT_d = din("xT", [D, T])
    cc_d = din("cc", [128, KC, 2])
    ada_w_d = din("ada_w", [DEPTH, D, 6 * D])
    ada_b_d = din("ada_b", [128, DEPTH, 48])
    n1g_d = din("n1g", [128, DEPTH, KC])
    n2g_d = din("n2g", [128, DEPTH, KC])
    fing_d = din("fing", [128, KC])
    lwin_d = din("lru_w_in", [2, D, 2 * D])
    lwout_d = din("lru_w_out", [2, D, D])
    convw_d = din("conv_w", [128, 2, KC, 4])
    convb_d = din("conv_b", [128, 2, KC])
    gatew_d = din("gate_w", [2, 2, 8, 128, 256])
    gateb_d = din("gate_b", [128, 2, 2, 8, 2])
    lam_d = din("lam", [128, 2, 2, 8])
    wqkv_d = din("w_qkv", [2, D, 1536])
    wqsw_d = din("w_qkv_sw", [2, D, 1280])
    sink_d = din("sink", [128, 2, 8])
    wo_d = din("w_o", [2, D, D])
    router_d = din("router", [128, DEPTH, KC, NE])
    wg_d = din("moe_w_gate", [DEPTH, NE, D, D])
    wu_d = din("moe_w_up", [DEPTH, NE, D, D])
    wd_d = din("moe_w_down", [DEPTH, NE, D, D])
    ident_d = din("ident", [128, 128])
    ropec_d = din("rope_c", [128, NL])
    ropes_d = din("rope_s", [128, NL])
    iota_d = din("iota_row", [128, NL])
    tokidx_d = din("tokidx", [128, 16])
    mprev_d = din("mask_prev", [128, 128])
    mnext_d = din("mask_next", [128, 128])
    pidx_d = din("pidx16", [16, 128])
    halfoff_d = din("halfoff", [32, 1])
    out_d = nc.dram_tensor("outT", [D, NL], F32, kind="ExternalOutput").ap()
    scr_d = nc.dram_tensor("scr_tabs", [DEPTH, 2, 16, 288], F32).ap()
    scr2_d = nc.dram_tensor("scr_half", [DEPTH, 2, 32, CAPL], F32).ap()
    hscr_d = [nc.dram_tensor(f"scr_htok{i}", [T, D], BF16).ap() for i in range(DEPTH)]
    dbg_d = None
    if stop_after is not None:
        dbg_d = nc.dram_tensor("dbgx", [D, T], F32, kind="ExternalOutput").ap()
        dbg_aff = nc.dram_tensor("dbg_aff", [16, T], F32, kind="ExternalOutput").ap()
        dbg_idx = nc.dram_tensor("dbg_idx", [16, 288], F32, kind="ExternalOutput").ap()
        dbg_val = nc.dram_tensor("dbg_val", [16, 288], F32, kind="ExternalOutput").ap()
        dbg_it = nc.dram_tensor("dbg_it", [128, 3, NE], F32, kind="ExternalOutput").ap()
        dbg_vt = nc.dram_tensor("dbg_vt", [128, 3, NE], F32, kind="ExternalOutput").ap()

    with ExitStack() as st:
        P = Prog(nc, st)
        bld = B(nc, P, st)
        mm, act, tt, ts, stt, cp, dma = bld.mm, bld.act, bld.tt, bld.ts, bld.stt, bld.cp, bld.dma

        def SB(stack, name, shape, dt):
            return stack.enter_context(nc.sbuf_tensor(name, list(shape), dt))

        banks = [st.enter_context(nc.psum_tensor(f"ps{i}", [128, 512], F32)) for i in range(7)]
        psb = st.enter_context(nc.psum_tensor("psb", [128, 1024], BF16))
        bld.banks = banks
        for en in ('act', 'dve', 'pool'):
            P.scratch[en] = SB(st, f"flush_{en}", [128, 1], F32)
        P.flush = os.environ.get('FLUSH', '0') == '1'
        P.pe_delay = os.environ.get('PE_DELAY', '1') == '1'

        xT = SB(st, "xT_sb", [128, KC, T], F32)
        ident_f = SB(st, "ident_f", [128, 128], F32)
        ident_b = SB(st, "ident_b", [128, 128], BF16)
        ones_b = SB(st, "ones_b", [128, 128], BF16)
        mod = SB(st, "mod", [128, DEPTH, 48, 2], F32)
        gp1 = SB(st, "gp1", [128, DEPTH, KC, 2], F32)
        gp2 = SB(st, "gp2", [128, DEPTH, KC, 2], F32)
        n1g = SB(st, "n1g_sb", [128, DEPTH, KC], F32)
        n2g = SB(st, "n2g_sb", [128, DEPTH, KC], F32)
        fing = SB(st, "fing_sb", [128, KC], F32)
        ada_b = SB(st, "ada_b_sb", [128, DEPTH, 48], F32)
        convw = SB(st, "convw_sb", [128, 2, KC, 4], F32)
        convb = SB(st, "convb_sb", [128, 2, KC], F32)
        gateb = SB(st, "gateb_sb", [128, 2, 2, 8, 2], F32)
        gatebh = SB(st, "gatebh_sb", [128, 2, 2, 8, 2], F32)
        lam = SB(st, "lam_sb", [128, 2, 2, 8], F32)
        ls4 = SB(st, "ls4", [128, 2, 2, 8], F32)
        ls8 = SB(st, "ls8", [128, 2, 2, 8], F32)
        esink = SB(st, "esink", [128, 2, 8], F32)
        router_b = SB(st, "router_b", [128, DEPTH, KC, NE], BF16)
        tokidx = SB(st, "tokidx_sb", [128, 16], F32)
        ntok = SB(st, "ntok_sb", [128, 16], F32)
        pidx16 = SB(st, "pidx16_sb", [16, 128], F32)
        halfoff = SB(st, "halfoff_sb", [32, 1], F32)
        scc = SB(st, "scc", [128, KC, 2], BF16)

        for k in range(KC):
            dma('sp', xT[:, k, :], xT_d[k * 128:(k + 1) * 128, :])
        rstk = ExitStack()
        router_f = SB(rstk, "router_f", [128, DEPTH, KC, NE], F32)
        for sb_t, d_t in [(ident_f, ident_d), (ada_b, ada_b_d), (n1g, n1g_d), (n2g, n2g_d), (fing, fing_d),
                          (convw, convw_d), (convb, convb_d), (gateb, gateb_d), (lam, lam_d), (esink, sink_d),
                          (router_f, router_d), (tokidx, tokidx_d), (pidx16, pidx_d), (halfoff, halfoff_d)]:
            dma('sp', sb_t[:], d_t)
        cp('dve', ident_b[:], ident_f[:])
        cp('dve', router_b[:], router_f[:])
        bld.memset('dve', ones_b[:], 1.0)
        act(esink[:], esink[:], AF.Exp)
        act(lam[:], lam[:], AF.Exp, scale=-1.0)
        ts('dve', lam[:], lam[:], 1.0, None, ALU.add)
        act(lam[:], lam[:], AF.Ln)
        ts('dve', ls4[:], lam[:], -4.0, None, ALU.mult)
        ts('dve', ls8[:], lam[:], -8.0, None, ALU.mult)
        ts('dve', gatebh[:], gateb[:], 0.5, None, ALU.mult)
        ts('dve', ntok[:], tokidx[:], -1.0, None, ALU.mult)
        P.barrier()
        rstk.close()

        def adaln_mm(l, bufs, W):
            psm = bld.ps()
            npc = (6 * D) // W
            for pc in range(npc):
                wb = bufs[pc % 2]
                dma('pool', wb[:], ada_w_d[l, :, pc * W:(pc + 1) * W].rearrange("(c p) f -> p c f", p=128))
                for jj in range(W // 128):
                    j = pc * (W // 128) + jj
                    for k in range(KC):
                        mm(psm[:, 2 * j:2 * j + 2], wb[:, k, jj * 128:(jj + 1) * 128], scc[:, k, :],
                           start=(k == 0), stop=(k == KC - 1))
            return psm

        def adaln_fin(l, psm):
            tt('dve', mod[:, l], psm[:, 0:96].rearrange("p (j s) -> p j s", s=2),
               ada_b[:, l, :].unsqueeze(2).to_broadcast([128, 48, 2]), ALU.add)
            for (gp, ng, grp) in ((gp1, n1g, 1), (gp2, n2g, 4)):
                ts('dve', gp[:, l], mod[:, l, grp * 8:(grp + 1) * 8, :], 1.0, None, ALU.add)
                tt('dve', gp[:, l], gp[:, l], ng[:, l, :].unsqueeze(2).to_broadcast([128, KC, 2]), ALU.mult)

        with ExitStack() as ph:
            cc = SB(ph, "cc_sb", [128, KC, 2], F32)
            adw = [SB(ph, f"adw{i}", [128, KC, 1024], BF16) for i in range(2)]
            dma('sp', cc[:], cc_d)
            act(scc[:], cc[:], AF.Silu)
            adaln_fin(0, adaln_mm(0, adw, 1024))
            P.barrier()

        def mod_ap(l, grp, k, s):
            return mod[:, l, grp * 8 + k, s:s + 1]

        def norm_mod(ph, hT, l, which, tiles):
            gp = gp1 if which == 1 else gp2
            shg = 0 if which == 1 else 3
            sq = [SB(ph, f"nm_sq{i}_{l}_{which}", [128, 512], BF16) for i in range(4)]
            rs = [SB(ph, f"nm_rs{i}_{l}_{which}", [128, 512], F32) for i in range(2)]
            tmp = [SB(ph, f"nm_tmp{i}_{l}_{which}", [128, 512], F32) for i in range(4)]
            for ti, (t0, n, s) in enumerate(tiles):
                pss = bld.ps()
                for k in range(KC):
                    q = sq[k % 4]
                    if k % 2 == 0:
                        act(q[:, 0:n], xT[:, k, t0:t0 + n], AF.Square)
                    else:
                        tt('dve', q[:, 0:n], xT[:, k, t0:t0 + n], xT[:, k, t0:t0 + n], ALU.mult)
                    mm(pss[:, 0:n], ones_b[:], q[:, 0:n], start=(k == 0), stop=(k == KC - 1))
                r = rs[ti % 2]
                act(r[:, 0:n], pss[:, 0:n], AF.Ln, scale=1.0 / D, bias=EPS)
                act(r[:, 0:n], r[:, 0:n], AF.Exp, scale=-0.5)
                for k in range(KC):
                    tm = tmp[k % 4]
                    tt('dve', tm[:, 0:n], xT[:, k, t0:t0 + n], r[:, 0:n], ALU.mult)
                    act(hT[:, k, t0:t0 + n], tm[:, 0:n], AF.Identity,
                        scale=gp[:, l, k, s:s + 1], bias=mod_ap(l, shg, k, s))

        def out_proj(ph, name, w_dram, mT_, l, ggrp, tiles):
            wo = SB(ph, name, [128, KC, D], BF16)
            for h in range(2):
                dma('pool', wo[:, :, h * 512:(h + 1) * 512],
                    w_dram[:, h * 512:(h + 1) * 512].rearrange("(k p) f -> p k f", p=128))
            for (t0, n, s) in tiles:
                for dm in range(KC):
                    po = bld.ps()
                    for c in range(KC):
                        mm(po[:, 0:n], wo[:, c, dm * 128:(dm + 1) * 128], mT_[:, c, t0:t0 + n],
                           start=(c == 0), stop=(c == KC - 1))
                    stt('dve', xT[:, dm, t0:t0 + n], po[:, 0:n], mod_ap(l, ggrp, dm, s), xT[:, dm, t0:t0 + n],
                        ALU.mult, ALU.add)

        def lru_phase(l, need_ctx):
            j = l // 2
            with ExitStack() as ph:
                hT = SB(ph, f"hT_l{l}", [128, KC, T], BF16)
                mT = SB(ph, f"mT_l{l}", [128, KC, T], BF16)
                with ExitStack() as nt:
                    norm_mod(nt, hT, l, 1, TILES)
                    P.barrier()
                with ExitStack() as lt:
                    ub = SB(lt, f"ub{l}", [128, T], F32)
                    xb = SB(lt, f"xb{l}", [128, T], F32)
                    xbb = SB(lt, f"xbb{l}", [128, T], BF16)
                    winA = SB(lt, f"winA{l}", [128, KC, 128], BF16)
                    winB = SB(lt, f"winB{l}", [128, KC, 128], BF16)
                    gw = [SB(lt, f"gw{l}_{i}", [128, 256], BF16) for i in range(4)]
                    tr_ = [SB(lt, f"tr{l}_{i}", [128, 512], F32) for i in range(4)]
                    ti_ = [SB(lt, f"ti{l}_{i}", [128, 512], F32) for i in range(4)]
                    tb_ = [SB(lt, f"tb{l}_{i}", [128, 512], F32) for i in range(4)]
                    th_ = [SB(lt, f"th{l}_{i}", [128, 512], F32) for i in range(2)]
                    carry = SB(lt, f"carry{l}", [128, 2], F32)
                    for d in range(2):
                        dma('pool', gw[d][:], gatew_d[j, d, 0])
                    orders = [TILES, [TILES[0]] + TILES[:0:-1]]
                    first_dir = {0: 0, 256: 0, 768: 0, 1280: 1, 1792: 1}

                    def ldA(c):
                        dma('pool', winA[:], lwin_d[j, :, c * 128:(c + 1) * 128].rearrange("(k p) f -> p k f", p=128))

                    def ldB(c):
                        dma('pool', winB[:], lwin_d[j, :, D + c * 128:D + (c + 1) * 128].rearrange("(k p) f -> p k f", p=128))
                    ldB(0)
                    ldA(0)
                    for c in range(KC):
                        if c + 1 < KC:
                            for d in range(2):
                                dma('pool', gw[((c + 1) % 2) * 2 + d][:], gatew_d[j, d, c + 1])
                        for (t0, n, s) in TILES:
                            pu = bld.ps()
                            for k in range(KC):
                                mm(pu[:, 0:n], winB[:, k, :], hT[:, k, t0:t0 + n], start=(k == 0), stop=(k == KC - 1))
                            cp(bld.alt(), ub[:, t0:t0 + n], pu[:, 0:n])
                        if c + 1 < KC:
                            ldB(c + 1)
                        for (t0, n, s) in TILES:
                            s0, sn = (0, NCX) if s == 1 else (NCX, NL)
                            ts('dve', xb[:, t0:t0 + n], ub[:, t0:t0 + n], convw[:, j, c, 1:2], convb[:, j, c:c + 1], ALU.mult, ALU.add)
                            for (o, kk) in ((-1, 0), (1, 2), (2, 3)):
                                lo = max(t0, s0 - o)
                                hi = min(t0 + n, s0 + sn - o)
                                stt('dve', xb[:, lo:hi], ub[:, lo + o:hi + o], convw[:, j, c, kk:kk + 1], xb[:, lo:hi], ALU.mult, ALU.add)
                            cp('pool', xbb[:, t0:t0 + n], xb[:, t0:t0 + n])
                        for (ga, gb_) in ((0, 1), (1, 3), (3, 5)):
                            for d in range(2):
                                g = gw[(c % 2) * 2 + d]
                                for gi, (t0, n, s) in enumerate(orders[d][ga:gb_]):
                                    slot = d * 2 + gi
                                    pr = bld.ps()
                                    pi = bld.ps()
                                    mm(pr[:, 0:n], g[:, 0:128], xbb[:, t0:t0 + n])
                                    mm(pi[:, 0:n], g[:, 128:256], xbb[:, t0:t0 + n])
                                    r_ = tr_[slot]
                                    i_ = ti_[slot]
                                    b_ = tb_[slot]
                                    act(r_[:, 0:n], pr[:, 0:n], AF.Tanh, scale=0.5, bias=gatebh[:, j, d, c, 0:1])
                                    act(i_[:, 0:n], pi[:, 0:n], AF.Tanh, scale=0.5, bias=gatebh[:, j, d, c, 1:2])
                                    act(b_[:, 0:n], r_[:, 0:n], AF.Exp, scale=ls8[:, j, d, c:c + 1], bias=ls8[:, j, d, c:c + 1])
                                    act(r_[:, 0:n], r_[:, 0:n], AF.Exp, scale=ls4[:, j, d, c:c + 1], bias=ls4[:, j, d, c:c + 1])
                                    ts('dve', b_[:, 0:n], b_[:, 0:n], 1.0, None, ALU.min)
                            for d in range(2):
                                for gi, (t0, n, s) in enumerate(orders[d][ga:gb_]):
                                    slot = d * 2 + gi
                                    r_ = tr_[slot]
                                    i_ = ti_[slot]
                                    b_ = tb_[slot]
                                    act(b_[:, 0:n], b_[:, 0:n], AF.Sqrt, scale=-0.25, bias=0.25)
                                    stt('dve', i_[:, 0:n], i_[:, 0:n], 1.0, xb[:, t0:t0 + n], ALU.add, ALU.mult)
                                    tt('dve', b_[:, 0:n], b_[:, 0:n], i_[:, 0:n], ALU.mult)
                                    init = 0.0 if ga == 0 else carry[:, d:d + 1]
                                    first = first_dir[t0] == d
                                    dst = ub[:, t0:t0 + n] if first else th_[gi][:, 0:n]
                                    if d == 0:
                                        bld.scan(dst, r_[:, 0:n], b_[:, 0:n], init)
                                        cp('dve', carry[:, 0:1], dst[:, n - 1:n])
                                    else:
                                        bld.scan(dst[:, ::-1], r_[:, n - 1::-1], b_[:, n - 1::-1], init)
                                        cp('dve', carry[:, 1:2], dst[:, 0:1])
                                    if not first:
                                        tt('pool', ub[:, t0:t0 + n], ub[:, t0:t0 + n], dst, ALU.add)
                        for ti, (t0, n, s) in enumerate(TILES):
                            if s == 1 and not need_ctx:
                                continue
                            pg = bld.ps()
                            for k in range(KC):
                                mm(pg[:, 0:n], winA[:, k, :], hT[:, k, t0:t0 + n], start=(k == 0), stop=(k == KC - 1))
                            y_ = th_[ti % 2]
                            act(y_[:, 0:n], pg[:, 0:n], AF.Gelu_apprx_tanh)
                            tt('dve', mT[:, c, t0:t0 + n], y_[:, 0:n], ub[:, t0:t0 + n], ALU.mult)
                        if c + 1 < KC:
                            ldA(c + 1)
                    P.barrier()
                with ExitStack() as ot:
                    tiles = TILES if need_ctx else TILES[1:]
                    out_proj(ot, f"lwo{l}", lwout_d[j], mT, l, 2, tiles)
                    P.barrier()

        def attn_phase(l, need_ctx):
            j = l // 2
            with ExitStack() as ph:
                hT = SB(ph, f"hT_l{l}", [128, KC, T], BF16)
                qT = SB(ph, f"qT_l{l}", [128, 8, T], BF16)
                kT = SB(ph, f"kT_l{l}", [128, 2, T], BF16)
                V = SB(ph, f"V_l{l}", [128, 18, 256], BF16)
                with ExitStack() as nt:
                    norm_mod(nt, hT, l, 1, TILES)
                    P.barrier()
                with ExitStack() as qt:
                    rc = SB(qt, f"rc{l}", [128, NL], F32)
                    rs_ = SB(qt, f"rs{l}", [128, NL], F32)
                    dma('sp', rc[:], ropec_d)
                    dma('sp', rs_[:], ropes_d)
                    wq = [SB(qt, f"wq{l}_{i}", [128, KC, 128], BF16) for i in range(2)]
                    ws = [SB(qt, f"ws{l}_{i}", [128, KC, 128], BF16) for i in range(2)]
                    wv = SB(qt, f"wv{l}", [128, KC, 256], BF16)
                    t1 = [SB(qt, f"t1{l}_{i}", [128, 512], F32) for i in range(2)]
                    t2 = [SB(qt, f"t2{l}_{i}", [128, 512], F32) for i in range(2)]
                    for hh in range(10):
                        a = wq[hh % 2]
                        b = ws[hh % 2]
                        dma('pool', a[:], wqkv_d[j, :, hh * 128:(hh + 1) * 128].rearrange("(k p) f -> p k f", p=128))
                        dma('pool', b[:], wqsw_d[j, :, hh * 128:(hh + 1) * 128].rearrange("(k p) f -> p k f", p=128))
                        dst = qT[:, hh, :] if hh < 8 else kT[:, hh - 8, :]
                        for ti, (t0, n, s) in enumerate(TILES):
                            if s == 1 and hh < 8 and not need_ctx:
                                continue
                            p1 = bld.ps()
                            for k in range(KC):
                                mm(p1[:, 0:n], a[:, k, :], hT[:, k, t0:t0 + n], start=(k == 0), stop=(k == KC - 1))
                            if s == 1:
                                cp('act', dst[:, t0:t0 + n], p1[:, 0:n])
                                continue
                            p2 = bld.ps()
                            for k in range(KC):
                                mm(p2[:, 0:n], b[:, k, :], hT[:, k, t0:t0 + n], start=(k == 0), stop=(k == KC - 1))
                            a1 = t1[ti % 2]
                            a2 = t2[ti % 2]
                            tt('dve', a1[:, 0:n], p1[:, 0:n], rc[:, t0 - NCX:t0 - NCX + n], ALU.mult)
                            tt('dve', a2[:, 0:n], p2[:, 0:n], rs_[:, t0 - NCX:t0 - NCX + n], ALU.mult)
                            tt('pool', dst[:, t0:t0 + n], a1[:, 0:n], a2[:, 0:n], ALU.add)
                    dma('pool', wv[:], wqkv_d[j, :, 1280:1536].rearrange("(k p) f -> p k f", p=128))
                    for blk in range(18):
                        pv = bld.ps()
                        for k in range(KC):
                            mm(pv[:, 0:256], hT[:, k, blk * 128:(blk + 1) * 128], wv[:, k, :], start=(k == 0), stop=(k == KC - 1))
                        cp(bld.alt(), V[:, blk, :], pv[:, 0:256])
                    P.barrier()
                with ExitStack() as at:
                    oT = hT
                    mprev = SB(at, f"mprev{l}", [128, 128], BF16)
                    mnext = SB(at, f"mnext{l}", [128, 128], BF16)
                    dma('pool', mprev[:], mprev_d)
                    dma('pool', mnext[:], mnext_d)
                    ex = [SB(at, f"ex{l}_{i}", [128, 512], BF16) for i in range(6)]
                    dn = [SB(at, f"dn{l}_{i}", [128, 512], F32) for i in range(2)]
                    exc = 0
                    qblocks = [(b, True) for b in range(2, 18)]
                    if need_ctx:
                        qblocks += [(0, False), (1, False)]
                    for qi, (qb, is_lat) in enumerate(qblocks):
                        q0 = qb * 128
                        for kvh in range(2):
                            keys = [(0, None), (1, None)]
                            if is_lat:
                                if qb > 2:
                                    keys.append((qb - 1, mprev))
                                keys.append((qb, None))
                                if qb < 17:
                                    keys.append((qb + 1, mnext))
                            qsl = qT[:, kvh * 4:(kvh + 1) * 4, q0:q0 + 128]
                            po = bld.ps()
                            pd = bld.ps()
                            es = []
                            for (kb, msk) in keys:
                                psc = bld.ps()
                                mm(psc[:, :].rearrange("p (g q) -> p g q", g=4), kT[:, kvh, kb * 128:(kb + 1) * 128], qsl)
                                e_ = ex[exc % 6]
                                exc += 1
                                act(e_[:], psc[:], AF.Exp, scale=QSCALE)
                                if msk is not None:
                                    e3 = e_[:, :].rearrange("p (g q) -> p g q", g=4)
                                    tt('pool', e3, e3, msk[:, :].unsqueeze(1).to_broadcast([128, 4, 128]), ALU.mult)
                                es.append((kb, e_))
                            for i, (kb, e_) in enumerate(es):
                                mm(po[:], V[:, kb, kvh * 128:(kvh + 1) * 128], e_[:], start=(i == 0), stop=(i == len(es) - 1))
                            for i, (kb, e_) in enumerate(es):
                                mm(pd[:], ones_b[:], e_[:], start=(i == 0), stop=(i == len(es) - 1))
                            d_ = dn[(qi * 2 + kvh) % 2]
                            tt('dve', d_[:, :].rearrange("p (g q) -> p g q", g=4), pd[:, :].rearrange("p (g q) -> p g q", g=4),
                               esink[:, j, kvh * 4:(kvh + 1) * 4].unsqueeze(2).to_broadcast([128, 4, 128]), ALU.add)
                            P.op('dve', (lambda dd: lambda e: e.reciprocal(dd[:], dd[:]))(d_), reads=[d_[:]], writes=[d_[:]])
                            tt('dve', oT[:, kvh * 4:(kvh + 1) * 4, q0:q0 + 128], po[:, :].rearrange("p (g q) -> p g q", g=4),
                               d_[:, :].rearrange("p (g q) -> p g q", g=4), ALU.mult)
                    P.barrier()
                with ExitStack() as ot:
                    tiles = TILES if need_ctx else TILES[1:]
                    out_proj(ot, f"awo{l}", wo_d[j], oT, l, 2, tiles)
                    P.barrier()

        def moe_phase(l, need_ctx):
            tiles = TILES if need_ctx else TILES[1:]
            CT = CAPL + CAPC if need_ctx else CAPL
            cbs = [(0, 128, 0), (1, 128, 128)] + ([(2, 32, 256)] if need_ctx else [])
            with ExitStack() as ph:
                idx_i = SB(ph, f"idxi{l}", [128, 3, NE], I32)
                val_tok = SB(ph, f"valtok{l}", [128, 3, NE], F32)
                nbias = SB(ph, f"nbias{l}", [128, 3, NE, 4], F32)
                iota_row = SB(ph, f"iota{l}", [128, 512], F32)
                dma('sp', iota_row[:], iota_d[:, 0:512])
                NW = 6
                wring = [SB(ph, f"wr{l}_{i}", [128, KC, 512], BF16) for i in range(NW)]
                pieces = []
                for e in range(NE):
                    for fh in range(2):
                        pieces.append(wg_d[l, e, :, fh * 512:(fh + 1) * 512])
                        pieces.append(wu_d[l, e, :, fh * 512:(fh + 1) * 512])
                    for dh in range(2):
                        pieces.append(wd_d[l, e, :, dh * 512:(dh + 1) * 512])
                issued = [6 * MOE_E0]

                def ensure(i):
                    while issued[0] < min(len(pieces), i + NW):
                        pi_ = issued[0]
                        dma('pool', wring[pi_ % NW][:], pieces[pi_].rearrange("(k p) f -> p k f", p=128))
                        issued[0] += 1

                ensure(6 * MOE_E0)
                with ExitStack() as rt:
                    hT = SB(rt, f"hTm{l}", [128, KC, T], BF16)
                    htok = [SB(rt, f"htok{l}_{i}", [128, D], BF16) for i in range(3)]
                    with ExitStack() as nt:
                        norm_mod(nt, hT, l, 2, tiles)
                        P.barrier()
                    aff = SB(rt, f"aff{l}", [128, 18, NE], F32)
                    mx = SB(rt, f"mx{l}", [128, 18], F32)
                    affC = SB(rt, f"affC{l}", [16, NCX], F32)
                    affL = SB(rt, f"affL{l}", [32, 1024], F32)
                    valsL = SB(rt, f"valsL{l}", [32, CAPL], F32)
                    idxL_u = SB(rt, f"idxLu{l}", [32, CAPL], U32)
                    idxL_f = SB(rt, f"idxLf{l}", [32, CAPL], F32)
                    Bv = SB(rt, f"Bv{l}", [16, CAPL], F32)
                    Bi = SB(rt, f"Bi{l}", [16, CAPL], F32)
                    sel = SB(rt, f"sel{l}", [16, CAPL], F32)
                    dd = SB(rt, f"dd{l}", [16, CAPL], F32)
                    vals = SB(rt, f"vals{l}", [16, 288], F32)
                    idx_u = SB(rt, f"idxu{l}", [16, 288], U32)
                    idx_f = SB(rt, f"idxf{l}", [16, 288], F32)
                    idxT = SB(rt, f"idxT{l}", [128, 3, NE], F32)
                    blks = list(range(18)) if need_ctx else list(range(2, 18))
                    pl = bld.ps()

                    def posb(b):
                        if b < 2:
                            return b
                        lb = b - 2
                        return 2 + (lb % 8) * 2 + lb // 8
                    for b in blks:
                        pb_ = posb(b)
                        for k in range(KC):
                            mm(pl[:, pb_ * NE:(pb_ + 1) * NE], hT[:, k, b * 128:(b + 1) * 128], router_b[:, l, k, :],
                               start=(k == 0), stop=(k == KC - 1))
                    b0, nb = blks[0], len(blks)
                    pl3 = pl[:, b0 * NE:(b0 + nb) * NE].rearrange("p (b e) -> p b e", e=NE)
                    a3 = aff[:, b0:b0 + nb, :]
                    P.op('dve', lambda e: e.tensor_reduce(mx[:, b0:b0 + nb], pl3, AX.X, ALU.max), reads=[pl3], writes=[mx[:, b0:b0 + nb]])
                    tt('dve', a3, pl3, mx[:, b0:b0 + nb].unsqueeze(2).to_broadcast([128, nb, NE]), ALU.subtract)
                    act(a3, a3, AF.Exp)
                    P.op('dve', lambda e: e.tensor_reduce(mx[:, b0:b0 + nb], a3, AX.X, ALU.add), reads=[a3], writes=[mx[:, b0:b0 + nb]])
                    P.op('dve', lambda e: e.reciprocal(mx[:, b0:b0 + nb], mx[:, b0:b0 + nb]), reads=[mx[:, b0:b0 + nb]], writes=[mx[:, b0:b0 + nb]])
                    tt('dve', a3, a3, mx[:, b0:b0 + nb].unsqueeze(2).to_broadcast([128, nb, NE]), ALU.mult)
                    if need_ctx:
                        pt = bld.ps()
                        for b in range(2):
                            bld.tr(pt[0:16, b * 128:(b + 1) * 128], aff[:, b, :], ident_f[:])
                        cp('act', affC[:, 0:NCX], pt[0:16, 0:NCX])
                    for g in range(2):
                        pt = bld.ps()
                        for bb in range(4):
                            b = g * 4 + bb
                            bld.tr(pt[0:32, bb * 128:(bb + 1) * 128], aff[:, 2 + 2 * b:4 + 2 * b, :].rearrange("p a e -> p (a e)"), ident_f[:])
                        cp('act', affL[:, g * 512:(g + 1) * 512], pt[0:32, :])
                    for b in blks:
                        for half in range(2):
                            pt = bld.ps()
                            for kk in range(4):
                                k = half * 4 + kk
                                bld.tr(pt[:, kk * 128:(kk + 1) * 128], hT[:, k, b * 128:(b + 1) * 128], ident_b[:])
                            cp('act', htok[b % 3][:, half * 512:(half + 1) * 512], pt[:])
                        dma('sp', hscr_d[l][b * 128:(b + 1) * 128, :], htok[b % 3][:])
                    ada_psm = None
                    if l + 1 < n_layers:
                        adw2 = [SB(rt, f"adw2_{l}_{i}", [128, KC, 512], BF16) for i in range(2)]
                        ada_psm = adaln_mm(l + 1, adw2, 512)
                    def topk(w, vout, iout, cap):
                        for it in range(cap // 8):
                            v8 = vout[:, it * 8:it * 8 + 8]
                            i8 = iout[:, it * 8:it * 8 + 8]
                            P.op('dve', (lambda v8, w: lambda e: e.max(out=v8, in_=w))(v8, w), reads=[w], writes=[v8])
                            P.op('dve', (lambda v8, i8, w: lambda e: e.max_index(out=i8, in_max=v8, in_values=w))(v8, i8, w), reads=[w, v8], writes=[i8])
                            P.op('dve', (lambda v8, w: lambda e: e.match_replace(out=w, in_to_replace=v8, in_values=w, imm_value=0.0))(v8, w), reads=[w, v8], writes=[w])
                    topk(affL[:], valsL[:], idxL_u[:], CAPL)
                    if need_ctx:
                        topk(affC[:], vals[:, CAPL:CAPL + CAPC], idx_u[:, CAPL:CAPL + CAPC], CAPC)
                        cp('dve', idx_f[:, CAPL:CAPL + CAPC], idx_u[:, CAPL:CAPL + CAPC])
                    cp('dve', idxL_f[:], idxL_u[:])
                    ts('dve', idxL_f[:], idxL_f[:], halfoff[:, 0:1], None, ALU.add)
                    dma('sp', scr2_d[l, 0], valsL[:])
                    dma('sp', scr2_d[l, 1], idxL_f[:])
                    dma('sp', Bv[:], scr2_d[l, 0, 16:32, :])
                    dma('sp', Bi[:], scr2_d[l, 1, 16:32, :])
                    tt('dve', sel[:], valsL[0:16, :], Bv[:, ::-1], ALU.is_gt)
                    for (A_, B_, out_) in ((valsL[0:16, :], Bv, vals[:, 0:CAPL]), (idxL_f[0:16, :], Bi, idx_f[:, 0:CAPL])):
                        tt('dve', dd[:], A_, B_[:, ::-1], ALU.subtract)
                        tt('dve', dd[:], dd[:], sel[:], ALU.mult)
                        tt('dve', out_, dd[:], B_[:, ::-1], ALU.add)
                    if not need_ctx:
                        bld.memset('dve', idx_f[:, CAPL:288], 0.0)
                        bld.memset('dve', vals[:, CAPL:288], 0.0)
                    dma('sp', scr_d[l, 0], idx_f[:])
                    dma('sp', scr_d[l, 1], vals[:])
                    for (cb, m, c0) in cbs:
                        P.dma('sp', idxT[0:m, cb, :], scr_d[l, 0, :, c0:c0 + m].rearrange("e c -> c e"), allow_slow_non_contiguous=True)
                        P.dma('sp', val_tok[0:m, cb, :], scr_d[l, 1, :, c0:c0 + m].rearrange("e c -> c e"), allow_slow_non_contiguous=True)
                    for (cb, m, c0) in cbs:
                        if cb < 2:
                            ts('dve', nbias[0:m, cb, :, 0], idxT[0:m, cb, :], float(NCX), None, ALU.add)
                            cp('dve', idx_i[0:m, cb, :], nbias[0:m, cb, :, 0])
                            for q4 in range(4):
                                ts('dve', nbias[0:m, cb, :, q4], idxT[0:m, cb, :], -1.0, float(q4 * 512), ALU.mult, ALU.add)
                        else:
                            cp('dve', idx_i[0:m, cb, :], idxT[0:m, cb, :])
                            ts('dve', nbias[0:m, cb, :, 0], idxT[0:m, cb, :], -1.0, None, ALU.mult)
                    if ada_psm is not None:
                        adaln_fin(l + 1, ada_psm)
                    P.barrier()
                with ExitStack() as et:
                    xgt = [SB(et, f"xgt{l}_{i}", [128, 3, D], BF16) for i in range(2)]
                    dtmp = [SB(et, f"dtmp{l}_{i}", [128, 512], F32) for i in range(3)]
                    xg = SB(et, f"xg{l}", [128, KC, 288], BF16)
                    sa = [SB(et, f"sa{l}_{i}", [128, 288], F32) for i in range(2)]
                    actT = SB(et, f"actT{l}", [128, KC, 288], BF16)
                    ye = [SB(et, f"ye{l}_{i}", [128, 3, D], BF16) for i in range(2)]
                    ST_lat = [SB(et, f"STl{l}_{i}", [128, 2, NL], BF16) for i in range(2)]
                    ST_ctx = [SB(et, f"STc{l}_{i}", [32, NCX], BF16) for i in range(2)]

                    def wget(i, base):
                        ensure(base)
                        assert i < issued[0]
                        return wring[i % NW]

                    def gather(e):
                        buf = xgt[e % 2]
                        for (cb, m, c0) in cbs:
                            def mk(cb, m, e):
                                return lambda en, out, in_, kw: en.indirect_dma_start(
                                    out=out, out_offset=None, in_=in_,
                                    in_offset=bass.IndirectOffsetOnAxis(ap=idx_i[0:m, cb, e:e + 1], axis=0))
                            P.dma_custom('pool', buf[0:m, cb, :], hscr_d[l][:, :], mk(cb, m, e), extra_reads=[idx_i[0:m, cb, e:e + 1]])

                    gather(MOE_E0)
                    for e in range(MOE_E0, MOE_E1):
                        par = e % 2
                        if e + 1 < MOE_E1:
                            gather(e + 1)
                        ensure(6 * e)
                        st_jobs = []
                        for cb in range(2):
                            for q4 in range(4):
                                def job(cb=cb, q4=q4, e=e, par=par):
                                    dt_ = dtmp[(cb * 4 + q4) % 3]
                                    act(dt_[:], iota_row[:], AF.Abs, bias=nbias[:, cb, e, q4:q4 + 1])
                                    act(ST_lat[par][:, cb, q4 * 512:(q4 + 1) * 512], dt_[:], AF.Relu, scale=-1.0, bias=1.0)
                                st_jobs.append(job)
                        if need_ctx:
                            def jobc(e=e, par=par):
                                dt_ = dtmp[2]
                                act(dt_[0:32, 0:NCX], iota_row[0:32, 0:NCX], AF.Abs, bias=nbias[0:32, 2, e, 0:1])
                                act(ST_ctx[par][:], dt_[0:32, 0:NCX], AF.Relu, scale=-1.0, bias=1.0)
                            st_jobs.append(jobc)
                        buf = xgt[par]
                        for k in range(KC):
                            pg = bld.ps()
                            for (cb, m, c0) in cbs:
                                mm(pg[:, c0:c0 + m], buf[0:m, cb, k * 128:(k + 1) * 128], ident_b[0:m, 0:m])
                            cp(bld.alt(), xg[:, k, 0:CT], pg[:, 0:CT])
                        for fh in range(2):
                            wgb = wget(6 * e + 2 * fh, 6 * e + 2 * fh)
                            wub = wget(6 * e + 2 * fh + 1, 6 * e + 2 * fh)
                            for ff in range(4):
                                f = fh * 4 + ff
                                pa = bld.ps()
                                pu = bld.ps()
                                for k in range(KC):
                                    mm(pa[:, 0:CT], wgb[:, k, ff * 128:(ff + 1) * 128], xg[:, k, 0:CT], start=(k == 0), stop=(k == KC - 1))
                                for k in range(KC):
                                    mm(pu[:, 0:CT], wub[:, k, ff * 128:(ff + 1) * 128], xg[:, k, 0:CT], start=(k == 0), stop=(k == KC - 1))
                                s_ = sa[f % 2]
                                act(s_[:, 0:CT], pa[:, 0:CT], AF.Silu)
                                tt('dve', actT[:, f, 0:CT], s_[:, 0:CT], pu[:, 0:CT], ALU.mult)
                                if st_jobs:
                                    st_jobs.pop(0)()
                                if f == KC - 1:
                                    while st_jobs:
                                        st_jobs.pop(0)()
                        for dh in range(2):
                            wdb = wget(6 * e + 4 + dh, 6 * e + 4 + dh)
                            for (cb, m, c0) in cbs:
                                py = bld.ps()
                                for f in range(KC):
                                    mm(py[0:m, :], actT[:, f, c0:c0 + m], wdb[:, f, :], start=(f == 0), stop=(f == KC - 1))
                                act(ye[par][0:m, cb, dh * 512:(dh + 1) * 512], py[0:m, :], AF.Copy, scale=val_tok[0:m, cb, e:e + 1])
                        if par == 1:
                            for k in range(KC):
                                for (t0, n, s) in TILES[1:]:
                                    pz = bld.ps()
                                    i = 0
                                    for pp in range(2):
                                        for cb in range(2):
                                            mm(pz[:, 0:n], ye[pp][:, cb, k * 128:(k + 1) * 128], ST_lat[pp][:, cb, t0 - NCX:t0 - NCX + n],
                                               start=(i == 0), stop=(i == 3))
                                            i += 1
                                    stt('dve', xT[:, k, t0:t0 + n], pz[:, 0:n], mod_ap(l, 5, k, 0), xT[:, k, t0:t0 + n], ALU.mult, ALU.add)
                                if need_ctx:
                                    pz = bld.ps()
                                    for pp in range(2):
                                        mm(pz[:, 0:NCX], ye[pp][0:32, 2, k * 128:(k + 1) * 128], ST_ctx[pp][:], start=(pp == 0), stop=(pp == 1))
                                    stt('dve', xT[:, k, 0:NCX], pz[:, 0:NCX], mod_ap(l, 5, k, 1), xT[:, k, 0:NCX], ALU.mult, ALU.add)
                    P.barrier()

        def final_phase():
            with ExitStack() as ph:
                sq = [SB(ph, f"fn_sq{i}", [128, 512], BF16) for i in range(4)]
                rs = [SB(ph, f"fn_rs{i}", [128, 512], F32) for i in range(2)]
                ot = [SB(ph, f"fn_o{i}", [128, 512], F32) for i in range(3)]
                oc = 0
                for ti, (t0, n, s) in enumerate(TILES[1:]):
                    pss = bld.ps()
                    for k in range(KC):
                        q = sq[k % 4]
                        act(q[:, 0:n], xT[:, k, t0:t0 + n], AF.Square)
                        mm(pss[:, 0:n], ones_b[:], q[:, 0:n], start=(k == 0), stop=(k == KC - 1))
                    r = rs[ti % 2]
                    act(r[:, 0:n], pss[:, 0:n], AF.Ln, scale=1.0 / D, bias=EPS)
                    act(r[:, 0:n], r[:, 0:n], AF.Exp, scale=-0.5)
                    for k in range(KC):
                        o = ot[oc % 3]
                        oc += 1
                        stt('dve', o[:, 0:n], xT[:, k, t0:t0 + n], fing[:, k:k + 1], r[:, 0:n], ALU.mult, ALU.mult)
                        dma('sp', out_d[k * 128:(k + 1) * 128, t0 - NCX:t0 - NCX + n], o[:, 0:n])
                P.barrier()

        def dump():
            for k in range(KC):
                dma('sp', dbg_d[k * 128:(k + 1) * 128, :], xT[:, k, :])
            P.barrier()

        done = False
        for l in range(n_layers):
            need_ctx = l < DEPTH - 1
            if l % 2 == 0:
                lru_phase(l, need_ctx)
            else:
                attn_phase(l, need_ctx)
            if stop_after == (l, 'mix'):
                dump()
                done = True
                break
            moe_phase(l, need_ctx)
            if stop_after == (l, 'moe'):
                dump()
                done = True
                break
        if not done:
            final_phase()
            if stop_after is not None:
                dump()
        P.emit()
    return nc, P


def _fm(v):
    v = np.asarray(v, np.float32)
    lead = v.shape[:-1]
    r = v.reshape(lead + (8, 128))
    return np.ascontiguousarray(np.moveaxis(r, -1, 0))


def make_in_maps(inp):
    f32 = np.float32
    x = np.asarray(inp["x"], f32)
    ctx = np.asarray(inp["ctx"], f32)
    c = np.asarray(inp["c"], f32)
    c_ctx = np.asarray(inp["c_ctx"], f32)
    nb = x.shape[0]
    shared = {}
    shared["ada_w"] = np.ascontiguousarray(inp["ada_w"], f32)
    shared["ada_b"] = np.ascontiguousarray(np.asarray(inp["ada_b"], f32).reshape(4, 48, 128).transpose(2, 0, 1))
    shared["n1g"] = _fm(inp["norm1_g"])
    shared["n2g"] = _fm(inp["norm2_g"])
    shared["fing"] = _fm(inp["final_g"])
    shared["lru_w_in"] = np.ascontiguousarray(inp["lru_w_in"], f32)
    shared["lru_w_out"] = np.ascontiguousarray(inp["lru_w_out"], f32)
    cw = np.asarray(inp["lru_conv_w"], f32)
    shared["conv_w"] = np.ascontiguousarray(cw.reshape(2, 4, 8, 128).transpose(3, 0, 2, 1))
    shared["conv_b"] = _fm(inp["lru_conv_b"])
    shared["gate_w"] = np.ascontiguousarray(inp["lru_gate_w"], f32)
    gb = np.asarray(inp["lru_gate_b"], f32)
    shared["gate_b"] = np.ascontiguousarray(gb.reshape(2, 2, 8, 2, 128).transpose(4, 0, 1, 2, 3))
    shared["lam"] = _fm(inp["lru_lambda"])
    wqkv = np.asarray(inp["attn_w_qkv"], f32)
    shared["w_qkv"] = np.ascontiguousarray(wqkv)
    perm = np.concatenate([np.arange(32, 64), np.arange(0, 32), np.arange(96, 128), np.arange(64, 96)])
    cols = (np.arange(10)[:, None] * 128 + perm[None, :]).reshape(-1)
    shared["w_qkv_sw"] = np.ascontiguousarray(wqkv[:, :, cols])
    shared["sink"] = np.ascontiguousarray(np.broadcast_to(np.asarray(inp["attn_sink"], f32)[None], (128, 2, 8)))
    shared["w_o"] = np.ascontiguousarray(inp["attn_w_o"], f32)
    r = np.asarray(inp["moe_router"], f32)
    shared["router"] = np.ascontiguousarray(r.reshape(4, 8, 128, 16).transpose(2, 0, 1, 3))
    shared["moe_w_gate"] = np.ascontiguousarray(inp["moe_w_gate"], f32)
    shared["moe_w_up"] = np.ascontiguousarray(inp["moe_w_up"], f32)
    shared["moe_w_down"] = np.ascontiguousarray(inp["moe_w_down"], f32)
    shared["ident"] = np.eye(128, dtype=f32)
    half = 64
    freqs = (10000.0 ** (-np.arange(0, half, 2, dtype=np.float32) / half)).astype(f32)
    t = np.arange(NL)
    rows = (t // 64).astype(f32)
    colsp = (t % 64).astype(f32)
    rc = np.zeros((128, NL), f32)
    rs = np.zeros((128, NL), f32)
    for p in range(128):
        pos = rows if p < 64 else colsp
        jf = p % 32
        ang = (pos * freqs[jf]).astype(f32)
        rc[p] = np.cos(ang)
        sgn = -1.0 if (p % 64) < 32 else 1.0
        rs[p] = sgn * np.sin(ang)
    shared["rope_c"] = rc
    shared["rope_s"] = rs
    shared["iota_row"] = np.ascontiguousarray(np.broadcast_to(np.arange(NL, dtype=f32)[None], (128, NL)))
    shared["tokidx"] = (np.arange(128, dtype=f32)[:, None] + 128.0 * np.arange(16, dtype=f32)[None, :]).astype(f32)
    s_i = np.arange(128)[:, None]
    q_i = np.arange(128)[None, :]
    shared["mask_prev"] = (s_i >= q_i).astype(f32)
    shared["mask_next"] = (s_i <= q_i).astype(f32)
    shared["halfoff"] = np.concatenate([np.zeros((16, 1), f32), np.full((16, 1), 1024.0, f32)], axis=0)
    shared["pidx16"] = np.ascontiguousarray(np.broadcast_to(np.arange(16, dtype=f32)[:, None], (16, 128)))
    maps = []
    for b in range(nb):
        m = dict(shared)
        m["xT"] = np.ascontiguousarray(np.concatenate([ctx[b].T, x[b].T], axis=1))
        cc = np.stack([c[b].reshape(8, 128).T, c_ctx.reshape(8, 128).T], axis=-1)
        m["cc"] = np.ascontiguousarray(cc, dtype=f32)
        maps.append(m)
    return maps


_CACHE = {}


def kernel(**inputs):
    if "nc" not in _CACHE:
        _CACHE["nc"] = build_program()[0]
    nc = _CACHE["nc"]
    maps = make_in_maps(inputs)
    res = run_bass_kernel_spmd(nc, maps, core_ids=list(range(len(maps))))
    out = np.stack([np.ascontiguousarray(r["outT"].T) for r in res.results], axis=0)
    return out.astype(np.float32)
```

```python
import numpy as np
import concourse.bass as bass
import concourse.mybir as mybir

F32 = mybir.dt.float32
BF16 = mybir.dt.bfloat16
I32 = mybir.dt.int32
U32 = mybir.dt.uint32
AF = mybir.ActivationFunctionType
ALU = mybir.AluOpType
AX = mybir.AxisListType

ENGS = ['pe', 'act', 'dve', 'pool', 'sp']
NDSEM = 6
PE_DELAY_OPS = {'act': 1, 'dve': 4, 'pool': 4}


def _prod(xs):
    r = 1
    for v in xs:
        r *= int(v)
    return r


def region(ap):
    t = ap.tensor
    name = ap.name
    shape = tuple(t.shape)
    pairs = [(int(s), int(c)) for s, c in ap.ap]
    off = int(ap.offset)
    sp = str(ap.space)
    if 'DRAM' in sp.upper() or 'HBM' in sp.upper():
        lo = off
        hi = off
        for s, c in pairs:
            if s >= 0:
                hi += s * (c - 1)
            else:
                lo += s * (c - 1)
        return (name, 0, 1, lo, hi + 1)
    fsz = _prod(shape[1:])
    p0 = off // fsz
    rem = off % fsz
    ps, pc = pairs[0]
    p1 = p0 + (ps // fsz) * (pc - 1) + 1 if pc > 1 else p0 + 1
    lo = rem
    hi = rem
    for s, c in pairs[1:]:
        if s >= 0:
            hi += s * (c - 1)
        else:
            lo += s * (c - 1)
    return (name, p0, p1, lo, hi + 1)


def _overlap(a, b):
    return a[1] < b[2] and b[1] < a[2] and a[3] < b[4] and b[3] < a[4]


def _contains(a, b):
    return a[1] <= b[1] and b[2] <= a[2] and a[3] <= b[3] and b[4] <= a[4]


class Prog:
    def __init__(self, nc, stack):
        self.nc = nc
        self.ops = {e: [] for e in ENGS}
        self.cnt = {e: 0 for e in ENGS}
        self.sems = {}
        for e in ENGS:
            self.sems[('e', e)] = stack.enter_context(nc.semaphore(f"s_{e}"))
        self.dcnt = {}
        self.dnext = {}
        for q in ['sp', 'act', 'pool']:
            self.dnext[q] = 0
            for i in range(NDSEM):
                self.sems[('d', q, i)] = stack.enter_context(nc.semaphore(f"d_{q}{i}"))
                self.dcnt[(q, i)] = 0
        self.waited = {e: {} for e in ENGS}
        self.wr = {}
        self.rd = {}
        self.nwaits = 0
        self.flush = False
        self.pe_delay = True
        self.scratch = {}

    def _deps(self, reads, writes, token):
        deps = {}

        def add(k, v):
            if deps.get(k, 0) < v:
                deps[k] = v
        for ap in reads:
            r = region(ap)
            w = self.wr.setdefault(r[0], {})
            for (rg, sk), v in w.items():
                if _overlap(rg, r):
                    add(sk, v)
        for ap in writes:
            r = region(ap)
            w = self.wr.setdefault(r[0], {})
            d = self.rd.setdefault(r[0], {})
            for tab in (w, d):
                dead = []
                for (rg, sk), v in tab.items():
                    if _overlap(rg, r):
                        add(sk, v)
                        if _contains(r, rg):
                            dead.append((rg, sk))
                for k in dead:
                    del tab[k]
        for ap in reads:
            r = region(ap)
            self.rd.setdefault(r[0], {})[(r, token[0])] = token[1]
        for ap in writes:
            r = region(ap)
            self.wr.setdefault(r[0], {})[(r, token[0])] = token[1]
        return deps

    def _emit_waits(self, eng, deps):
        pe_wait = False
        for sk, v in deps.items():
            if eng == 'pe' and sk == ('e', 'pe'):
                continue
            if self.waited[eng].get(sk, 0) >= v:
                continue
            self.waited[eng][sk] = v
            self.ops[eng].append(('wait', sk, v))
            self.nwaits += 1
            if sk == ('e', 'pe'):
                pe_wait = True
        if pe_wait and self.pe_delay and eng in self.scratch:
            sc = self.scratch[eng]
            for _ in range(PE_DELAY_OPS[eng]):
                if eng == 'act':
                    self.ops[eng].append(('raw', lambda e: e.copy(sc[:], sc[:])))
                else:
                    self.ops[eng].append(('raw', lambda e: e.memset(sc[:], 0.0)))

    def op(self, eng, fn, reads=(), writes=()):
        fl = self.flush and eng in ('act', 'dve', 'pool') and eng in self.scratch
        token = (('e', eng), self.cnt[eng] + (2 if fl else 1))
        deps = self._deps(list(reads), list(writes), token)
        self._emit_waits(eng, deps)
        self.cnt[eng] += 1
        self.ops[eng].append(('op', fn, token[0]))
        if fl:
            sc = self.scratch[eng]
            self.cnt[eng] += 1
            if eng == 'act':
                self.ops[eng].append(('op', lambda e: e.copy(sc[:], sc[:]), token[0]))
            else:
                self.ops[eng].append(('op', lambda e: e.memset(sc[:], 0.0), token[0]))

    def dma(self, q, out, in_, **kw):
        i = self.dnext[q] % NDSEM
        self.dnext[q] += 1
        sk = ('d', q, i)
        prev = self.dcnt[(q, i)]
        self.dcnt[(q, i)] = prev + 16
        token = (sk, prev + 16)
        deps = self._deps([in_], [out], token)
        if prev > 0:
            if deps.get(sk, 0) < prev:
                deps[sk] = prev
        self._emit_waits(q, deps)
        self.ops[q].append(('dma', (out, in_, kw), sk))

    def dma_custom(self, q, out, in_, fn, extra_reads=()):
        i = self.dnext[q] % NDSEM
        self.dnext[q] += 1
        sk = ('d', q, i)
        prev = self.dcnt[(q, i)]
        self.dcnt[(q, i)] = prev + 16
        token = (sk, prev + 16)
        deps = self._deps([in_] + list(extra_reads), [out], token)
        if prev > 0:
            if deps.get(sk, 0) < prev:
                deps[sk] = prev
        self._emit_waits(q, deps)
        self.ops[q].append(('dmac', (out, in_, fn), sk))

    def barrier(self):
        for e in ENGS:
            deps = {}
            for e2 in ENGS:
                if e2 != e and self.cnt[e2] > 0:
                    deps[('e', e2)] = self.cnt[e2]
            if e != 'pe' and self.cnt[e] > 0:
                deps[('e', e)] = self.cnt[e]
            for (q, i), v in self.dcnt.items():
                if v > 0:
                    deps[('d', q, i)] = v
            self._emit_waits(e, deps)
        self.wr = {}
        self.rd = {}

    def emit(self):
        nc = self.nc
        sems = self.sems
        ops = self.ops

        def run(eng_name, eng):
            for item in ops[eng_name]:
                if item[0] == 'wait':
                    eng.wait_ge(sems[item[1]], item[2])
                elif item[0] == 'op':
                    ins = item[1](eng)
                    ins.then_inc(sems[item[2]], 1)
                elif item[0] == 'raw':
                    item[1](eng)
                elif item[0] == 'dmac':
                    out, in_, fn = item[1]
                    fn(eng, out, in_, {}).then_inc(sems[item[2]], 16)
                else:
                    out, in_, kw = item[1]
                    eng.dma_start(out=out, in_=in_, **kw).then_inc(sems[item[2]], 16)

        with nc.Block() as block:
            @block.tensor
            def _(e):
                run('pe', e)

            @block.scalar
            def _(e):
                run('act', e)

            @block.vector
            def _(e):
                run('dve', e)

            @block.gpsimd
            def _(e):
                run('pool', e)

            @block.sync
            def _(e):
                run('sp', e)

from contextlib import ExitStack
import os
MOE_CUT = int(os.environ.get('MOE_CUT', '99'))
MOE_E0 = int(os.environ.get('MOE_E0', '0'))
MOE_E1 = int(os.environ.get('MOE_E1', '16'))
from concourse.bass_utils import run_bass_kernel_spmd

D = 1024
KC = 8
NL = 2048
NCX = 256
T = NL + NCX
DEPTH = 4
NE = 16
CAPL = 256
CAPC = 32
EPS = 1e-6
QSCALE = 128 ** -0.5
TILES = [(0, 256, 1), (256, 512, 0), (768, 512, 0), (1280, 512, 0), (1792, 512, 0)]


class B:
    def __init__(self, nc, P, st):
        self.nc = nc
        self.P = P
        self.st = st
        self.psn = 0
        self.rr = 0

    def mm(self, out, lhsT, rhs, start=True, stop=True):
        self.P.op('pe', lambda e: e.matmul(out, lhsT, rhs, start=start, stop=stop),
                  reads=[lhsT, rhs], writes=[out])

    def tr(self, out, in_, ident):
        self.mm(out, in_, ident)

    def act(self, out, in_, func, bias=None, scale=None):
        kw = {}
        rd = [in_]
        if bias is not None:
            kw['bias'] = bias
            if not isinstance(bias, float):
                rd.append(bias)
        if scale is not None:
            kw['scale'] = scale
            if not isinstance(scale, float):
                rd.append(scale)
        self.P.op('act', lambda e: e.activation(out, in_, func, **kw), reads=rd, writes=[out])

    def tt(self, eng, out, in0, in1, op):
        self.P.op(eng, lambda e: e.tensor_tensor(out, in0, in1, op), reads=[in0, in1], writes=[out])

    def ts(self, eng, out, in0, s1, s2, op0, op1=None):
        rd = [in0]
        if not isinstance(s1, float):
            rd.append(s1)
        if s2 is not None and not isinstance(s2, float):
            rd.append(s2)
        if op1 is None:
            self.P.op(eng, lambda e: e.tensor_scalar(out, in0, s1, None, op0), reads=rd, writes=[out])
        else:
            self.P.op(eng, lambda e: e.tensor_scalar(out, in0, s1, s2, op0, op1), reads=rd, writes=[out])

    def stt(self, eng, out, in0, scalar, in1, op0, op1):
        rd = [in0, in1]
        if not isinstance(scalar, float):
            rd.append(scalar)
        self.P.op(eng, lambda e: e.scalar_tensor_tensor(out, in0, scalar, in1, op0, op1), reads=rd, writes=[out])

    def cp(self, eng, out, in_):
        if eng == 'act':
            self.P.op('act', lambda e: e.copy(out, in_), reads=[in_], writes=[out])
        else:
            self.P.op(eng, lambda e: e.tensor_copy(out, in_), reads=[in_], writes=[out])

    def memset(self, eng, out, val):
        self.P.op(eng, lambda e: e.memset(out, val), reads=[], writes=[out])

    def scan(self, out, a, b, init):
        rd = [a, b]
        if not isinstance(init, float):
            rd.append(init)
        self.P.op('dve', lambda e: e.tensor_tensor_scan(out, a, b, init, ALU.mult, ALU.add), reads=rd, writes=[out])

    def dma(self, q, out, in_):
        self.P.dma(q, out, in_)

    def ps(self):
        b = self.banks[self.psn % len(self.banks)]
        self.psn += 1
        return b

    def alt(self, engs=('act', 'dve')):
        self.rr += 1
        return engs[self.rr % len(engs)]


def build_program(stop_after=None, n_layers=DEPTH):
    nc = bass.Bass("TRN2", target_bir_lowering=False)
    dram = {}

    def din(name, shape, dt=F32):
        dram[name] = nc.dram_tensor(name, list(shape), dt, kind="ExternalInput").ap()
        return dram[name]

    xT_d = din("xT", [D, T])
    cc_d = din("cc", [128, KC, 2])
    ada_w_d = din("ada_w", [DEPTH, D, 6 * D])
    ada_b_d = din("ada_b", [128, DEPTH, 48])
    n1g_d = din("n1g", [128, DEPTH, KC])
    n2g_d = din("n2g", [128, DEPTH, KC])
    fing_d = din("fing", [128, KC])
    lwin_d = din("lru_w_in", [2, D, 2 * D])
    lwout_d = din("lru_w_out", [2, D, D])
    convw_d = din("conv_w", [128, 2, KC, 4])
    convb_d = din("conv_b", [128, 2, KC])
    gatew_d = din("gate_w", [2, 2, 8, 128, 256])
    gateb_d = din("gate_b", [128, 2, 2, 8, 2])
    lam_d = din("lam", [128, 2, 2, 8])
    wqkv_d = din("w_qkv", [2, D, 1536])
    wqsw_d = din("w_qkv_sw", [2, D, 1280])
    sink_d = din("sink", [128, 2, 8])
    wo_d = din("w_o", [2, D, D])
    router_d = din("router", [128, DEPTH, KC, NE])
    wg_d = din("moe_w_gate", [DEPTH, NE, D, D])
    wu_d = din("moe_w_up", [DEPTH, NE, D, D])
    wd_d = din("moe_w_down", [DEPTH, NE, D, D])
    ident_d = din("ident", [128, 128])
    ropec_d = din("rope_c", [128, NL])
    ropes_d = din("rope_s", [128, NL])
    iota_d = din("iota_row", [128, NL])
    tokidx_d = din("tokidx", [128, 16])
    mprev_d = din("mask_prev", [128, 128])
    mnext_d = din("mask_next", [128, 128])
    pidx_d = din("pidx16", [16, 128])
    halfoff_d = din("halfoff", [32, 1])
    out_d = nc.dram_tensor("outT", [D, NL], F32, kind="ExternalOutput").ap()
    scr_d = nc.dram_tensor("scr_tabs", [DEPTH, 2, 16, 288], F32).ap()
    scr2_d = nc.dram_tensor("scr_half", [DEPTH, 2, 32, CAPL], F32).ap()
    hscr_d = [nc.dram_tensor(f"scr_htok{i}", [T, D], BF16).ap() for i in range(DEPTH)]
    dbg_d = None
    if stop_after is not None:
        dbg_d = nc.dram_tensor("dbgx", [D, T], F32, kind="ExternalOutput").ap()
        dbg_aff = nc.dram_tensor("dbg_aff", [16, T], F32, kind="ExternalOutput").ap()
        dbg_idx = nc.dram_tensor("dbg_idx", [16, 288], F32, kind="ExternalOutput").ap()
        dbg_val = nc.dram_tensor("dbg_val", [16, 288], F32, kind="ExternalOutput").ap()
        dbg_it = nc.dram_tensor("dbg_it", [128, 3, NE], F32, kind="ExternalOutput").ap()
        dbg_vt = nc.dram_tensor("dbg_vt", [128, 3, NE], F32, kind="ExternalOutput").ap()

    with ExitStack() as st:
        P = Prog(nc, st)
        bld = B(nc, P, st)
        mm, act, tt, ts, stt, cp, dma = bld.mm, bld.act, bld.tt, bld.ts, bld.stt, bld.cp, bld.dma

        def SB(stack, name, shape, dt):
            return stack.enter_context(nc.sbuf_tensor(name, list(shape), dt))

        banks = [st.enter_context(nc.psum_tensor(f"ps{i}", [128, 512], F32)) for i in range(7)]
        psb = st.enter_context(nc.psum_tensor("psb", [128, 1024], BF16))
        bld.banks = banks
        for en in ('act', 'dve', 'pool'):
            P.scratch[en] = SB(st, f"flush_{en}", [128, 1], F32)
        P.flush = os.environ.get('FLUSH', '0') == '1'
        P.pe_delay = os.environ.get('PE_DELAY', '1') == '1'

        xT = SB(st, "xT_sb", [128, KC, T], F32)
        ident_f = SB(st, "ident_f", [128, 128], F32)
        ident_b = SB(st, "ident_b", [128, 128], BF16)
        ones_b = SB(st, "ones_b", [128, 128], BF16)
        mod = SB(st, "mod", [128, DEPTH, 48, 2], F32)
        gp1 = SB(st, "gp1", [128, DEPTH, KC, 2], F32)
        gp2 = SB(st, "gp2", [128, DEPTH, KC, 2], F32)
        n1g = SB(st, "n1g_sb", [128, DEPTH, KC], F32)
        n2g = SB(st, "n2g_sb", [128, DEPTH, KC], F32)
        fing = SB(st, "fing_sb", [128, KC], F32)
        ada_b = SB(st, "ada_b_sb", [128, DEPTH, 48], F32)
        convw = SB(st, "convw_sb", [128, 2, KC, 4], F32)
        convb = SB(st, "convb_sb", [128, 2, KC], F32)
        gateb = SB(st, "gateb_sb", [128, 2, 2, 8, 2], F32)
        gatebh = SB(st, "gatebh_sb", [128, 2, 2, 8, 2], F32)
        lam = SB(st, "lam_sb", [128, 2, 2, 8], F32)
        ls4 = SB(st, "ls4", [128, 2, 2, 8], F32)
        ls8 = SB(st, "ls8", [128, 2, 2, 8], F32)
        esink = SB(st, "esink", [128, 2, 8], F32)
        router_b = SB(st, "router_b", [128, DEPTH, KC, NE], BF16)
        tokidx = SB(st, "tokidx_sb", [128, 16], F32)
        ntok = SB(st, "ntok_sb", [128, 16], F32)
        pidx16 = SB(st, "pidx16_sb", [16, 128], F32)
        halfoff = SB(st, "halfoff_sb", [32, 1], F32)
        scc = SB(st, "scc", [128, KC, 2], BF16)

        for k in range(KC):
            dma('sp', xT[:, k, :], xT_d[k * 128:(k + 1) * 128, :])
        rstk = ExitStack()
        router_f = SB(rstk, "router_f", [128, DEPTH, KC, NE], F32)
        for sb_t, d_t in [(ident_f, ident_d), (ada_b, ada_b_d), (n1g, n1g_d), (n2g, n2g_d), (fing, fing_d),
                          (convw, convw_d), (convb, convb_d), (gateb, gateb_d), (lam, lam_d), (esink, sink_d),
                          (router_f, router_d), (tokidx, tokidx_d), (pidx16, pidx_d), (halfoff, halfoff_d)]:
            dma('sp', sb_t[:], d_t)
        cp('dve', ident_b[:], ident_f[:])
        cp('dve', router_b[:], router_f[:])
        bld.memset('dve', ones_b[:], 1.0)
        act(esink[:], esink[:], AF.Exp)
        act(lam[:], lam[:], AF.Exp, scale=-1.0)
        ts('dve', lam[:], lam[:], 1.0, None, ALU.add)
        act(lam[:], lam[:], AF.Ln)
        ts('dve', ls4[:], lam[:], -4.0, None, ALU.mult)
        ts('dve', ls8[:], lam[:], -8.0, None, ALU.mult)
        ts('dve', gatebh[:], gateb[:], 0.5, None, ALU.mult)
        ts('dve', ntok[:], tokidx[:], -1.0, None, ALU.mult)
        P.barrier()
        rstk.close()

        def adaln_mm(l, bufs, W):
            psm = bld.ps()
            npc = (6 * D) // W
            for pc in range(npc):
                wb = bufs[pc % 2]
                dma('pool', wb[:], ada_w_d[l, :, pc * W:(pc + 1) * W].rearrange("(c p) f -> p c f", p=128))
                for jj in range(W // 128):
                    j = pc * (W // 128) + jj
                    for k in range(KC):
                        mm(psm[:, 2 * j:2 * j + 2], wb[:, k, jj * 128:(jj + 1) * 128], scc[:, k, :],
                           start=(k == 0), stop=(k == KC - 1))
            return psm

        def adaln_fin(l, psm):
            tt('dve', mod[:, l], psm[:, 0:96].rearrange("p (j s) -> p j s", s=2),
               ada_b[:, l, :].unsqueeze(2).to_broadcast([128, 48, 2]), ALU.add)
            for (gp, ng, grp) in ((gp1, n1g, 1), (gp2, n2g, 4)):
                ts('dve', gp[:, l], mod[:, l, grp * 8:(grp + 1) * 8, :], 1.0, None, ALU.add)
                tt('dve', gp[:, l], gp[:, l], ng[:, l, :].unsqueeze(2).to_broadcast([128, KC, 2]), ALU.mult)

        with ExitStack() as ph:
            cc = SB(ph, "cc_sb", [128, KC, 2], F32)
            adw = [SB(ph, f"adw{i}", [128, KC, 1024], BF16) for i in range(2)]
            dma('sp', cc[:], cc_d)
            act(scc[:], cc[:], AF.Silu)
            adaln_fin(0, adaln_mm(0, adw, 1024))
            P.barrier()

        def mod_ap(l, grp, k, s):
            return mod[:, l, grp * 8 + k, s:s + 1]

        def norm_mod(ph, hT, l, which, tiles):
            gp = gp1 if which == 1 else gp2
            shg = 0 if which == 1 else 3
            sq = [SB(ph, f"nm_sq{i}_{l}_{which}", [128, 512], BF16) for i in range(4)]
            rs = [SB(ph, f"nm_rs{i}_{l}_{which}", [128, 512], F32) for i in range(2)]
            tmp = [SB(ph, f"nm_tmp{i}_{l}_{which}", [128, 512], F32) for i in range(4)]
            for ti, (t0, n, s) in enumerate(tiles):
                pss = bld.ps()
                for k in range(KC):
                    q = sq[k % 4]
                    if k % 2 == 0:
                        act(q[:, 0:n], xT[:, k, t0:t0 + n], AF.Square)
                    else:
                        tt('dve', q[:, 0:n], xT[:, k, t0:t0 + n], xT[:, k, t0:t0 + n], ALU.mult)
                    mm(pss[:, 0:n], ones_b[:], q[:, 0:n], start=(k == 0), stop=(k == KC - 1))
                r = rs[ti % 2]
                act(r[:, 0:n], pss[:, 0:n], AF.Ln, scale=1.0 / D, bias=EPS)
                act(r[:, 0:n], r[:, 0:n], AF.Exp, scale=-0.5)
                for k in range(KC):
                    tm = tmp[k % 4]
                    tt('dve', tm[:, 0:n], xT[:, k, t0:t0 + n], r[:, 0:n], ALU.mult)
                    act(hT[:, k, t0:t0 + n], tm[:, 0:n], AF.Identity,
                        scale=gp[:, l, k, s:s + 1], bias=mod_ap(l, shg, k, s))

        def out_proj(ph, name, w_dram, mT_, l, ggrp, tiles):
            wo = SB(ph, name, [128, KC, D], BF16)
            for h in range(4):
                dma('pool', wo[:, :, h * 256:(h + 1) * 256],
                    w_dram[:, h * 256:(h + 1) * 256].rearrange("(k p) f -> p k f", p=128))
            for (t0, n, s) in tiles:
                for dm in range(KC):
                    po = bld.ps()
                    for c in range(KC):
                        mm(po[:, 0:n], wo[:, c, dm * 128:(dm + 1) * 128], mT_[:, c, t0:t0 + n],
                           start=(c == 0), stop=(c == KC - 1))
                    stt('dve', xT[:, dm, t0:t0 + n], po[:, 0:n], mod_ap(l, ggrp, dm, s), xT[:, dm, t0:t0 + n],
                        ALU.mult, ALU.add)

        def lru_phase(l, need_ctx):
            j = l // 2
            with ExitStack() as ph:
                hT = SB(ph, f"hT_l{l}", [128, KC, T], BF16)
                mT = SB(ph, f"mT_l{l}", [128, KC, T], BF16)
                winA = SB(ph, f"winA{l}", [128, KC, 128], BF16)
                winB = SB(ph, f"winB{l}", [128, KC, 128], BF16)
                gw = [SB(ph, f"gw{l}_{i}", [128, 256], BF16) for i in range(4)]

                def ldA(c):
                    dma('pool', winA[:], lwin_d[j, :, c * 128:(c + 1) * 128].rearrange("(k p) f -> p k f", p=128))

                def ldB(c):
                    dma('pool', winB[:], lwin_d[j, :, D + c * 128:D + (c + 1) * 128].rearrange("(k p) f -> p k f", p=128))
                ldB(0)
                for d in range(2):
                    dma('pool', gw[d][:], gatew_d[j, d, 0])
                ldA(0)
                with ExitStack() as nt:
                    norm_mod(nt, hT, l, 1, TILES)
                    P.barrier()
                with ExitStack() as lt:
                    ub = SB(lt, f"ub{l}", [128, T], F32)
                    xb = SB(lt, f"xb{l}", [128, T], F32)
                    xbb = SB(lt, f"xbb{l}", [128, T], BF16)
                    tr_ = [SB(lt, f"tr{l}_{i}", [128, 512], F32) for i in range(4)]
                    ti_ = [SB(lt, f"ti{l}_{i}", [128, 512], F32) for i in range(4)]
                    tb_ = [SB(lt, f"tb{l}_{i}", [128, 512], F32) for i in range(4)]
                    th_ = [SB(lt, f"th{l}_{i}", [128, 512], F32) for i in range(2)]
                    carry = SB(lt, f"carry{l}", [128, 2], F32)
                    orders = [TILES, [TILES[0]] + TILES[:0:-1]]
                    first_dir = {0: 0, 256: 0, 768: 0, 1280: 1, 1792: 1}
                    for c in range(KC):
                        if c + 1 < KC:
                            for d in range(2):
                                dma('pool', gw[((c + 1) % 2) * 2 + d][:], gatew_d[j, d, c + 1])
                        for (t0, n, s) in TILES:
                            pu = bld.ps()
                            for k in range(KC):
                                mm(pu[:, 0:n], winB[:, k, :], hT[:, k, t0:t0 + n], start=(k == 0), stop=(k == KC - 1))
                            cp(bld.alt(), ub[:, t0:t0 + n], pu[:, 0:n])
                        if c + 1 < KC:
                            ldB(c + 1)
                        for (t0, n, s) in TILES:
                            s0, sn = (0, NCX) if s == 1 else (NCX, NL)
                            ts('dve', xb[:, t0:t0 + n], ub[:, t0:t0 + n], convw[:, j, c, 1:2], convb[:, j, c:c + 1], ALU.mult, ALU.add)
                            for (o, kk) in ((-1, 0), (1, 2), (2, 3)):
                                lo = max(t0, s0 - o)
                                hi = min(t0 + n, s0 + sn - o)
                                stt('dve', xb[:, lo:hi], ub[:, lo + o:hi + o], convw[:, j, c, kk:kk + 1], xb[:, lo:hi], ALU.mult, ALU.add)
                            cp('pool', xbb[:, t0:t0 + n], xb[:, t0:t0 + n])
                        for (ga, gb_) in ((0, 1), (1, 3), (3, 5)):
                            for d in range(2):
                                g = gw[(c % 2) * 2 + d]
                                for gi, (t0, n, s) in enumerate(orders[d][ga:gb_]):
                                    slot = d * 2 + gi
                                    pr = bld.ps()
                                    pi = bld.ps()
                                    mm(pr[:, 0:n], g[:, 0:128], xbb[:, t0:t0 + n])
                                    mm(pi[:, 0:n], g[:, 128:256], xbb[:, t0:t0 + n])
                                    r_ = tr_[slot]
                                    i_ = ti_[slot]
                                    b_ = tb_[slot]
                                    act(r_[:, 0:n], pr[:, 0:n], AF.Tanh, scale=0.5, bias=gatebh[:, j, d, c, 0:1])
                                    act(i_[:, 0:n], pi[:, 0:n], AF.Tanh, scale=0.5, bias=gatebh[:, j, d, c, 1:2])
                                    act(b_[:, 0:n], r_[:, 0:n], AF.Exp, scale=ls8[:, j, d, c:c + 1], bias=ls8[:, j, d, c:c + 1])
                                    act(r_[:, 0:n], r_[:, 0:n], AF.Exp, scale=ls4[:, j, d, c:c + 1], bias=ls4[:, j, d, c:c + 1])
                                    ts('dve', b_[:, 0:n], b_[:, 0:n], 1.0, None, ALU.min)
                            for d in range(2):
                                for gi, (t0, n, s) in enumerate(orders[d][ga:gb_]):
                                    slot = d * 2 + gi
                                    r_ = tr_[slot]
                                    i_ = ti_[slot]
                                    b_ = tb_[slot]
                                    act(b_[:, 0:n], b_[:, 0:n], AF.Sqrt, scale=-0.25, bias=0.25)
                                    stt('dve', i_[:, 0:n], i_[:, 0:n], 1.0, xb[:, t0:t0 + n], ALU.add, ALU.mult)
                                    tt('dve', b_[:, 0:n], b_[:, 0:n], i_[:, 0:n], ALU.mult)
                                    init = 0.0 if ga == 0 else carry[:, d:d + 1]
                                    first = first_dir[t0] == d
                                    dst = ub[:, t0:t0 + n] if first else th_[gi][:, 0:n]
                                    if d == 0:
                                        bld.scan(dst, r_[:, 0:n], b_[:, 0:n], init)
                                        cp('dve', carry[:, 0:1], dst[:, n - 1:n])
                                    else:
                                        bld.scan(dst[:, ::-1], r_[:, n - 1::-1], b_[:, n - 1::-1], init)
                                        cp('dve', carry[:, 1:2], dst[:, 0:1])
                                    if not first:
                                        tt('pool', ub[:, t0:t0 + n], ub[:, t0:t0 + n], dst, ALU.add)
                        for ti, (t0, n, s) in enumerate(TILES):
                            if s == 1 and not need_ctx:
                                continue
                            pg = bld.ps()
                            for k in range(KC):
                                mm(pg[:, 0:n], winA[:, k, :], hT[:, k, t0:t0 + n], start=(k == 0), stop=(k == KC - 1))
                            y_ = th_[ti % 2]
                            act(y_[:, 0:n], pg[:, 0:n], AF.Gelu_apprx_tanh)
                            tt('dve', mT[:, c, t0:t0 + n], y_[:, 0:n], ub[:, t0:t0 + n], ALU.mult)
                        if c + 1 < KC:
                            ldA(c + 1)
                    P.barrier()
                with ExitStack() as ot:
                    tiles = TILES if need_ctx else TILES[1:]
                    out_proj(ot, f"lwo{l}", lwout_d[j], mT, l, 2, tiles)
                    P.barrier()

        def attn_phase(l, need_ctx):
            j = l // 2
            with ExitStack() as ph:
                hT = SB(ph, f"hT_l{l}", [128, KC, T], BF16)
                qT = SB(ph, f"qT_l{l}", [128, 8, T], BF16)
                kT = SB(ph, f"kT_l{l}", [128, 2, T], BF16)
                V = SB(ph, f"V_l{l}", [128, 18, 256], BF16)
                with ExitStack() as nt:
                    norm_mod(nt, hT, l, 1, TILES)
                    P.barrier()
                with ExitStack() as qt:
                    rc = SB(qt, f"rc{l}", [128, NL], F32)
                    rs_ = SB(qt, f"rs{l}", [128, NL], F32)
                    dma('sp', rc[:], ropec_d)
                    dma('sp', rs_[:], ropes_d)
                    wq = [SB(qt, f"wq{l}_{i}", [128, KC, 128], BF16) for i in range(2)]
                    ws = [SB(qt, f"ws{l}_{i}", [128, KC, 128], BF16) for i in range(2)]
                    wv = SB(qt, f"wv{l}", [128, KC, 256], BF16)
                    t1 = [SB(qt, f"t1{l}_{i}", [128, 512], F32) for i in range(2)]
                    t2 = [SB(qt, f"t2{l}_{i}", [128, 512], F32) for i in range(2)]
                    for hh in range(10):
                        a = wq[hh % 2]
                        b = ws[hh % 2]
                        dma('pool', a[:], wqkv_d[j, :, hh * 128:(hh + 1) * 128].rearrange("(k p) f -> p k f", p=128))
                        dma('pool', b[:], wqsw_d[j, :, hh * 128:(hh + 1) * 128].rearrange("(k p) f -> p k f", p=128))
                        dst = qT[:, hh, :] if hh < 8 else kT[:, hh - 8, :]
                        for ti, (t0, n, s) in enumerate(TILES):
                            if s == 1 and hh < 8 and not need_ctx:
                                continue
                            p1 = bld.ps()
                            for k in range(KC):
                                mm(p1[:, 0:n], a[:, k, :], hT[:, k, t0:t0 + n], start=(k == 0), stop=(k == KC - 1))
                            if s == 1:
                                cp('act', dst[:, t0:t0 + n], p1[:, 0:n])
                                continue
                            p2 = bld.ps()
                            for k in range(KC):
                                mm(p2[:, 0:n], b[:, k, :], hT[:, k, t0:t0 + n], start=(k == 0), stop=(k == KC - 1))
                            a1 = t1[ti % 2]
                            a2 = t2[ti % 2]
                            tt('dve', a1[:, 0:n], p1[:, 0:n], rc[:, t0 - NCX:t0 - NCX + n], ALU.mult)
                            tt('dve', a2[:, 0:n], p2[:, 0:n], rs_[:, t0 - NCX:t0 - NCX + n], ALU.mult)
                            tt('pool', dst[:, t0:t0 + n], a1[:, 0:n], a2[:, 0:n], ALU.add)
                    dma('pool', wv[:], wqkv_d[j, :, 1280:1536].rearrange("(k p) f -> p k f", p=128))
                    for blk in range(18):
                        pv = bld.ps()
                        for k in range(KC):
                            mm(pv[:, 0:256], hT[:, k, blk * 128:(blk + 1) * 128], wv[:, k, :], start=(k == 0), stop=(k == KC - 1))
                        cp(bld.alt(), V[:, blk, :], pv[:, 0:256])
                    P.barrier()
                with ExitStack() as at:
                    oT = hT
                    mprev = SB(at, f"mprev{l}", [128, 128], BF16)
                    mnext = SB(at, f"mnext{l}", [128, 128], BF16)
                    dma('pool', mprev[:], mprev_d)
                    dma('pool', mnext[:], mnext_d)
                    ex = [SB(at, f"ex{l}_{i}", [128, 512], BF16) for i in range(6)]
                    dn = [SB(at, f"dn{l}_{i}", [128, 512], F32) for i in range(2)]
                    exc = 0
                    qblocks = [(b, True) for b in range(2, 18)]
                    if need_ctx:
                        qblocks += [(0, False), (1, False)]
                    for qi, (qb, is_lat) in enumerate(qblocks):
                        q0 = qb * 128
                        for kvh in range(2):
                            keys = [(0, None), (1, None)]
                            if is_lat:
                                if qb > 2:
                                    keys.append((qb - 1, mprev))
                                keys.append((qb, None))
                                if qb < 17:
                                    keys.append((qb + 1, mnext))
                            qsl = qT[:, kvh * 4:(kvh + 1) * 4, q0:q0 + 128]
                            po = bld.ps()
                            pd = bld.ps()
                            es = []
                            for (kb, msk) in keys:
                                psc = bld.ps()
                                mm(psc[:, :].rearrange("p (g q) -> p g q", g=4), kT[:, kvh, kb * 128:(kb + 1) * 128], qsl)
                                e_ = ex[exc % 6]
                                exc += 1
                                act(e_[:], psc[:], AF.Exp, scale=QSCALE)
                                if msk is not None:
                                    e3 = e_[:, :].rearrange("p (g q) -> p g q", g=4)
                                    tt('pool', e3, e3, msk[:, :].unsqueeze(1).to_broadcast([128, 4, 128]), ALU.mult)
                                es.append((kb, e_))
                            for i, (kb, e_) in enumerate(es):
                                mm(po[:], V[:, kb, kvh * 128:(kvh + 1) * 128], e_[:], start=(i == 0), stop=(i == len(es) - 1))
                            for i, (kb, e_) in enumerate(es):
                                mm(pd[:], ones_b[:], e_[:], start=(i == 0), stop=(i == len(es) - 1))
                            d_ = dn[(qi * 2 + kvh) % 2]
                            tt('dve', d_[:, :].rearrange("p (g q) -> p g q", g=4), pd[:, :].rearrange("p (g q) -> p g q", g=4),
                               esink[:, j, kvh * 4:(kvh + 1) * 4].unsqueeze(2).to_broadcast([128, 4, 128]), ALU.add)
                            P.op('dve', (lambda dd: lambda e: e.reciprocal(dd[:], dd[:]))(d_), reads=[d_[:]], writes=[d_[:]])
                            tt('dve', oT[:, kvh * 4:(kvh + 1) * 4, q0:q0 + 128], po[:, :].rearrange("p (g q) -> p g q", g=4),
                               d_[:, :].rearrange("p (g q) -> p g q", g=4), ALU.mult)
                    P.barrier()
                with ExitStack() as ot:
                    tiles = TILES if need_ctx else TILES[1:]
                    out_proj(ot, f"awo{l}", wo_d[j], oT, l, 2, tiles)
                    P.barrier()

        def moe_phase(l, need_ctx):
            tiles = TILES if need_ctx else TILES[1:]
            CT = CAPL + CAPC if need_ctx else CAPL
            cbs = [(0, 128, 0), (1, 128, 128)] + ([(2, 32, 256)] if need_ctx else [])
            with ExitStack() as ph:
                idx_i = SB(ph, f"idxi{l}", [128, 3, NE], I32)
                val_tok = SB(ph, f"valtok{l}", [128, 3, NE], F32)
                nbias = SB(ph, f"nbias{l}", [128, 3, NE, 4], F32)
                iota_row = SB(ph, f"iota{l}", [128, 512], F32)
                dma('sp', iota_row[:], iota_d[:, 0:512])
                NW = 6
                wring = [SB(ph, f"wr{l}_{i}", [128, KC, 512], BF16) for i in range(NW)]
                pieces = []
                for e in range(NE):
                    for fh in range(2):
                        pieces.append(wg_d[l, e, :, fh * 512:(fh + 1) * 512])
                        pieces.append(wu_d[l, e, :, fh * 512:(fh + 1) * 512])
                    for dh in range(2):
                        pieces.append(wd_d[l, e, :, dh * 512:(dh + 1) * 512])
                issued = [6 * MOE_E0]

                def ensure(i):
                    while issued[0] < min(len(pieces), i + NW):
                        pi_ = issued[0]
                        dma('pool', wring[pi_ % NW][:], pieces[pi_].rearrange("(k p) f -> p k f", p=128))
                        issued[0] += 1

                ensure(6 * MOE_E0)
                with ExitStack() as rt:
                    hT = SB(rt, f"hTm{l}", [128, KC, T], BF16)
                    htok = [SB(rt, f"htok{l}_{i}", [128, D], BF16) for i in range(3)]
                    with ExitStack() as nt:
                        norm_mod(nt, hT, l, 2, tiles)
                        P.barrier()
                    aff = SB(rt, f"aff{l}", [128, 18, NE], F32)
                    mx = SB(rt, f"mx{l}", [128, 18], F32)
                    affC = SB(rt, f"affC{l}", [16, NCX], F32)
                    affL = SB(rt, f"affL{l}", [32, 1024], F32)
                    valsL = SB(rt, f"valsL{l}", [32, CAPL], F32)
                    idxL_u = SB(rt, f"idxLu{l}", [32, CAPL], U32)
                    idxL_f = SB(rt, f"idxLf{l}", [32, CAPL], F32)
                    Bv = SB(rt, f"Bv{l}", [16, CAPL], F32)
                    Bi = SB(rt, f"Bi{l}", [16, CAPL], F32)
                    sel = SB(rt, f"sel{l}", [16, CAPL], F32)
                    dd = SB(rt, f"dd{l}", [16, CAPL], F32)
                    vals = SB(rt, f"vals{l}", [16, 288], F32)
                    idx_u = SB(rt, f"idxu{l}", [16, 288], U32)
                    idx_f = SB(rt, f"idxf{l}", [16, 288], F32)
                    idxT = SB(rt, f"idxT{l}", [128, 3, NE], F32)
                    blks = list(range(18)) if need_ctx else list(range(2, 18))
                    pl = bld.ps()

                    def posb(b):
                        if b < 2:
                            return b
                        lb = b - 2
                        return 2 + (lb % 8) * 2 + lb // 8
                    for b in blks:
                        pb_ = posb(b)
                        for k in range(KC):
                            mm(pl[:, pb_ * NE:(pb_ + 1) * NE], hT[:, k, b * 128:(b + 1) * 128], router_b[:, l, k, :],
                               start=(k == 0), stop=(k == KC - 1))
                    b0, nb = blks[0], len(blks)
                    pl3 = pl[:, b0 * NE:(b0 + nb) * NE].rearrange("p (b e) -> p b e", e=NE)
                    a3 = aff[:, b0:b0 + nb, :]
                    P.op('dve', lambda e: e.tensor_reduce(mx[:, b0:b0 + nb], pl3, AX.X, ALU.max), reads=[pl3], writes=[mx[:, b0:b0 + nb]])
                    tt('dve', a3, pl3, mx[:, b0:b0 + nb].unsqueeze(2).to_broadcast([128, nb, NE]), ALU.subtract)
                    act(a3, a3, AF.Exp)
                    P.op('dve', lambda e: e.tensor_reduce(mx[:, b0:b0 + nb], a3, AX.X, ALU.add), reads=[a3], writes=[mx[:, b0:b0 + nb]])
                    P.op('dve', lambda e: e.reciprocal(mx[:, b0:b0 + nb], mx[:, b0:b0 + nb]), reads=[mx[:, b0:b0 + nb]], writes=[mx[:, b0:b0 + nb]])
                    tt('dve', a3, a3, mx[:, b0:b0 + nb].unsqueeze(2).to_broadcast([128, nb, NE]), ALU.mult)
                    if need_ctx:
                        pt = bld.ps()
                        for b in range(2):
                            bld.tr(pt[0:16, b * 128:(b + 1) * 128], aff[:, b, :], ident_f[:])
                        cp('act', affC[:, 0:NCX], pt[0:16, 0:NCX])
                    for g in range(2):
                        pt = bld.ps()
                        for bb in range(4):
                            b = g * 4 + bb
                            bld.tr(pt[0:32, bb * 128:(bb + 1) * 128], aff[:, 2 + 2 * b:4 + 2 * b, :].rearrange("p a e -> p (a e)"), ident_f[:])
                        cp('act', affL[:, g * 512:(g + 1) * 512], pt[0:32, :])
                    for b in blks:
                        for half in range(2):
                            pt = bld.ps()
                            for kk in range(4):
                                k = half * 4 + kk
                                bld.tr(pt[:, kk * 128:(kk + 1) * 128], hT[:, k, b * 128:(b + 1) * 128], ident_b[:])
                            cp('act', htok[b % 3][:, half * 512:(half + 1) * 512], pt[:])
                        dma('sp', hscr_d[l][b * 128:(b + 1) * 128, :], htok[b % 3][:])
                    ada_psm = None
                    if l + 1 < n_layers:
                        adw2 = [SB(rt, f"adw2_{l}_{i}", [128, KC, 512], BF16) for i in range(2)]
                        ada_psm = adaln_mm(l + 1, adw2, 512)
                    def topk(w, vout, iout, cap):
                        for it in range(cap // 8):
                            v8 = vout[:, it * 8:it * 8 + 8]
                            i8 = iout[:, it * 8:it * 8 + 8]
                            P.op('dve', (lambda v8, w: lambda e: e.max(out=v8, in_=w))(v8, w), reads=[w], writes=[v8])
                            P.op('dve', (lambda v8, i8, w: lambda e: e.max_index(out=i8, in_max=v8, in_values=w))(v8, i8, w), reads=[w, v8], writes=[i8])
                            P.op('dve', (lambda v8, w: lambda e: e.match_replace(out=w, in_to_replace=v8, in_values=w, imm_value=0.0))(v8, w), reads=[w, v8], writes=[w])
                    topk(affL[:], valsL[:], idxL_u[:], CAPL)
                    if need_ctx:
                        topk(affC[:], vals[:, CAPL:CAPL + CAPC], idx_u[:, CAPL:CAPL + CAPC], CAPC)
                        cp('dve', idx_f[:, CAPL:CAPL + CAPC], idx_u[:, CAPL:CAPL + CAPC])
                    cp('dve', idxL_f[:], idxL_u[:])
                    ts('dve', idxL_f[:], idxL_f[:], halfoff[:, 0:1], None, ALU.add)
                    dma('sp', scr2_d[l, 0], valsL[:])
                    dma('sp', scr2_d[l, 1], idxL_f[:])
                    dma('sp', Bv[:], scr2_d[l, 0, 16:32, :])
                    dma('sp', Bi[:], scr2_d[l, 1, 16:32, :])
                    tt('dve', sel[:], valsL[0:16, :], Bv[:, ::-1], ALU.is_gt)
                    for (A_, B_, out_) in ((valsL[0:16, :], Bv, vals[:, 0:CAPL]), (idxL_f[0:16, :], Bi, idx_f[:, 0:CAPL])):
                        tt('dve', dd[:], A_, B_[:, ::-1], ALU.subtract)
                        tt('dve', dd[:], dd[:], sel[:], ALU.mult)
                        tt('dve', out_, dd[:], B_[:, ::-1], ALU.add)
                    if not need_ctx:
                        bld.memset('dve', idx_f[:, CAPL:288], 0.0)
                        bld.memset('dve', vals[:, CAPL:288], 0.0)
                    dma('sp', scr_d[l, 0], idx_f[:])
                    dma('sp', scr_d[l, 1], vals[:])
                    for (cb, m, c0) in cbs:
                        P.dma('sp', idxT[0:m, cb, :], scr_d[l, 0, :, c0:c0 + m].rearrange("e c -> c e"), allow_slow_non_contiguous=True)
                        P.dma('sp', val_tok[0:m, cb, :], scr_d[l, 1, :, c0:c0 + m].rearrange("e c -> c e"), allow_slow_non_contiguous=True)
                    for (cb, m, c0) in cbs:
                        if cb < 2:
                            ts('dve', nbias[0:m, cb, :, 0], idxT[0:m, cb, :], float(NCX), None, ALU.add)
                            cp('dve', idx_i[0:m, cb, :], nbias[0:m, cb, :, 0])
                            for q4 in range(4):
                                ts('dve', nbias[0:m, cb, :, q4], idxT[0:m, cb, :], -1.0, float(q4 * 512), ALU.mult, ALU.add)
                        else:
                            cp('dve', idx_i[0:m, cb, :], idxT[0:m, cb, :])
                            ts('dve', nbias[0:m, cb, :, 0], idxT[0:m, cb, :], -1.0, None, ALU.mult)
                    if ada_psm is not None:
                        adaln_fin(l + 1, ada_psm)
                    P.barrier()
                with ExitStack() as et:
                    xgt = [SB(et, f"xgt{l}_{i}", [128, 3, D], BF16) for i in range(2)]
                    dtmp = [SB(et, f"dtmp{l}_{i}", [128, 512], F32) for i in range(3)]
                    xg = SB(et, f"xg{l}", [128, KC, 288], BF16)
                    sa = [SB(et, f"sa{l}_{i}", [128, 288], F32) for i in range(2)]
                    actT = SB(et, f"actT{l}", [128, KC, 288], BF16)
                    ye = [SB(et, f"ye{l}_{i}", [128, 3, D], BF16) for i in range(2)]
                    ST_lat = [SB(et, f"STl{l}_{i}", [128, 2, NL], BF16) for i in range(2)]
                    ST_ctx = [SB(et, f"STc{l}_{i}", [32, NCX], BF16) for i in range(2)]

                    def wget(i, base):
                        ensure(base)
                        assert i < issued[0]
                        return wring[i % NW]

                    def gather(e):
                        buf = xgt[e % 2]
                        for (cb, m, c0) in cbs:
                            def mk(cb, m, e):
                                return lambda en, out, in_, kw: en.indirect_dma_start(
                                    out=out, out_offset=None, in_=in_,
                                    in_offset=bass.IndirectOffsetOnAxis(ap=idx_i[0:m, cb, e:e + 1], axis=0))
                            P.dma_custom('pool', buf[0:m, cb, :], hscr_d[l][:, :], mk(cb, m, e), extra_reads=[idx_i[0:m, cb, e:e + 1]])

                    gather(MOE_E0)
                    for e in range(MOE_E0, MOE_E1):
                        par = e % 2
                        if e + 1 < MOE_E1:
                            gather(e + 1)
                        ensure(6 * e)
                        st_jobs = []
                        for cb in range(2):
                            for q4 in range(4):
                                def job(cb=cb, q4=q4, e=e, par=par):
                                    dt_ = dtmp[(cb * 4 + q4) % 3]
                                    act(dt_[:], iota_row[:], AF.Abs, bias=nbias[:, cb, e, q4:q4 + 1])
                                    act(ST_lat[par][:, cb, q4 * 512:(q4 + 1) * 512], dt_[:], AF.Relu, scale=-1.0, bias=1.0)
                                st_jobs.append(job)
                        if need_ctx:
                            def jobc(e=e, par=par):
                                dt_ = dtmp[2]
                                act(dt_[0:32, 0:NCX], iota_row[0:32, 0:NCX], AF.Abs, bias=nbias[0:32, 2, e, 0:1])
                                act(ST_ctx[par][:], dt_[0:32, 0:NCX], AF.Relu, scale=-1.0, bias=1.0)
                            st_jobs.append(jobc)
                        buf = xgt[par]
                        for k in range(KC):
                            pg = bld.ps()
                            for (cb, m, c0) in cbs:
                                mm(pg[:, c0:c0 + m], buf[0:m, cb, k * 128:(k + 1) * 128], ident_b[0:m, 0:m])
                            cp(bld.alt(), xg[:, k, 0:CT], pg[:, 0:CT])
                        for fh in range(2):
                            wgb = wget(6 * e + 2 * fh, 6 * e + 2 * fh)
                            wub = wget(6 * e + 2 * fh + 1, 6 * e + 2 * fh)
                            for ff in range(4):
                                f = fh * 4 + ff
                                pa = bld.ps()
                                pu = bld.ps()
                                for k in range(KC):
                                    mm(pa[:, 0:CT], wgb[:, k, ff * 128:(ff + 1) * 128], xg[:, k, 0:CT], start=(k == 0), stop=(k == KC - 1))
                                for k in range(KC):
                                    mm(pu[:, 0:CT], wub[:, k, ff * 128:(ff + 1) * 128], xg[:, k, 0:CT], start=(k == 0), stop=(k == KC - 1))
                                s_ = sa[f % 2]
                                act(s_[:, 0:CT], pa[:, 0:CT], AF.Silu)
                                tt('dve', actT[:, f, 0:CT], s_[:, 0:CT], pu[:, 0:CT], ALU.mult)
                                if st_jobs:
                                    st_jobs.pop(0)()
                                if f == KC - 1:
                                    while st_jobs:
                                        st_jobs.pop(0)()
                        for dh in range(2):
                            wdb = wget(6 * e + 4 + dh, 6 * e + 4 + dh)
                            for (cb, m, c0) in cbs:
                                py = bld.ps()
                                for f in range(KC):
                                    mm(py[0:m, :], actT[:, f, c0:c0 + m], wdb[:, f, :], start=(f == 0), stop=(f == KC - 1))
                                act(ye[par][0:m, cb, dh * 512:(dh + 1) * 512], py[0:m, :], AF.Copy, scale=val_tok[0:m, cb, e:e + 1])
                        if par == 1:
                            for k in range(KC):
                                for (t0, n, s) in TILES[1:]:
                                    pz = bld.ps()
                                    i = 0
                                    for pp in range(2):
                                        for cb in range(2):
                                            mm(pz[:, 0:n], ye[pp][:, cb, k * 128:(k + 1) * 128], ST_lat[pp][:, cb, t0 - NCX:t0 - NCX + n],
                                               start=(i == 0), stop=(i == 3))
                                            i += 1
                                    stt('dve', xT[:, k, t0:t0 + n], pz[:, 0:n], mod_ap(l, 5, k, 0), xT[:, k, t0:t0 + n], ALU.mult, ALU.add)
                                if need_ctx:
                                    pz = bld.ps()
                                    for pp in range(2):
                                        mm(pz[:, 0:NCX], ye[pp][0:32, 2, k * 128:(k + 1) * 128], ST_ctx[pp][:], start=(pp == 0), stop=(pp == 1))
                                    stt('dve', xT[:, k, 0:NCX], pz[:, 0:NCX], mod_ap(l, 5, k, 1), xT[:, k, 0:NCX], ALU.mult, ALU.add)
                    P.barrier()

        def final_phase():
            with ExitStack() as ph:
                sq = [SB(ph, f"fn_sq{i}", [128, 512], BF16) for i in range(4)]
                rs = [SB(ph, f"fn_rs{i}", [128, 512], F32) for i in range(2)]
                ot = [SB(ph, f"fn_o{i}", [128, 512], F32) for i in range(3)]
                oc = 0
                for ti, (t0, n, s) in enumerate(TILES[1:]):
                    pss = bld.ps()
                    for k in range(KC):
                        q = sq[k % 4]
                        act(q[:, 0:n], xT[:, k, t0:t0 + n], AF.Square)
                        mm(pss[:, 0:n], ones_b[:], q[:, 0:n], start=(k == 0), stop=(k == KC - 1))
                    r = rs[ti % 2]
                    act(r[:, 0:n], pss[:, 0:n], AF.Ln, scale=1.0 / D, bias=EPS)
                    act(r[:, 0:n], r[:, 0:n], AF.Exp, scale=-0.5)
                    for k in range(KC):
                        o = ot[oc % 3]
                        oc += 1
                        stt('dve', o[:, 0:n], xT[:, k, t0:t0 + n], fing[:, k:k + 1], r[:, 0:n], ALU.mult, ALU.mult)
                        dma('sp', out_d[k * 128:(k + 1) * 128, t0 - NCX:t0 - NCX + n], o[:, 0:n])
                P.barrier()

        def dump():
            for k in range(KC):
                dma('sp', dbg_d[k * 128:(k + 1) * 128, :], xT[:, k, :])
            P.barrier()

        done = False
        for l in range(n_layers):
            need_ctx = l < DEPTH - 1
            if l % 2 == 0:
                lru_phase(l, need_ctx)
            else:
                attn_phase(l, need_ctx)
            if stop_after == (l, 'mix'):
                dump()
                done = True
                break
            moe_phase(l, need_ctx)
            if stop_after == (l, 'moe'):
                dump()
                done = True
                break
        if not done:
            final_phase()
            if stop_after is not None:
                dump()
        P.emit()
    return nc, P


def _fm(v):
    v = np.asarray(v, np.float32)
    lead = v.shape[:-1]
    r = v.reshape(lead + (8, 128))
    return np.ascontiguousarray(np.moveaxis(r, -1, 0))


def make_in_maps(inp):
    f32 = np.float32
    x = np.asarray(inp["x"], f32)
    ctx = np.asarray(inp["ctx"], f32)
    c = np.asarray(inp["c"], f32)
    c_ctx = np.asarray(inp["c_ctx"], f32)
    nb = x.shape[0]
    shared = {}
    shared["ada_w"] = np.ascontiguousarray(inp["ada_w"], f32)
    shared["ada_b"] = np.ascontiguousarray(np.asarray(inp["ada_b"], f32).reshape(4, 48, 128).transpose(2, 0, 1))
    shared["n1g"] = _fm(inp["norm1_g"])
    shared["n2g"] = _fm(inp["norm2_g"])
    shared["fing"] = _fm(inp["final_g"])
    shared["lru_w_in"] = np.ascontiguousarray(inp["lru_w_in"], f32)
    shared["lru_w_out"] = np.ascontiguousarray(inp["lru_w_out"], f32)
    cw = np.asarray(inp["lru_conv_w"], f32)
    shared["conv_w"] = np.ascontiguousarray(cw.reshape(2, 4, 8, 128).transpose(3, 0, 2, 1))
    shared["conv_b"] = _fm(inp["lru_conv_b"])
    shared["gate_w"] = np.ascontiguousarray(inp["lru_gate_w"], f32)
    gb = np.asarray(inp["lru_gate_b"], f32)
    shared["gate_b"] = np.ascontiguousarray(gb.reshape(2, 2, 8, 2, 128).transpose(4, 0, 1, 2, 3))
    shared["lam"] = _fm(inp["lru_lambda"])
    wqkv = np.asarray(inp["attn_w_qkv"], f32)
    shared["w_qkv"] = np.ascontiguousarray(wqkv)
    perm = np.concatenate([np.arange(32, 64), np.arange(0, 32), np.arange(96, 128), np.arange(64, 96)])
    cols = (np.arange(10)[:, None] * 128 + perm[None, :]).reshape(-1)
    shared["w_qkv_sw"] = np.ascontiguousarray(wqkv[:, :, cols])
    shared["sink"] = np.ascontiguousarray(np.broadcast_to(np.asarray(inp["attn_sink"], f32)[None], (128, 2, 8)))
    shared["w_o"] = np.ascontiguousarray(inp["attn_w_o"], f32)
    r = np.asarray(inp["moe_router"], f32)
    shared["router"] = np.ascontiguousarray(r.reshape(4, 8, 128, 16).transpose(2, 0, 1, 3))
    shared["moe_w_gate"] = np.ascontiguousarray(inp["moe_w_gate"], f32)
    shared["moe_w_up"] = np.ascontiguousarray(inp["moe_w_up"], f32)
    shared["moe_w_down"] = np.ascontiguousarray(inp["moe_w_down"], f32)
    shared["ident"] = np.eye(128, dtype=f32)
    half = 64
    freqs = (10000.0 ** (-np.arange(0, half, 2, dtype=np.float32) / half)).astype(f32)
    t = np.arange(NL)
    rows = (t // 64).astype(f32)
    colsp = (t % 64).astype(f32)
    rc = np.zeros((128, NL), f32)
    rs = np.zeros((128, NL), f32)
    for p in range(128):
        pos = rows if p < 64 else colsp
        jf = p % 32
        ang = (pos * freqs[jf]).astype(f32)
        rc[p] = np.cos(ang)
        sgn = -1.0 if (p % 64) < 32 else 1.0
        rs[p] = sgn * np.sin(ang)
    shared["rope_c"] = rc
    shared["rope_s"] = rs
    shared["iota_row"] = np.ascontiguousarray(np.broadcast_to(np.arange(NL, dtype=f32)[None], (128, NL)))
    shared["tokidx"] = (np.arange(128, dtype=f32)[:, None] + 128.0 * np.arange(16, dtype=f32)[None, :]).astype(f32)
    s_i = np.arange(128)[:, None]
    q_i = np.arange(128)[None, :]
    shared["mask_prev"] = (s_i >= q_i).astype(f32)
    shared["mask_next"] = (s_i <= q_i).astype(f32)
    shared["halfoff"] = np.concatenate([np.zeros((16, 1), f32), np.full((16, 1), 1024.0, f32)], axis=0)
    shared["pidx16"] = np.ascontiguousarray(np.broadcast_to(np.arange(16, dtype=f32)[:, None], (16, 128)))
    maps = []
    for b in range(nb):
        m = dict(shared)
        m["xT"] = np.ascontiguousarray(np.concatenate([ctx[b].T, x[b].T], axis=1))
        cc = np.stack([c[b].reshape(8, 128).T, c_ctx.reshape(8, 128).T], axis=-1)
        m["cc"] = np.ascontiguousarray(cc, dtype=f32)
        maps.append(m)
    return maps


_CACHE = {}


def kernel(**inputs):
    if "nc" not in _CACHE:
        _CACHE["nc"] = build_program()[0]
    nc = _CACHE["nc"]
    maps = make_in_maps(inputs)
    res = run_bass_kernel_spmd(nc, maps, core_ids=list(range(len(maps))))
    out = np.stack([np.ascontiguousarray(r["outT"].T) for r in res.results], axis=0)
    return out.astype(np.float32)
```

```python
import numpy as np
import concourse.bass as bass
import concourse.mybir as mybir

F32 = mybir.dt.float32
BF16 = mybir.dt.bfloat16
I32 = mybir.dt.int32
U32 = mybir.dt.uint32
AF = mybir.ActivationFunctionType
ALU = mybir.AluOpType
AX = mybir.AxisListType

ENGS = ['pe', 'act', 'dve', 'pool', 'sp']
NDSEM = 6
PE_DELAY_OPS = {'act': 1, 'dve': 4, 'pool': 4}


def _prod(xs):
    r = 1
    for v in xs:
        r *= int(v)
    return r


def region(ap):
    t = ap.tensor
    name = ap.name
    shape = tuple(t.shape)
    pairs = [(int(s), int(c)) for s, c in ap.ap]
    off = int(ap.offset)
    sp = str(ap.space)
    if 'DRAM' in sp.upper() or 'HBM' in sp.upper():
        lo = off
        hi = off
        for s, c in pairs:
            if s >= 0:
                hi += s * (c - 1)
            else:
                lo += s * (c - 1)
        return (name, 0, 1, lo, hi + 1)
    fsz = _prod(shape[1:])
    p0 = off // fsz
    rem = off % fsz
    ps, pc = pairs[0]
    p1 = p0 + (ps // fsz) * (pc - 1) + 1 if pc > 1 else p0 + 1
    lo = rem
    hi = rem
    for s, c in pairs[1:]:
        if s >= 0:
            hi += s * (c - 1)
        else:
            lo += s * (c - 1)
    return (name, p0, p1, lo, hi + 1)


def _overlap(a, b):
    return a[1] < b[2] and b[1] < a[2] and a[3] < b[4] and b[3] < a[4]


def _contains(a, b):
    return a[1] <= b[1] and b[2] <= a[2] and a[3] <= b[3] and b[4] <= a[4]


class Prog:
    def __init__(self, nc, stack):
        self.nc = nc
        self.ops = {e: [] for e in ENGS}
        self.cnt = {e: 0 for e in ENGS}
        self.sems = {}
        for e in ENGS:
            self.sems[('e', e)] = stack.enter_context(nc.semaphore(f"s_{e}"))
        self.dcnt = {}
        self.dnext = {}
        for q in ['sp', 'act', 'pool']:
            self.dnext[q] = 0
            for i in range(NDSEM):
                self.sems[('d', q, i)] = stack.enter_context(nc.semaphore(f"d_{q}{i}"))
                self.dcnt[(q, i)] = 0
        self.waited = {e: {} for e in ENGS}
        self.wr = {}
        self.rd = {}
        self.nwaits = 0
        self.flush = False
        self.pe_delay = True
        self.scratch = {}

    def _deps(self, reads, writes, token):
        deps = {}

        def add(k, v):
            if deps.get(k, 0) < v:
                deps[k] = v
        for ap in reads:
            r = region(ap)
            w = self.wr.setdefault(r[0], {})
            for (rg, sk), v in w.items():
                if _overlap(rg, r):
                    add(sk, v)
        for ap in writes:
            r = region(ap)
            w = self.wr.setdefault(r[0], {})
            d = self.rd.setdefault(r[0], {})
            for tab in (w, d):
                dead = []
                for (rg, sk), v in tab.items():
                    if _overlap(rg, r):
                        add(sk, v)
                        if _contains(r, rg):
                            dead.append((rg, sk))
                for k in dead:
                    del tab[k]
        for ap in reads:
            r = region(ap)
            self.rd.setdefault(r[0], {})[(r, token[0])] = token[1]
        for ap in writes:
            r = region(ap)
            self.wr.setdefault(r[0], {})[(r, token[0])] = token[1]
        return deps

    def _emit_waits(self, eng, deps):
        pe_wait = False
        for sk, v in deps.items():
            if eng == 'pe' and sk == ('e', 'pe'):
                continue
            if self.waited[eng].get(sk, 0) >= v:
                continue
            self.waited[eng][sk] = v
            self.ops[eng].append(('wait', sk, v))
            self.nwaits += 1
            if sk == ('e', 'pe'):
                pe_wait = True
        if pe_wait and self.pe_delay and eng in self.scratch:
            sc = self.scratch[eng]
            for _ in range(PE_DELAY_OPS[eng]):
                if eng == 'act':
                    self.ops[eng].append(('raw', lambda e: e.copy(sc[:], sc[:])))
                else:
                    self.ops[eng].append(('raw', lambda e: e.memset(sc[:], 0.0)))

    def op(self, eng, fn, reads=(), writes=()):
        fl = self.flush and eng in ('act', 'dve', 'pool') and eng in self.scratch
        token = (('e', eng), self.cnt[eng] + (2 if fl else 1))
        deps = self._deps(list(reads), list(writes), token)
        self._emit_waits(eng, deps)
        self.cnt[eng] += 1
        self.ops[eng].append(('op', fn, token[0]))
        if fl:
            sc = self.scratch[eng]
            self.cnt[eng] += 1
            if eng == 'act':
                self.ops[eng].append(('op', lambda e: e.copy(sc[:], sc[:]), token[0]))
            else:
                self.ops[eng].append(('op', lambda e: e.memset(sc[:], 0.0), token[0]))

    def dma(self, q, out, in_, **kw):
        i = self.dnext[q] % NDSEM
        self.dnext[q] += 1
        sk = ('d', q, i)
        prev = self.dcnt[(q, i)]
        self.dcnt[(q, i)] = prev + 16
        token = (sk, prev + 16)
        deps = self._deps([in_], [out], token)
        if prev > 0:
            if deps.get(sk, 0) < prev:
                deps[sk] = prev
        self._emit_waits(q, deps)
        self.ops[q].append(('dma', (out, in_, kw), sk))

    def dma_custom(self, q, out, in_, fn, extra_reads=()):
        i = self.dnext[q] % NDSEM
        self.dnext[q] += 1
        sk = ('d', q, i)
        prev = self.dcnt[(q, i)]
        self.dcnt[(q, i)] = prev + 16
        token = (sk, prev + 16)
        deps = self._deps([in_] + list(extra_reads), [out], token)
        if prev > 0:
            if deps.get(sk, 0) < prev:
                deps[sk] = prev
        self._emit_waits(q, deps)
        self.ops[q].append(('dmac', (out, in_, fn), sk))

    def barrier(self):
        for e in ENGS:
            deps = {}
            for e2 in ENGS:
                if e2 != e and self.cnt[e2] > 0:
                    deps[('e', e2)] = self.cnt[e2]
            if e != 'pe' and self.cnt[e] > 0:
                deps[('e', e)] = self.cnt[e]
            for (q, i), v in self.dcnt.items():
                if v > 0:
                    deps[('d', q, i)] = v
            self._emit_waits(e, deps)
        self.wr = {}
        self.rd = {}

    def emit(self):
        nc = self.nc
        sems = self.sems
        ops = self.ops

        def run(eng_name, eng):
            for item in ops[eng_name]:
                if item[0] == 'wait':
                    eng.wait_ge(sems[item[1]], item[2])
                elif item[0] == 'op':
                    ins = item[1](eng)
                    ins.then_inc(sems[item[2]], 1)
                elif item[0] == 'raw':
                    item[1](eng)
                elif item[0] == 'dmac':
                    out, in_, fn = item[1]
                    fn(eng, out, in_, {}).then_inc(sems[item[2]], 16)
                else:
                    out, in_, kw = item[1]
                    eng.dma_start(out=out, in_=in_, **kw).then_inc(sems[item[2]], 16)

        with nc.Block() as block:
            @block.tensor
            def _(e):
                run('pe', e)

            @block.scalar
            def _(e):
                run('act', e)

            @block.vector
            def _(e):
                run('dve', e)

            @block.gpsimd
            def _(e):
                run('pool', e)

            @block.sync
            def _(e):
                run('sp', e)

from contextlib import ExitStack
import os
MOE_CUT = int(os.environ.get('MOE_CUT', '99'))
MOE_E0 = int(os.environ.get('MOE_E0', '0'))
MOE_E1 = int(os.environ.get('MOE_E1', '16'))
from concourse.bass_utils import run_bass_kernel_spmd

D = 1024
KC = 8
NL = 2048
NCX = 256
T = NL + NCX
DEPTH = 4
NE = 16
CAPL = 256
CAPC = 32
EPS = 1e-6
QSCALE = 128 ** -0.5
TILES = [(0, 256, 1), (256, 512, 0), (768, 512, 0), (1280, 512, 0), (1792, 512, 0)]


class B:
    def __init__(self, nc, P, st):
        self.nc = nc
        self.P = P
        self.st = st
        self.psn = 0
        self.rr = 0

    def mm(self, out, lhsT, rhs, start=True, stop=True):
        self.P.op('pe', lambda e: e.matmul(out, lhsT, rhs, start=start, stop=stop),
                  reads=[lhsT, rhs], writes=[out])

    def tr(self, out, in_, ident):
        self.mm(out, in_, ident)

    def act(self, out, in_, func, bias=None, scale=None):
        kw = {}
        rd = [in_]
        if bias is not None:
            kw['bias'] = bias
            if not isinstance(bias, float):
                rd.append(bias)
        if scale is not None:
            kw['scale'] = scale
            if not isinstance(scale, float):
                rd.append(scale)
        self.P.op('act', lambda e: e.activation(out, in_, func, **kw), reads=rd, writes=[out])

    def tt(self, eng, out, in0, in1, op):
        self.P.op(eng, lambda e: e.tensor_tensor(out, in0, in1, op), reads=[in0, in1], writes=[out])

    def ts(self, eng, out, in0, s1, s2, op0, op1=None):
        rd = [in0]
        if not isinstance(s1, float):
            rd.append(s1)
        if s2 is not None and not isinstance(s2, float):
            rd.append(s2)
        if op1 is None:
            self.P.op(eng, lambda e: e.tensor_scalar(out, in0, s1, None, op0), reads=rd, writes=[out])
        else:
            self.P.op(eng, lambda e: e.tensor_scalar(out, in0, s1, s2, op0, op1), reads=rd, writes=[out])

    def stt(self, eng, out, in0, scalar, in1, op0, op1):
        rd = [in0, in1]
        if not isinstance(scalar, float):
            rd.append(scalar)
        self.P.op(eng, lambda e: e.scalar_tensor_tensor(out, in0, scalar, in1, op0, op1), reads=rd, writes=[out])

    def cp(self, eng, out, in_):
        if eng == 'act':
            self.P.op('act', lambda e: e.copy(out, in_), reads=[in_], writes=[out])
        else:
            self.P.op(eng, lambda e: e.tensor_copy(out, in_), reads=[in_], writes=[out])

    def memset(self, eng, out, val):
        self.P.op(eng, lambda e: e.memset(out, val), reads=[], writes=[out])

    def scan(self, out, a, b, init):
        rd = [a, b]
        if not isinstance(init, float):
            rd.append(init)
        self.P.op('dve', lambda e: e.tensor_tensor_scan(out, a, b, init, ALU.mult, ALU.add), reads=rd, writes=[out])

    def dma(self, q, out, in_):
        self.P.dma(q, out, in_)

    def ps(self):
        b = self.banks[self.psn % len(self.banks)]
        self.psn += 1
        return b

    def alt(self, engs=('act', 'dve')):
        self.rr += 1
        return engs[self.rr % len(engs)]


def build_program(stop_after=None, n_layers=DEPTH):
    nc = bass.Bass("TRN2", target_bir_lowering=False)
    dram = {}

    def din(name, shape, dt=F32):
        dram[name] = nc.dram_tensor(name, list(shape), dt, kind="ExternalInput").ap()
        return dram[name]

    xT_d = din("xT", [D, T])
    cc_d = din("cc", [128, KC, 2])
    ada_w_d = din("ada_w", [DEPTH, D, 6 * D])
    ada_b_d = din("ada_b", [128, DEPTH, 48])
    n1g_d = din("n1g", [128, DEPTH, KC])
    n2g_d = din("n2g", [128, DEPTH, KC])
    fing_d = din("fing", [128, KC])
    lwin_d = din("lru_w_in", [2, D, 2 * D])
    lwout_d = din("lru_w_out", [2, D, D])
    convw_d = din("conv_w", [128, 2, KC, 4])
    convb_d = din("conv_b", [128, 2, KC])
    gatew_d = din("gate_w", [2, 2, 8, 128, 256])
    gateb_d = din("gate_b", [128, 2, 2, 8, 2])
    lam_d = din("lam", [128, 2, 2, 8])
    wqkv_d = din("w_qkv", [2, D, 1536])
    wqsw_d = din("w_qkv_sw", [2, D, 1280])
    sink_d = din("sink", [128, 2, 8])
    wo_d = din("w_o", [2, D, D])
    router_d = din("router", [128, DEPTH, KC, NE])
    wg_d = din("moe_w_gate", [DEPTH, NE, D, D])
    wu_d = din("moe_w_up", [DEPTH, NE, D, D])
    wd_d = din("moe_w_down", [DEPTH, NE, D, D])
    ident_d = din("ident", [128, 128])
    ropec_d = din("rope_c", [128, NL])
    ropes_d = din("rope_s", [128, NL])
    iota_d = din("iota_row", [128, NL])
    tokidx_d = din("tokidx", [128, 16])
    mprev_d = din("mask_prev", [128, 128])
    mnext_d = din("mask_next", [128, 128])
    pidx_d = din("pidx16", [16, 128])
    halfoff_d = din("halfoff", [32, 1])
    out_d = nc.dram_tensor("outT", [D, NL], F32, kind="ExternalOutput").ap()
    scr_d = nc.dram_tensor("scr_tabs", [DEPTH, 2, 16, 288], F32).ap()
    scr2_d = nc.dram_tensor("scr_half", [DEPTH, 2, 32, CAPL], F32).ap()
    hscr_d = [nc.dram_tensor(f"scr_htok{i}", [T, D], BF16).ap() for i in range(DEPTH)]
    dbg_d = None
    if stop_after is not None:
        dbg_d = nc.dram_tensor("dbgx", [D, T], F32, kind="ExternalOutput").ap()
        dbg_aff = nc.dram_tensor("dbg_aff", [16, T], F32, kind="ExternalOutput").ap()
        dbg_idx = nc.dram_tensor("dbg_idx", [16, 288], F32, kind="ExternalOutput").ap()
        dbg_val = nc.dram_tensor("dbg_val", [16, 288], F32, kind="ExternalOutput").ap()
        dbg_it = nc.dram_tensor("dbg_it", [128, 3, NE], F32, kind="ExternalOutput").ap()
        dbg_vt = nc.dram_tensor("dbg_vt", [128, 3, NE], F32, kind="ExternalOutput").ap()

    with ExitStack() as st:
        P = Prog(nc, st)
        bld = B(nc, P, st)
        mm, act, tt, ts, stt, cp, dma = bld.mm, bld.act, bld.tt, bld.ts, bld.stt, bld.cp, bld.dma

        def SB(stack, name, shape, dt):
            return stack.enter_context(nc.sbuf_tensor(name, list(shape), dt))

        banks = [st.enter_context(nc.psum_tensor(f"ps{i}", [128, 512], F32)) for i in range(7)]
        psb = st.enter_context(nc.psum_tensor("psb", [128, 1024], BF16))
        bld.banks = banks
        for en in ('act', 'dve', 'pool'):
            P.scratch[en] = SB(st, f"flush_{en}", [128, 1], F32)
        P.flush = os.environ.get('FLUSH', '0') == '1'
        P.pe_delay = os.environ.get('PE_DELAY', '1') == '1'

        xT = SB(st, "xT_sb", [128, KC, T], F32)
        ident_f = SB(st, "ident_f", [128, 128], F32)
        ident_b = SB(st, "ident_b", [128, 128], BF16)
        ones_b = SB(st, "ones_b", [128, 128], BF16)
        mod = SB(st, "mod", [128, DEPTH, 48, 2], F32)
        gp1 = SB(st, "gp1", [128, DEPTH, KC, 2], F32)
        gp2 = SB(st, "gp2", [128, DEPTH, KC, 2], F32)
        n1g = SB(st, "n1g_sb", [128, DEPTH, KC], F32)
        n2g = SB(st, "n2g_sb", [128, DEPTH, KC], F32)
        fing = SB(st, "fing_sb", [128, KC], F32)
        ada_b = SB(st, "ada_b_sb", [128, DEPTH, 48], F32)
        convw = SB(st, "convw_sb", [128, 2, KC, 4], F32)
        convb = SB(st, "convb_sb", [128, 2, KC], F32)
        gateb = SB(st, "gateb_sb", [128, 2, 2, 8, 2], F32)
        gatebh = SB(st, "gatebh_sb", [128, 2, 2, 8, 2], F32)
        lam = SB(st, "lam_sb", [128, 2, 2, 8], F32)
        ls4 = SB(st, "ls4", [128, 2, 2, 8], F32)
        ls8 = SB(st, "ls8", [128, 2, 2, 8], F32)
        esink = SB(st, "esink", [128, 2, 8], F32)
        router_b = SB(st, "router_b", [128, DEPTH, KC, NE], BF16)
        tokidx = SB(st, "tokidx_sb", [128, 16], F32)
        ntok = SB(st, "ntok_sb", [128, 16], F32)
        pidx16 = SB(st, "pidx16_sb", [16, 128], F32)
        halfoff = SB(st, "halfoff_sb", [32, 1], F32)
        scc = SB(st, "scc", [128, KC, 2], BF16)

        for k in range(KC):
            dma('sp', xT[:, k, :], xT_d[k * 128:(k + 1) * 128, :])
        rstk = ExitStack()
        router_f = SB(rstk, "router_f", [128, DEPTH, KC, NE], F32)
        for sb_t, d_t in [(ident_f, ident_d), (ada_b, ada_b_d), (n1g, n1g_d), (n2g, n2g_d), (fing, fing_d),
                          (convw, convw_d), (convb, convb_d), (gateb, gateb_d), (lam, lam_d), (esink, sink_d),
                          (router_f, router_d), (tokidx, tokidx_d), (pidx16, pidx_d), (halfoff, halfoff_d)]:
            dma('sp', sb_t[:], d_t)
        cp('dve', ident_b[:], ident_f[:])
        cp('dve', router_b[:], router_f[:])
        bld.memset('dve', ones_b[:], 1.0)
        act(esink[:], esink[:], AF.Exp)
        act(lam[:], lam[:], AF.Exp, scale=-1.0)
        ts('dve', lam[:], lam[:], 1.0, None, ALU.add)
        act(lam[:], lam[:], AF.Ln)
        ts('dve', ls4[:], lam[:], -4.0, None, ALU.mult)
        ts('dve', ls8[:], lam[:], -8.0, None, ALU.mult)
        ts('dve', gatebh[:], gateb[:], 0.5, None, ALU.mult)
        ts('dve', ntok[:], tokidx[:], -1.0, None, ALU.mult)
        P.barrier()
        rstk.close()

        def adaln_mm(l, bufs, W):
            psm = bld.ps()
            npc = (6 * D) // W
            for pc in range(npc):
                wb = bufs[pc % 2]
                dma('pool', wb[:], ada_w_d[l, :, pc * W:(pc + 1) * W].rearrange("(c p) f -> p c f", p=128))
                for jj in range(W // 128):
                    j = pc * (W // 128) + jj
                    for k in range(KC):
                        mm(psm[:, 2 * j:2 * j + 2], wb[:, k, jj * 128:(jj + 1) * 128], scc[:, k, :],
                           start=(k == 0), stop=(k == KC - 1))
            return psm

        def adaln_fin(l, psm):
            tt('dve', mod[:, l], psm[:, 0:96].rearrange("p (j s) -> p j s", s=2),
               ada_b[:, l, :].unsqueeze(2).to_broadcast([128, 48, 2]), ALU.add)
            for (gp, ng, grp) in ((gp1, n1g, 1), (gp2, n2g, 4)):
                ts('dve', gp[:, l], mod[:, l, grp * 8:(grp + 1) * 8, :], 1.0, None, ALU.add)
                tt('dve', gp[:, l], gp[:, l], ng[:, l, :].unsqueeze(2).to_broadcast([128, KC, 2]), ALU.mult)

        with ExitStack() as ph:
            cc = SB(ph, "cc_sb", [128, KC, 2], F32)
            adw = [SB(ph, f"adw{i}", [128, KC, 1024], BF16) for i in range(2)]
            dma('sp', cc[:], cc_d)
            act(scc[:], cc[:], AF.Silu)
            adaln_fin(0, adaln_mm(0, adw, 1024))
            P.barrier()

        def mod_ap(l, grp, k, s):
            return mod[:, l, grp * 8 + k, s:s + 1]

        def norm_mod(ph, hT, l, which, tiles):
            gp = gp1 if which == 1 else gp2
            shg = 0 if which == 1 else 3
            sq = [SB(ph, f"nm_sq{i}_{l}_{which}", [128, 512], BF16) for i in range(4)]
            rs = [SB(ph, f"nm_rs{i}_{l}_{which}", [128, 512], F32) for i in range(2)]
            tmp = [SB(ph, f"nm_tmp{i}_{l}_{which}", [128, 512], F32) for i in range(4)]
            for ti, (t0, n, s) in enumerate(tiles):
                pss = bld.ps()
                for k in range(KC):
                    q = sq[k % 4]
                    if k % 2 == 0:
                        act(q[:, 0:n], xT[:, k, t0:t0 + n], AF.Square)
                    else:
                        tt('dve', q[:, 0:n], xT[:, k, t0:t0 + n], xT[:, k, t0:t0 + n], ALU.mult)
                    mm(pss[:, 0:n], ones_b[:], q[:, 0:n], start=(k == 0), stop=(k == KC - 1))
                r = rs[ti % 2]
                act(r[:, 0:n], pss[:, 0:n], AF.Ln, scale=1.0 / D, bias=EPS)
                act(r[:, 0:n], r[:, 0:n], AF.Exp, scale=-0.5)
                for k in range(KC):
                    tm = tmp[k % 4]
                    tt('dve', tm[:, 0:n], xT[:, k, t0:t0 + n], r[:, 0:n], ALU.mult)
                    act(hT[:, k, t0:t0 + n], tm[:, 0:n], AF.Identity,
                        scale=gp[:, l, k, s:s + 1], bias=mod_ap(l, shg, k, s))

        def load_wo(ph, name, w_dram):
            wo = SB(ph, name, [128, KC, D], BF16)
            for h in range(2):
                dma('pool', wo[:, :, h * 512:(h + 1) * 512],
                    w_dram[:, h * 512:(h + 1) * 512].rearrange("(k p) f -> p k f", p=128))
            return wo

        def out_proj(ph, name, w_dram, mT_, l, ggrp, tiles, wo=None):
            if wo is None:
                wo = load_wo(ph, name, w_dram)
            for (t0, n, s) in tiles:
                for dm in range(KC):
                    po = bld.ps()
                    for c in range(KC):
                        mm(po[:, 0:n], wo[:, c, dm * 128:(dm + 1) * 128], mT_[:, c, t0:t0 + n],
                           start=(c == 0), stop=(c == KC - 1))
                    stt('dve', xT[:, dm, t0:t0 + n], po[:, 0:n], mod_ap(l, ggrp, dm, s), xT[:, dm, t0:t0 + n],
                        ALU.mult, ALU.add)

        def lru_phase(l, need_ctx):
            j = l // 2
            with ExitStack() as ph:
                hT = SB(ph, f"hT_l{l}", [128, KC, T], BF16)
                mT = SB(ph, f"mT_l{l}", [128, KC, T], BF16)
                with ExitStack() as nt:
                    norm_mod(nt, hT, l, 1, TILES)
                    P.barrier()
                with ExitStack() as lt:
                    ub = SB(lt, f"ub{l}", [128, T], F32)
                    xb = SB(lt, f"xb{l}", [128, T], F32)
                    xbb = SB(lt, f"xbb{l}", [128, T], BF16)
                    winA = SB(lt, f"winA{l}", [128, KC, 128], BF16)
                    winB = SB(lt, f"winB{l}", [128, KC, 128], BF16)
                    gw = [SB(lt, f"gw{l}_{i}", [128, 256], BF16) for i in range(4)]
                    tr_ = [SB(lt, f"tr{l}_{i}", [128, 512], F32) for i in range(4)]
                    ti_ = [SB(lt, f"ti{l}_{i}", [128, 512], F32) for i in range(4)]
                    tb_ = [SB(lt, f"tb{l}_{i}", [128, 512], F32) for i in range(4)]
                    th_ = [SB(lt, f"th{l}_{i}", [128, 512], F32) for i in range(2)]
                    carry = SB(lt, f"carry{l}", [128, 2], F32)
                    for d in range(2):
                        dma('pool', gw[d][:], gatew_d[j, d, 0])
                    orders = [TILES, [TILES[0]] + TILES[:0:-1]]
                    first_dir = {0: 0, 256: 0, 768: 0, 1280: 1, 1792: 1}

                    def ldA(c):
                        dma('pool', winA[:], lwin_d[j, :, c * 128:(c + 1) * 128].rearrange("(k p) f -> p k f", p=128))

                    def ldB(c):
                        dma('pool', winB[:], lwin_d[j, :, D + c * 128:D + (c + 1) * 128].rearrange("(k p) f -> p k f", p=128))
                    ldB(0)
                    ldA(0)
                    for c in range(KC):
                        if c + 1 < KC:
                            for d in range(2):
                                dma('pool', gw[((c + 1) % 2) * 2 + d][:], gatew_d[j, d, c + 1])
                        for (t0, n, s) in TILES:
                            pu = bld.ps()
                            for k in range(KC):
                                mm(pu[:, 0:n], winB[:, k, :], hT[:, k, t0:t0 + n], start=(k == 0), stop=(k == KC - 1))
                            cp(bld.alt(), ub[:, t0:t0 + n], pu[:, 0:n])
                        if c + 1 < KC:
                            ldB(c + 1)
                        for (t0, n, s) in TILES:
                            s0, sn = (0, NCX) if s == 1 else (NCX, NL)
                            ts('dve', xb[:, t0:t0 + n], ub[:, t0:t0 + n], convw[:, j, c, 1:2], convb[:, j, c:c + 1], ALU.mult, ALU.add)
                            for (o, kk) in ((-1, 0), (1, 2), (2, 3)):
                                lo = max(t0, s0 - o)
                                hi = min(t0 + n, s0 + sn - o)
                                stt('dve', xb[:, lo:hi], ub[:, lo + o:hi + o], convw[:, j, c, kk:kk + 1], xb[:, lo:hi], ALU.mult, ALU.add)
                            cp('pool', xbb[:, t0:t0 + n], xb[:, t0:t0 + n])
                        for (ga, gb_) in ((0, 1), (1, 3), (3, 5)):
                            for d in range(2):
                                g = gw[(c % 2) * 2 + d]
                                for gi, (t0, n, s) in enumerate(orders[d][ga:gb_]):
                                    slot = d * 2 + gi
                                    pr = bld.ps()
                                    pi = bld.ps()
                                    mm(pr[:, 0:n], g[:, 0:128], xbb[:, t0:t0 + n])
                                    mm(pi[:, 0:n], g[:, 128:256], xbb[:, t0:t0 + n])
                                    r_ = tr_[slot]
                                    i_ = ti_[slot]
                                    b_ = tb_[slot]
                                    act(r_[:, 0:n], pr[:, 0:n], AF.Tanh, scale=0.5, bias=gatebh[:, j, d, c, 0:1])
                                    act(i_[:, 0:n], pi[:, 0:n], AF.Tanh, scale=0.5, bias=gatebh[:, j, d, c, 1:2])
                                    act(b_[:, 0:n], r_[:, 0:n], AF.Exp, scale=ls8[:, j, d, c:c + 1], bias=ls8[:, j, d, c:c + 1])
                                    act(r_[:, 0:n], r_[:, 0:n], AF.Exp, scale=ls4[:, j, d, c:c + 1], bias=ls4[:, j, d, c:c + 1])
                                    ts('dve', b_[:, 0:n], b_[:, 0:n], 1.0, None, ALU.min)
                            for d in range(2):
                                for gi, (t0, n, s) in enumerate(orders[d][ga:gb_]):
                                    slot = d * 2 + gi
                                    r_ = tr_[slot]
                                    i_ = ti_[slot]
                                    b_ = tb_[slot]
                                    act(b_[:, 0:n], b_[:, 0:n], AF.Sqrt, scale=-0.25, bias=0.25)
                                    stt('dve', i_[:, 0:n], i_[:, 0:n], 1.0, xb[:, t0:t0 + n], ALU.add, ALU.mult)
                                    tt('dve', b_[:, 0:n], b_[:, 0:n], i_[:, 0:n], ALU.mult)
                                    init = 0.0 if ga == 0 else carry[:, d:d + 1]
                                    first = first_dir[t0] == d
                                    dst = ub[:, t0:t0 + n] if first else th_[gi][:, 0:n]
                                    if d == 0:
                                        bld.scan(dst, r_[:, 0:n], b_[:, 0:n], init)
                                        cp('dve', carry[:, 0:1], dst[:, n - 1:n])
                                    else:
                                        bld.scan(dst[:, ::-1], r_[:, n - 1::-1], b_[:, n - 1::-1], init)
                                        cp('dve', carry[:, 1:2], dst[:, 0:1])
                                    if not first:
                                        tt('pool', ub[:, t0:t0 + n], ub[:, t0:t0 + n], dst, ALU.add)
                        for ti, (t0, n, s) in enumerate(TILES):
                            if s == 1 and not need_ctx:
                                continue
                            pg = bld.ps()
                            for k in range(KC):
                                mm(pg[:, 0:n], winA[:, k, :], hT[:, k, t0:t0 + n], start=(k == 0), stop=(k == KC - 1))
                            y_ = th_[ti % 2]
                            act(y_[:, 0:n], pg[:, 0:n], AF.Gelu_apprx_tanh)
                            tt('dve', mT[:, c, t0:t0 + n], y_[:, 0:n], ub[:, t0:t0 + n], ALU.mult)
                        if c + 1 < KC:
                            ldA(c + 1)
                    P.barrier()
                with ExitStack() as ot:
                    tiles = TILES if need_ctx else TILES[1:]
                    out_proj(ot, f"lwo{l}", lwout_d[j], mT, l, 2, tiles)
                    P.barrier()

        def attn_phase(l, need_ctx):
            j = l // 2
            with ExitStack() as ph:
                hT = SB(ph, f"hT_l{l}", [128, KC, T], BF16)
                qT = SB(ph, f"qT_l{l}", [128, 8, T], BF16)
                kT = SB(ph, f"kT_l{l}", [128, 2, T], BF16)
                V = SB(ph, f"V_l{l}", [128, 18, 256], BF16)
                with ExitStack() as nt:
                    norm_mod(nt, hT, l, 1, TILES)
                    P.barrier()
                with ExitStack() as qt:
                    rc = SB(qt, f"rc{l}", [128, NL], F32)
                    rs_ = SB(qt, f"rs{l}", [128, NL], F32)
                    dma('sp', rc[:], ropec_d)
                    dma('sp', rs_[:], ropes_d)
                    wq = [SB(qt, f"wq{l}_{i}", [128, KC, 128], BF16) for i in range(2)]
                    ws = [SB(qt, f"ws{l}_{i}", [128, KC, 128], BF16) for i in range(2)]
                    wv = SB(qt, f"wv{l}", [128, KC, 256], BF16)
                    t1 = [SB(qt, f"t1{l}_{i}", [128, 512], F32) for i in range(2)]
                    t2 = [SB(qt, f"t2{l}_{i}", [128, 512], F32) for i in range(2)]

                    def ldh(hh):
                        dma('pool', wq[hh % 2][:], wqkv_d[j, :, hh * 128:(hh + 1) * 128].rearrange("(k p) f -> p k f", p=128))
                        dma('pool', ws[hh % 2][:], wqsw_d[j, :, hh * 128:(hh + 1) * 128].rearrange("(k p) f -> p k f", p=128))
                    ldh(0)
                    dma('pool', wv[:], wqkv_d[j, :, 1280:1536].rearrange("(k p) f -> p k f", p=128))
                    for hh in range(10):
                        a = wq[hh % 2]
                        b = ws[hh % 2]
                        if hh + 1 < 10:
                            ldh(hh + 1)
                        dst = qT[:, hh, :] if hh < 8 else kT[:, hh - 8, :]
                        for ti, (t0, n, s) in enumerate(TILES):
                            if s == 1 and hh < 8 and not need_ctx:
                                continue
                            p1 = bld.ps()
                            for k in range(KC):
                                mm(p1[:, 0:n], a[:, k, :], hT[:, k, t0:t0 + n], start=(k == 0), stop=(k == KC - 1))
                            if s == 1:
                                cp('act', dst[:, t0:t0 + n], p1[:, 0:n])
                                continue
                            p2 = bld.ps()
                            for k in range(KC):
                                mm(p2[:, 0:n], b[:, k, :], hT[:, k, t0:t0 + n], start=(k == 0), stop=(k == KC - 1))
                            a1 = t1[ti % 2]
                            a2 = t2[ti % 2]
                            tt('dve', a1[:, 0:n], p1[:, 0:n], rc[:, t0 - NCX:t0 - NCX + n], ALU.mult)
                            tt('dve', a2[:, 0:n], p2[:, 0:n], rs_[:, t0 - NCX:t0 - NCX + n], ALU.mult)
                            tt('pool', dst[:, t0:t0 + n], a1[:, 0:n], a2[:, 0:n], ALU.add)
                    for blk in range(18):
                        pv = bld.ps()
                        for k in range(KC):
                            mm(pv[:, 0:256], hT[:, k, blk * 128:(blk + 1) * 128], wv[:, k, :], start=(k == 0), stop=(k == KC - 1))
                        cp(bld.alt(), V[:, blk, :], pv[:, 0:256])
                    P.barrier()
                with ExitStack() as at:
                    oT = hT
                    wo_pre = load_wo(at, f"awo{l}", wo_d[j])
                    mprev = SB(at, f"mprev{l}", [128, 128], BF16)
                    mnext = SB(at, f"mnext{l}", [128, 128], BF16)
                    dma('pool', mprev[:], mprev_d)
                    dma('pool', mnext[:], mnext_d)
                    ex = [SB(at, f"ex{l}_{i}", [128, 512], BF16) for i in range(6)]
                    dn = [SB(at, f"dn{l}_{i}", [128, 512], F32) for i in range(2)]
                    exc = 0
                    qblocks = [(b, True) for b in range(2, 18)]
                    if need_ctx:
                        qblocks += [(0, False), (1, False)]
                    for qi, (qb, is_lat) in enumerate(qblocks):
                        q0 = qb * 128
                        for kvh in range(2):
                            keys = [(0, None), (1, None)]
                            if is_lat:
                                if qb > 2:
                                    keys.append((qb - 1, mprev))
                                keys.append((qb, None))
                                if qb < 17:
                                    keys.append((qb + 1, mnext))
                            qsl = qT[:, kvh * 4:(kvh + 1) * 4, q0:q0 + 128]
                            po = bld.ps()
                            pd = bld.ps()
                            es = []
                            for (kb, msk) in keys:
                                psc = bld.ps()
                                mm(psc[:, :].rearrange("p (g q) -> p g q", g=4), kT[:, kvh, kb * 128:(kb + 1) * 128], qsl)
                                e_ = ex[exc % 6]
                                exc += 1
                                act(e_[:], psc[:], AF.Exp, scale=QSCALE)
                                if msk is not None:
                                    e3 = e_[:, :].rearrange("p (g q) -> p g q", g=4)
                                    tt('pool', e3, e3, msk[:, :].unsqueeze(1).to_broadcast([128, 4, 128]), ALU.mult)
                                es.append((kb, e_))
                            for i, (kb, e_) in enumerate(es):
                                mm(po[:], V[:, kb, kvh * 128:(kvh + 1) * 128], e_[:], start=(i == 0), stop=(i == len(es) - 1))
                            for i, (kb, e_) in enumerate(es):
                                mm(pd[:], ones_b[:], e_[:], start=(i == 0), stop=(i == len(es) - 1))
                            d_ = dn[(qi * 2 + kvh) % 2]
                            tt('dve', d_[:, :].rearrange("p (g q) -> p g q", g=4), pd[:, :].rearrange("p (g q) -> p g q", g=4),
                               esink[:, j, kvh * 4:(kvh + 1) * 4].unsqueeze(2).to_broadcast([128, 4, 128]), ALU.add)
                            P.op('dve', (lambda dd: lambda e: e.reciprocal(dd[:], dd[:]))(d_), reads=[d_[:]], writes=[d_[:]])
                            tt('dve', oT[:, kvh * 4:(kvh + 1) * 4, q0:q0 + 128], po[:, :].rearrange("p (g q) -> p g q", g=4),
                               d_[:, :].rearrange("p (g q) -> p g q", g=4), ALU.mult)
                    P.barrier()
                    tiles = TILES if need_ctx else TILES[1:]
                    out_proj(at, f"awo{l}", wo_d[j], oT, l, 2, tiles, wo=wo_pre)
                    P.barrier()

        def moe_phase(l, need_ctx):
            tiles = TILES if need_ctx else TILES[1:]
            CT = CAPL + CAPC if need_ctx else CAPL
            cbs = [(0, 128, 0), (1, 128, 128)] + ([(2, 32, 256)] if need_ctx else [])
            with ExitStack() as ph:
                idx_i = SB(ph, f"idxi{l}", [128, 3, NE], I32)
                val_tok = SB(ph, f"valtok{l}", [128, 3, NE], F32)
                nbias = SB(ph, f"nbias{l}", [128, 3, NE, 4], F32)
                iota_row = SB(ph, f"iota{l}", [128, 512], F32)
                dma('sp', iota_row[:], iota_d[:, 0:512])
                NW = 6
                wring = [SB(ph, f"wr{l}_{i}", [128, KC, 512], BF16) for i in range(NW)]
                pieces = []
                for e in range(NE):
                    for fh in range(2):
                        pieces.append(wg_d[l, e, :, fh * 512:(fh + 1) * 512])
                        pieces.append(wu_d[l, e, :, fh * 512:(fh + 1) * 512])
                    for dh in range(2):
                        pieces.append(wd_d[l, e, :, dh * 512:(dh + 1) * 512])
                issued = [6 * MOE_E0]

                def ensure(i):
                    while issued[0] < min(len(pieces), i + NW):
                        pi_ = issued[0]
                        dma('pool', wring[pi_ % NW][:], pieces[pi_].rearrange("(k p) f -> p k f", p=128))
                        issued[0] += 1

                ensure(6 * MOE_E0)
                with ExitStack() as rt:
                    hT = SB(rt, f"hTm{l}", [128, KC, T], BF16)
                    htok = [SB(rt, f"htok{l}_{i}", [128, D], BF16) for i in range(3)]
                    with ExitStack() as nt:
                        norm_mod(nt, hT, l, 2, tiles)
                        P.barrier()
                    aff = SB(rt, f"aff{l}", [128, 18, NE], F32)
                    mx = SB(rt, f"mx{l}", [128, 18], F32)
                    affC = SB(rt, f"affC{l}", [16, NCX], F32)
                    affL = SB(rt, f"affL{l}", [32, 1024], F32)
                    valsL = SB(rt, f"valsL{l}", [32, CAPL], F32)
                    idxL_u = SB(rt, f"idxLu{l}", [32, CAPL], U32)
                    idxL_f = SB(rt, f"idxLf{l}", [32, CAPL], F32)
                    Bv = SB(rt, f"Bv{l}", [16, CAPL], F32)
                    Bi = SB(rt, f"Bi{l}", [16, CAPL], F32)
                    sel = SB(rt, f"sel{l}", [16, CAPL], F32)
                    dd = SB(rt, f"dd{l}", [16, CAPL], F32)
                    vals = SB(rt, f"vals{l}", [16, 288], F32)
                    idx_u = SB(rt, f"idxu{l}", [16, 288], U32)
                    idx_f = SB(rt, f"idxf{l}", [16, 288], F32)
                    idxT = SB(rt, f"idxT{l}", [128, 3, NE], F32)
                    blks = list(range(18)) if need_ctx else list(range(2, 18))
                    pl = bld.ps()

                    def posb(b):
                        if b < 2:
                            return b
                        lb = b - 2
                        return 2 + (lb % 8) * 2 + lb // 8
                    for b in blks:
                        pb_ = posb(b)
                        for k in range(KC):
                            mm(pl[:, pb_ * NE:(pb_ + 1) * NE], hT[:, k, b * 128:(b + 1) * 128], router_b[:, l, k, :],
                               start=(k == 0), stop=(k == KC - 1))
                    b0, nb = blks[0], len(blks)
                    pl3 = pl[:, b0 * NE:(b0 + nb) * NE].rearrange("p (b e) -> p b e", e=NE)
                    a3 = aff[:, b0:b0 + nb, :]
                    P.op('dve', lambda e: e.tensor_reduce(mx[:, b0:b0 + nb], pl3, AX.X, ALU.max), reads=[pl3], writes=[mx[:, b0:b0 + nb]])
                    tt('dve', a3, pl3, mx[:, b0:b0 + nb].unsqueeze(2).to_broadcast([128, nb, NE]), ALU.subtract)
                    act(a3, a3, AF.Exp)
                    P.op('dve', lambda e: e.tensor_reduce(mx[:, b0:b0 + nb], a3, AX.X, ALU.add), reads=[a3], writes=[mx[:, b0:b0 + nb]])
                    P.op('dve', lambda e: e.reciprocal(mx[:, b0:b0 + nb], mx[:, b0:b0 + nb]), reads=[mx[:, b0:b0 + nb]], writes=[mx[:, b0:b0 + nb]])
                    tt('dve', a3, a3, mx[:, b0:b0 + nb].unsqueeze(2).to_broadcast([128, nb, NE]), ALU.mult)
                    if need_ctx:
                        pt = bld.ps()
                        for b in range(2):
                            bld.tr(pt[0:16, b * 128:(b + 1) * 128], aff[:, b, :], ident_f[:])
                        cp('act', affC[:, 0:NCX], pt[0:16, 0:NCX])
                    for g in range(2):
                        pt = bld.ps()
                        for bb in range(4):
                            b = g * 4 + bb
                            bld.tr(pt[0:32, bb * 128:(bb + 1) * 128], aff[:, 2 + 2 * b:4 + 2 * b, :].rearrange("p a e -> p (a e)"), ident_f[:])
                        cp('act', affL[:, g * 512:(g + 1) * 512], pt[0:32, :])
                    for b in blks:
                        for half in range(2):
                            pt = bld.ps()
                            for kk in range(4):
                                k = half * 4 + kk
                                bld.tr(pt[:, kk * 128:(kk + 1) * 128], hT[:, k, b * 128:(b + 1) * 128], ident_b[:])
                            cp('act', htok[b % 3][:, half * 512:(half + 1) * 512], pt[:])
                        dma('sp', hscr_d[l][b * 128:(b + 1) * 128, :], htok[b % 3][:])
                    ada_psm = None
                    if l + 1 < n_layers:
                        adw2 = [SB(rt, f"adw2_{l}_{i}", [128, KC, 512], BF16) for i in range(2)]
                        ada_psm = adaln_mm(l + 1, adw2, 512)
                    def topk(w, vout, iout, cap):
                        for it in range(cap // 8):
                            v8 = vout[:, it * 8:it * 8 + 8]
                            i8 = iout[:, it * 8:it * 8 + 8]
                            P.op('dve', (lambda v8, w: lambda e: e.max(out=v8, in_=w))(v8, w), reads=[w], writes=[v8])
                            P.op('dve', (lambda v8, i8, w: lambda e: e.max_index(out=i8, in_max=v8, in_values=w))(v8, i8, w), reads=[w, v8], writes=[i8])
                            P.op('dve', (lambda v8, w: lambda e: e.match_replace(out=w, in_to_replace=v8, in_values=w, imm_value=0.0))(v8, w), reads=[w, v8], writes=[w])
                    topk(affL[:], valsL[:], idxL_u[:], CAPL)
                    if need_ctx:
                        topk(affC[:], vals[:, CAPL:CAPL + CAPC], idx_u[:, CAPL:CAPL + CAPC], CAPC)
                        cp('dve', idx_f[:, CAPL:CAPL + CAPC], idx_u[:, CAPL:CAPL + CAPC])
                    cp('dve', idxL_f[:], idxL_u[:])
                    ts('dve', idxL_f[:], idxL_f[:], halfoff[:, 0:1], None, ALU.add)
                    dma('sp', scr2_d[l, 0], valsL[:])
                    dma('sp', scr2_d[l, 1], idxL_f[:])
                    dma('sp', Bv[:], scr2_d[l, 0, 16:32, :])
                    dma('sp', Bi[:], scr2_d[l, 1, 16:32, :])
                    tt('dve', sel[:], valsL[0:16, :], Bv[:, ::-1], ALU.is_gt)
                    for (A_, B_, out_) in ((valsL[0:16, :], Bv, vals[:, 0:CAPL]), (idxL_f[0:16, :], Bi, idx_f[:, 0:CAPL])):
                        tt('dve', dd[:], A_, B_[:, ::-1], ALU.subtract)
                        tt('dve', dd[:], dd[:], sel[:], ALU.mult)
                        tt('dve', out_, dd[:], B_[:, ::-1], ALU.add)
                    if not need_ctx:
                        bld.memset('dve', idx_f[:, CAPL:288], 0.0)
                        bld.memset('dve', vals[:, CAPL:288], 0.0)
                    dma('sp', scr_d[l, 0], idx_f[:])
                    dma('sp', scr_d[l, 1], vals[:])
                    for (cb, m, c0) in cbs:
                        P.dma('sp', idxT[0:m, cb, :], scr_d[l, 0, :, c0:c0 + m].rearrange("e c -> c e"), allow_slow_non_contiguous=True)
                        P.dma('sp', val_tok[0:m, cb, :], scr_d[l, 1, :, c0:c0 + m].rearrange("e c -> c e"), allow_slow_non_contiguous=True)
                    for (cb, m, c0) in cbs:
                        if cb < 2:
                            ts('dve', nbias[0:m, cb, :, 0], idxT[0:m, cb, :], float(NCX), None, ALU.add)
                            cp('dve', idx_i[0:m, cb, :], nbias[0:m, cb, :, 0])
                            for q4 in range(4):
                                ts('dve', nbias[0:m, cb, :, q4], idxT[0:m, cb, :], -1.0, float(q4 * 512), ALU.mult, ALU.add)
                        else:
                            cp('dve', idx_i[0:m, cb, :], idxT[0:m, cb, :])
                            ts('dve', nbias[0:m, cb, :, 0], idxT[0:m, cb, :], -1.0, None, ALU.mult)
                    if ada_psm is not None:
                        adaln_fin(l + 1, ada_psm)
                    P.barrier()
                with ExitStack() as et:
                    xgt = [SB(et, f"xgt{l}_{i}", [128, 3, D], BF16) for i in range(2)]
                    dtmp = [SB(et, f"dtmp{l}_{i}", [128, 512], F32) for i in range(3)]
                    xg = SB(et, f"xg{l}", [128, KC, 288], BF16)
                    sa = [SB(et, f"sa{l}_{i}", [128, 288], F32) for i in range(2)]
                    actT = SB(et, f"actT{l}", [128, KC, 288], BF16)
                    ye = [SB(et, f"ye{l}_{i}", [128, 3, D], BF16) for i in range(2)]
                    ST_lat = [SB(et, f"STl{l}_{i}", [128, 2, NL], BF16) for i in range(2)]
                    ST_ctx = [SB(et, f"STc{l}_{i}", [32, NCX], BF16) for i in range(2)]

                    def wget(i, base):
                        ensure(base)
                        assert i < issued[0]
                        return wring[i % NW]

                    def gather(e):
                        buf = xgt[e % 2]
                        for (cb, m, c0) in cbs:
                            def mk(cb, m, e):
                                return lambda en, out, in_, kw: en.indirect_dma_start(
                                    out=out, out_offset=None, in_=in_,
                                    in_offset=bass.IndirectOffsetOnAxis(ap=idx_i[0:m, cb, e:e + 1], axis=0))
                            P.dma_custom('pool', buf[0:m, cb, :], hscr_d[l][:, :], mk(cb, m, e), extra_reads=[idx_i[0:m, cb, e:e + 1]])

                    gather(MOE_E0)
                    for e in range(MOE_E0, MOE_E1):
                        par = e % 2
                        if e + 1 < MOE_E1:
                            gather(e + 1)
                        ensure(6 * e)
                        st_jobs = []
                        for cb in range(2):
                            for q4 in range(4):
                                def job(cb=cb, q4=q4, e=e, par=par):
                                    dt_ = dtmp[(cb * 4 + q4) % 3]
                                    act(dt_[:], iota_row[:], AF.Abs, bias=nbias[:, cb, e, q4:q4 + 1])
                                    act(ST_lat[par][:, cb, q4 * 512:(q4 + 1) * 512], dt_[:], AF.Relu, scale=-1.0, bias=1.0)
                                st_jobs.append(job)
                        if need_ctx:
                            def jobc(e=e, par=par):
                                dt_ = dtmp[2]
                                act(dt_[0:32, 0:NCX], iota_row[0:32, 0:NCX], AF.Abs, bias=nbias[0:32, 2, e, 0:1])
                                act(ST_ctx[par][:], dt_[0:32, 0:NCX], AF.Relu, scale=-1.0, bias=1.0)
                            st_jobs.append(jobc)
                        buf = xgt[par]
                        for k in range(KC):
                            pg = bld.ps()
                            for (cb, m, c0) in cbs:
                                mm(pg[:, c0:c0 + m], buf[0:m, cb, k * 128:(k + 1) * 128], ident_b[0:m, 0:m])
                            cp(bld.alt(), xg[:, k, 0:CT], pg[:, 0:CT])
                        for fh in range(2):
                            wgb = wget(6 * e + 2 * fh, 6 * e + 2 * fh)
                            wub = wget(6 * e + 2 * fh + 1, 6 * e + 2 * fh)
                            for ff in range(4):
                                f = fh * 4 + ff
                                pa = bld.ps()
                                pu = bld.ps()
                                for k in range(KC):
                                    mm(pa[:, 0:CT], wgb[:, k, ff * 128:(ff + 1) * 128], xg[:, k, 0:CT], start=(k == 0), stop=(k == KC - 1))
                                for k in range(KC):
                                    mm(pu[:, 0:CT], wub[:, k, ff * 128:(ff + 1) * 128], xg[:, k, 0:CT], start=(k == 0), stop=(k == KC - 1))
                                s_ = sa[f % 2]
                                act(s_[:, 0:CT], pa[:, 0:CT], AF.Silu)
                                tt('dve', actT[:, f, 0:CT], s_[:, 0:CT], pu[:, 0:CT], ALU.mult)
                                if st_jobs:
                                    st_jobs.pop(0)()
                                if f == KC - 1:
                                    while st_jobs:
                                        st_jobs.pop(0)()
                        for dh in range(2):
                            wdb = wget(6 * e + 4 + dh, 6 * e + 4 + dh)
                            for (cb, m, c0) in cbs:
                                py = bld.ps()
                                for f in range(KC):
                                    mm(py[0:m, :], actT[:, f, c0:c0 + m], wdb[:, f, :], start=(f == 0), stop=(f == KC - 1))
                                act(ye[par][0:m, cb, dh * 512:(dh + 1) * 512], py[0:m, :], AF.Copy, scale=val_tok[0:m, cb, e:e + 1])
                        if par == 1:
                            for k in range(KC):
                                for (t0, n, s) in TILES[1:]:
                                    pz = bld.ps()
                                    i = 0
                                    for pp in range(2):
                                        for cb in range(2):
                                            mm(pz[:, 0:n], ye[pp][:, cb, k * 128:(k + 1) * 128], ST_lat[pp][:, cb, t0 - NCX:t0 - NCX + n],
                                               start=(i == 0), stop=(i == 3))
                                            i += 1
                                    stt('dve', xT[:, k, t0:t0 + n], pz[:, 0:n], mod_ap(l, 5, k, 0), xT[:, k, t0:t0 + n], ALU.mult, ALU.add)
                                if need_ctx:
                                    pz = bld.ps()
                                    for pp in range(2):
                                        mm(pz[:, 0:NCX], ye[pp][0:32, 2, k * 128:(k + 1) * 128], ST_ctx[pp][:], start=(pp == 0), stop=(pp == 1))
                                    stt('dve', xT[:, k, 0:NCX], pz[:, 0:NCX], mod_ap(l, 5, k, 1), xT[:, k, 0:NCX], ALU.mult, ALU.add)
                    P.barrier()

        def final_phase():
            with ExitStack() as ph:
                sq = [SB(ph, f"fn_sq{i}", [128, 512], BF16) for i in range(4)]
                rs = [SB(ph, f"fn_rs{i}", [128, 512], F32) for i in range(2)]
                ot = [SB(ph, f"fn_o{i}", [128, 512], F32) for i in range(3)]
                oc = 0
                for ti, (t0, n, s) in enumerate(TILES[1:]):
                    pss = bld.ps()
                    for k in range(KC):
                        q = sq[k % 4]
                        act(q[:, 0:n], xT[:, k, t0:t0 + n], AF.Square)
                        mm(pss[:, 0:n], ones_b[:], q[:, 0:n], start=(k == 0), stop=(k == KC - 1))
                    r = rs[ti % 2]
                    act(r[:, 0:n], pss[:, 0:n], AF.Ln, scale=1.0 / D, bias=EPS)
                    act(r[:, 0:n], r[:, 0:n], AF.Exp, scale=-0.5)
                    for k in range(KC):
                        o = ot[oc % 3]
                        oc += 1
                        stt('dve', o[:, 0:n], xT[:, k, t0:t0 + n], fing[:, k:k + 1], r[:, 0:n], ALU.mult, ALU.mult)
                        dma('sp', out_d[k * 128:(k + 1) * 128, t0 - NCX:t0 - NCX + n], o[:, 0:n])
                P.barrier()

        def dump():
            for k in range(KC):
                dma('sp', dbg_d[k * 128:(k + 1) * 128, :], xT[:, k, :])
            P.barrier()

        done = False
        for l in range(n_layers):
            need_ctx = l < DEPTH - 1
            if l % 2 == 0:
                lru_phase(l, need_ctx)
            else:
                attn_phase(l, need_ctx)
            if stop_after == (l, 'mix'):
                dump()
                done = True
                break
            moe_phase(l, need_ctx)
            if stop_after == (l, 'moe'):
                dump()
                done = True
                break
        if not done:
            final_phase()
            if stop_after is not None:
                dump()
        P.emit()
    return nc, P


def _fm(v):
    v = np.asarray(v, np.float32)
    lead = v.shape[:-1]
    r = v.reshape(lead + (8, 128))
    return np.ascontiguousarray(np.moveaxis(r, -1, 0))


def make_in_maps(inp):
    f32 = np.float32
    x = np.asarray(inp["x"], f32)
    ctx = np.asarray(inp["ctx"], f32)
    c = np.asarray(inp["c"], f32)
    c_ctx = np.asarray(inp["c_ctx"], f32)
    nb = x.shape[0]
    shared = {}
    shared["ada_w"] = np.ascontiguousarray(inp["ada_w"], f32)
    shared["ada_b"] = np.ascontiguousarray(np.asarray(inp["ada_b"], f32).reshape(4, 48, 128).transpose(2, 0, 1))
    shared["n1g"] = _fm(inp["norm1_g"])
    shared["n2g"] = _fm(inp["norm2_g"])
    shared["fing"] = _fm(inp["final_g"])
    shared["lru_w_in"] = np.ascontiguousarray(inp["lru_w_in"], f32)
    shared["lru_w_out"] = np.ascontiguousarray(inp["lru_w_out"], f32)
    cw = np.asarray(inp["lru_conv_w"], f32)
    shared["conv_w"] = np.ascontiguousarray(cw.reshape(2, 4, 8, 128).transpose(3, 0, 2, 1))
    shared["conv_b"] = _fm(inp["lru_conv_b"])
    shared["gate_w"] = np.ascontiguousarray(inp["lru_gate_w"], f32)
    gb = np.asarray(inp["lru_gate_b"], f32)
    shared["gate_b"] = np.ascontiguousarray(gb.reshape(2, 2, 8, 2, 128).transpose(4, 0, 1, 2, 3))
    shared["lam"] = _fm(inp["lru_lambda"])
    wqkv = np.asarray(inp["attn_w_qkv"], f32)
    shared["w_qkv"] = np.ascontiguousarray(wqkv)
    perm = np.concatenate([np.arange(32, 64), np.arange(0, 32), np.arange(96, 128), np.arange(64, 96)])
    cols = (np.arange(10)[:, None] * 128 + perm[None, :]).reshape(-1)
    shared["w_qkv_sw"] = np.ascontiguousarray(wqkv[:, :, cols])
    shared["sink"] = np.ascontiguousarray(np.broadcast_to(np.asarray(inp["attn_sink"], f32)[None], (128, 2, 8)))
    shared["w_o"] = np.ascontiguousarray(inp["attn_w_o"], f32)
    r = np.asarray(inp["moe_router"], f32)
    shared["router"] = np.ascontiguousarray(r.reshape(4, 8, 128, 16).transpose(2, 0, 1, 3))
    shared["moe_w_gate"] = np.ascontiguousarray(inp["moe_w_gate"], f32)
    shared["moe_w_up"] = np.ascontiguousarray(inp["moe_w_up"], f32)
    shared["moe_w_down"] = np.ascontiguousarray(inp["moe_w_down"], f32)
    shared["ident"] = np.eye(128, dtype=f32)
    half = 64
    freqs = (10000.0 ** (-np.arange(0, half, 2, dtype=np.float32) / half)).astype(f32)
    t = np.arange(NL)
    rows = (t // 64).astype(f32)
    colsp = (t % 64).astype(f32)
    rc = np.zeros((128, NL), f32)
    rs = np.zeros((128, NL), f32)
    for p in range(128):
        pos = rows if p < 64 else colsp
        jf = p % 32
        ang = (pos * freqs[jf]).astype(f32)
        rc[p] = np.cos(ang)
        sgn = -1.0 if (p % 64) < 32 else 1.0
        rs[p] = sgn * np.sin(ang)
    shared["rope_c"] = rc
    shared["rope_s"] = rs
    shared["iota_row"] = np.ascontiguousarray(np.broadcast_to(np.arange(NL, dtype=f32)[None], (128, NL)))
    shared["tokidx"] = (np.arange(128, dtype=f32)[:, None] + 128.0 * np.arange(16, dtype=f32)[None, :]).astype(f32)
    s_i = np.arange(128)[:, None]
    q_i = np.arange(128)[None, :]
    shared["mask_prev"] = (s_i >= q_i).astype(f32)
    shared["mask_next"] = (s_i <= q_i).astype(f32)
    shared["halfoff"] = np.concatenate([np.zeros((16, 1), f32), np.full((16, 1), 1024.0, f32)], axis=0)
    shared["pidx16"] = np.ascontiguousarray(np.broadcast_to(np.arange(16, dtype=f32)[:, None], (16, 128)))
    maps = []
    for b in range(nb):
        m = dict(shared)
        m["xT"] = np.ascontiguousarray(np.concatenate([ctx[b].T, x[b].T], axis=1))
        cc = np.stack([c[b].reshape(8, 128).T, c_ctx.reshape(8, 128).T], axis=-1)
        m["cc"] = np.ascontiguousarray(cc, dtype=f32)
        maps.append(m)
    return maps


_CACHE = {}


def kernel(**inputs):
    if "nc" not in _CACHE:
        _CACHE["nc"] = build_program()[0]
    nc = _CACHE["nc"]
    maps = make_in_maps(inputs)
    res = run_bass_kernel_spmd(nc, maps, core_ids=list(range(len(maps))))
    out = np.stack([np.ascontiguousarray(r["outT"].T) for r in res.results], axis=0)
    return out.astype(np.float32)
```
